# Optimizing a Trainium2 kernel written in Bass

```python
import jax, jax.numpy as jnp
from jax import lax
import numpy as np

D_MODEL = 1024
BATCH = 8
SEQ = 4096
DEPTH = 2

GRID_W = 64
CTX_LEN = 256
EPS = 1e-6

HEAD_DIM = 128
N_Q_HEADS = 4
N_KV_HEADS = 2
GROUP = N_Q_HEADS // N_KV_HEADS
ATTN_W = N_Q_HEADS * HEAD_DIM
KV_W = N_KV_HEADS * HEAD_DIM
ROPE_PAIRS = HEAD_DIM // 4
ROPE_THETA = 10000.0
Q_BLOCK = 128

CONV_W = D_MODEL // 2
CONV_K = 3

AB_IN = ATTN_W + 2 * KV_W + 3 * CONV_W
AB_OUT = ATTN_W + CONV_W
AB_SPLITS = (ATTN_W, ATTN_W + KV_W, ATTN_W + 2 * KV_W, ATTN_W + 2 * KV_W + CONV_W, ATTN_W + 2 * KV_W + 2 * CONV_W)

CHUNK = 128
SG_W = D_MODEL
SG_GROUPS = 8
SG_GC = SG_W // SG_GROUPS

N_EXPERTS = 16
EC_FACTOR = 2
D_FF_EXPERT = 2816

N_EVEN = (DEPTH + 1) // 2
N_ODD = DEPTH // 2

kernel_name = 'hybrid_conv_gqa_gmlp_ec_diffusion'


def _rms(x, gain=None):
    xf = x.astype(jnp.float32)
    y = xf * lax.rsqrt(jnp.mean(xf * xf, axis=-1, keepdims=True) + EPS)
    if gain is not None:
        y = y * gain.astype(jnp.float32)
    return y.astype(x.dtype)


def _modulate(x, shift, scale):
    return _rms(x) * (1 + scale) + shift


def _axial_rope_tables(n):
    rows = n // GRID_W
    row_idx = jnp.repeat(jnp.arange(rows, dtype=jnp.float32), GRID_W)
    col_idx = jnp.tile(jnp.arange(GRID_W, dtype=jnp.float32), rows)
    inv_freq = ROPE_THETA ** (-jnp.arange(ROPE_PAIRS, dtype=jnp.float32) / ROPE_PAIRS)
    ang = jnp.stack([row_idx[:, None] * inv_freq, col_idx[:, None] * inv_freq], axis=1)
    return jnp.cos(ang), jnp.sin(ang)


def _apply_rope(x, cos, sin):
    b, n, h, d = x.shape
    xf = x.astype(jnp.float32).reshape(b, n, h, 2, 2, d // 4)
    x0 = xf[..., 0, :]
    x1 = xf[..., 1, :]
    cs = cos[None, :, None]
    sn = sin[None, :, None]
    out = jnp.stack([x0 * cs - x1 * sn, x1 * cs + x0 * sn], axis=-2)
    return out.reshape(b, n, h, d).astype(x.dtype)


def _attend(q, k, v):
    b, lq = q.shape[0], q.shape[1]
    qg = q.reshape(b, lq, N_KV_HEADS, GROUP, HEAD_DIM)
    s = jnp.einsum('bqkgd,bskd->bkgqs', qg, k, preferred_element_type=jnp.float32) * (HEAD_DIM ** -0.5)
    p = jax.nn.softmax(s, axis=-1).astype(v.dtype)
    o = jnp.einsum('bkgqs,bskd->bqkgd', p, v)
    return o.reshape(b, lq, ATTN_W)


def _short_conv(z, w):
    zp = jnp.pad(z, ((0, 0), (1, 1), (0, 0)))
    return zp[:, :-2] * w[0] + zp[:, 1:-1] * w[1] + zp[:, 2:] * w[2]


def _attn_conv_mixer(h_lat, h_ctx, w_in, q_norm, k_norm, conv_w, w_out, cos, sin, ctx_out):
    b, n, _ = h_lat.shape
    m = h_ctx.shape[1]
    q, k, v, gb, gc, hx = jnp.split(h_lat @ w_in, AB_SPLITS, axis=-1)
    q = _apply_rope(_rms(q.reshape(b, n, N_Q_HEADS, HEAD_DIM), q_norm), cos, sin)
    k = _apply_rope(_rms(k.reshape(b, n, N_KV_HEADS, HEAD_DIM), k_norm), cos, sin)
    v = v.reshape(b, n, N_KV_HEADS, HEAD_DIM)
    if ctx_out:
        qc, kc, vc, gbc, gcc, hxc = jnp.split(h_ctx @ w_in, AB_SPLITS, axis=-1)
    else:
        kc, vc = jnp.split(h_ctx @ w_in[:, ATTN_W:ATTN_W + 2 * KV_W], 2, axis=-1)
    kc = _rms(kc.reshape(b, m, N_KV_HEADS, HEAD_DIM), k_norm)
    vc = vc.reshape(b, m, N_KV_HEADS, HEAD_DIM)
    k_all = jnp.concatenate([kc, k], axis=1)
    v_all = jnp.concatenate([vc, v], axis=1)
    qb = q.reshape(b, n // Q_BLOCK, Q_BLOCK, N_Q_HEADS, HEAD_DIM).swapaxes(0, 1)
    a_lat = lax.map(lambda qi: _attend(qi, k_all, v_all), qb).swapaxes(0, 1).reshape(b, n, ATTN_W)
    s_lat = gb * _short_conv(gc * hx, conv_w)
    y_lat = jnp.concatenate([a_lat, s_lat], axis=-1) @ w_out
    y_ctx = None
    if ctx_out:
        qc = _rms(qc.reshape(b, m, N_Q_HEADS, HEAD_DIM), q_norm)
        a_ctx = _attend(qc, kc, vc)
        s_ctx = gbc * _short_conv(gcc * hxc, conv_w)
        y_ctx = jnp.concatenate([a_ctx, s_ctx], axis=-1) @ w_out
    return y_lat, y_ctx


def _chunk_gmlp(h, w_in, sg_norm, sg_w, sg_b, w_out):
    b, n, _ = h.shape
    u, v = jnp.split(jax.nn.gelu(h @ w_in), 2, axis=-1)
    v = _rms(v, sg_norm).reshape(b, n // CHUNK, CHUNK, SG_GROUPS, SG_GC)
    mixed = jnp.einsum('gqp,bnpgc->bnqgc', sg_w, v) + sg_b.T[:, :, None]
    return (u * mixed.reshape(b, n, SG_W)) @ w_out


def _ec_route_one(h, router_w, w1, w3, w2):
    n, d = h.shape
    cap = EC_FACTOR * n // N_EXPERTS
    aff = jax.nn.softmax(jnp.dot(h, router_w, preferred_element_type=jnp.float32), axis=-1)
    g, idx = lax.top_k(aff.T, cap)
    xs = h[idx]
    hid = jax.nn.silu(jnp.einsum('ecd,edf->ecf', xs, w1)) * jnp.einsum('ecd,edf->ecf', xs, w3)
    y = jnp.einsum('ecf,efd->ecd', hid, w2) * g[..., None].astype(h.dtype)
    return jnp.zeros_like(h).at[idx.reshape(-1)].add(y.reshape(-1, d))


def _expert_choice_ffn(h, router_w, w1, w3, w2):
    return lax.map(lambda hb: _ec_route_one(hb, router_w, w1, w3, w2), h)


def setup_inputs(seed: int = 0) -> dict:
    key = jax.random.key(seed)
    ks = jax.random.split(key, 20)
    D = D_MODEL

    def nrm(k, shape, scale):
        return jax.random.normal(k, shape, jnp.float32) * scale

    return {
        'x': nrm(ks[0], (BATCH, SEQ, D), 1.0),
        'c': nrm(ks[1], (BATCH, D), 1.0),
        'ctx': nrm(ks[2], (BATCH, CTX_LEN, D), 1.0),
        'c_ctx': nrm(ks[3], (D,), 1.0),
        'ada_w': nrm(ks[4], (DEPTH, D, 6 * D), 0.5 * D ** -0.5),
        'ada_b': nrm(ks[5], (DEPTH, 6 * D), 0.02),
        'ab_w_in': nrm(ks[6], (N_EVEN, D, AB_IN), D ** -0.5),
        'ab_q_norm': 1.0 + nrm(ks[7], (N_EVEN, HEAD_DIM), 0.02),
        'ab_k_norm': 1.0 + nrm(ks[8], (N_EVEN, HEAD_DIM), 0.02),
        'ab_conv_w': nrm(ks[9], (N_EVEN, CONV_K, CONV_W), CONV_K ** -0.5),
        'ab_w_out': nrm(ks[10], (N_EVEN, AB_OUT, D), AB_OUT ** -0.5),
        'sg_w_in': nrm(ks[11], (N_ODD, D, 2 * SG_W), D ** -0.5),
        'sg_norm': 1.0 + nrm(ks[12], (N_ODD, SG_W), 0.02),
        'sg_w': nrm(ks[13], (N_ODD, SG_GROUPS, CHUNK, CHUNK), CHUNK ** -0.5),
        'sg_b': 1.0 + nrm(ks[14], (N_ODD, SG_GROUPS, CHUNK), 0.02),
        'sg_w_out': nrm(ks[15], (N_ODD, SG_W, D), SG_W ** -0.5),
        'router_w': nrm(ks[16], (DEPTH, D, N_EXPERTS), D ** -0.5),
        'exp_w1': nrm(ks[17], (DEPTH, N_EXPERTS, D, D_FF_EXPERT), D ** -0.5),
        'exp_w3': nrm(ks[18], (DEPTH, N_EXPERTS, D, D_FF_EXPERT), D ** -0.5),
        'exp_w2': nrm(ks[19], (DEPTH, N_EXPERTS, D_FF_EXPERT, D), D_FF_EXPERT ** -0.5),
    }


def reference(x, c, ctx, c_ctx, ada_w, ada_b, ab_w_in, ab_q_norm, ab_k_norm, ab_conv_w, ab_w_out, sg_w_in, sg_norm, sg_w, sg_b, sg_w_out, router_w, exp_w1, exp_w3, exp_w2):
    b, n, _ = x.shape
    cos, sin = _axial_rope_tables(n)
    s_lat = jax.nn.silu(c)
    s_ctx = jax.nn.silu(c_ctx)
    for i in range(DEPTH):
        ctx_out = any(j % 2 == 0 for j in range(i + 1, DEPTH))
        need_ctx_in = (i % 2 == 0) or ctx_out
        mod = (s_lat @ ada_w[i] + ada_b[i]).reshape(b, 6, 1, D_MODEL)
        sh1, sc1, g1, sh2, sc2, g2 = (mod[:, 0], mod[:, 1], mod[:, 2], mod[:, 3], mod[:, 4], mod[:, 5])
        mc = (s_ctx @ ada_w[i] + ada_b[i]).reshape(6, D_MODEL)
        h_lat = _modulate(x, sh1, sc1)
        h_ctx = _modulate(ctx, mc[0], mc[1]) if need_ctx_in else None
        if i % 2 == 0:
            e = i // 2
            y_lat, y_ctx = _attn_conv_mixer(h_lat, h_ctx, ab_w_in[e], ab_q_norm[e], ab_k_norm[e], ab_conv_w[e], ab_w_out[e], cos, sin, ctx_out)
        else:
            o = i // 2
            y_lat = _chunk_gmlp(h_lat, sg_w_in[o], sg_norm[o], sg_w[o], sg_b[o], sg_w_out[o])
            y_ctx = _chunk_gmlp(h_ctx, sg_w_in[o], sg_norm[o], sg_w[o], sg_b[o], sg_w_out[o]) if ctx_out else None
        x = x + g1 * y_lat
        x = x + g2 * _expert_choice_ffn(_modulate(x, sh2, sc2), router_w[i], exp_w1[i], exp_w3[i], exp_w2[i])
        if ctx_out:
            ctx = ctx + mc[2] * y_ctx
            ctx = ctx + mc[5] * _expert_choice_ffn(_modulate(ctx, mc[3], mc[4]), router_w[i], exp_w1[i], exp_w3[i], exp_w2[i])
    return x
```

```python
import numpy as np
from contextlib import ExitStack
import concourse.bass as bass
import concourse.mybir as mybir
from concourse.bass_utils import run_bass_kernel_spmd

F32 = mybir.dt.float32
BF16 = mybir.dt.bfloat16
I32 = mybir.dt.int32
AF = mybir.ActivationFunctionType
ALU = mybir.AluOpType
AX = mybir.AxisListType

N = 4096
D = 1024
NT = 32
CTX = 256
NK = N + CTX
E = 16
CAP = 512
DFF = 2816
NF = 22
EPS = 1e-6
FG = [(0, 4), (4, 4), (8, 4), (12, 4), (16, 3), (19, 3)]
NG = len(FG)
FQ = [q for q, (f0, nf) in enumerate(FG) for _ in range(nf)]
NBIS = 30


class Prog:
    def __init__(self, nc, es, nds=40):
        self.nc = nc
        self.eng = {'pe': nc.tensor, 'act': nc.scalar, 'dve': nc.vector, 'pool': nc.gpsimd, 'sp': nc.sync}
        self.sem = {k: es.enter_context(nc.semaphore('S' + k)) for k in self.eng}
        self.cnt = {k: 0 for k in self.eng}
        self.waited = {k: {} for k in self.eng}
        self.dsem = [es.enter_context(nc.semaphore('Q%d' % i)) for i in range(nds)]
        self.dcnt = [0] * nds
        self.dn = 0
        self.lastw = {}
        self.readers = {}

    def _wait(self, e, tok):
        key, sem, val = tok
        if self.waited[e].get(key, 0) >= val:
            return
        self.eng[e].wait_ge(sem, val)
        self.waited[e][key] = val

    def _deps(self, e, reads, writes):
        for r in reads:
            t = self.lastw.get(r)
            if t is not None and not (t[0] == e and e == 'pe'):
                self._wait(e, t)
        for w in writes:
            t = self.lastw.get(w)
            if t is not None and t[0] != e:
                self._wait(e, t)
            for t in self.readers.get(w, {}).values():
                if t[0] != e:
                    self._wait(e, t)

    def _record(self, tok, reads, writes):
        for w in writes:
            self.lastw[w] = tok
            self.readers[w] = {}
        for r in reads:
            self.readers.setdefault(r, {})[tok[0]] = tok

    def op(self, e, fn, reads=(), writes=()):
        self._deps(e, reads, writes)
        self.cnt[e] += 1
        tok = (e, self.sem[e], self.cnt[e])
        fn(self.eng[e]).then_inc(self.sem[e], 1)
        self._record(tok, reads, writes)

    def dma(self, e, fn, reads=(), writes=()):
        self._deps(e, reads, writes)
        i = self.dn
        self.dn = (self.dn + 1) % len(self.dsem)
        if self.dcnt[i] > 0:
            self._wait(e, ('Q%d' % i, self.dsem[i], self.dcnt[i]))
        self.dcnt[i] += 16
        tok = ('Q%d' % i, self.dsem[i], self.dcnt[i])
        fn(self.eng[e]).then_inc(self.dsem[i], 16)
        self._record(tok, reads, writes)

    def barrier(self):
        for e in self.eng:
            for f in self.eng:
                if f != e and self.cnt[f] > 0:
                    self._wait(e, (f, self.sem[f], self.cnt[f]))
            for i, c in enumerate(self.dcnt):
                if c > 0:
                    self._wait(e, ('Q%d' % i, self.dsem[i], c))
        self.lastw.clear()
        self.readers.clear()


def build(phases=('m0', 'f0', 'm1', 'f1'), dbg=False):
    nc = bass.Bass("TRN2", target_bir_lowering=False)

    def din(name, shape, dt=F32):
        return nc.dram_tensor(name, list(shape), dt, kind="ExternalInput")

    x_in = din("x", [N, D]); ctx_in = din("ctx", [CTX, D])
    ccol = din("ccol", [128, 8]); cccol = din("cccol", [128, 8])
    ada_w = din("ada_w", [2, D, 6 * D]); ada_b = din("ada_b", [2, 6 * D])
    w_in = din("w_in", [D, 3328]); gains = din("gains", [128, 4]); convw = din("convw", [128, 12])
    w_out0 = din("w_out0", [D, D])
    sg_w_in = din("sg_w_in", [D, 2 * D]); sg_norm = din("sg_norm", [1, D])
    sgwT = din("sgwT", [128, 8 * 128]); sgb = din("sgb", [128, 8]); sg_w_out = din("sg_w_out", [D, D])
    router_w = din("router_w", [2, D, E])
    exp_w1 = din("exp_w1", [2, E, D, DFF]); exp_w3 = din("exp_w3", [2, E, D, DFF]); exp_w2 = din("exp_w2", [2, E, DFF, D])
    cos_t = din("cos_t", [128, N]); sin_t = din("sin_t", [128, N])
    ident_in = din("ident", [128, 128]); tri_in = din("tri", [128, 128]); iota_in = din("iota", [128, 512])
    idcols_in = din("idcols", [128, 64])
    out_d = nc.dram_tensor("out", [N, D], F32, kind="ExternalOutput")

    def dscr(name, shape, dt):
        return nc.dram_tensor(name, list(shape), dt, kind="Internal")

    qT_d = dscr("qT_d", [4, 128, N], BF16); kT_d = dscr("kT_d", [2, 128, NK], BF16)
    v_d = dscr("v_d", [NK, 256], BF16); uT_d = dscr("uT_d", [4, 128, N], BF16)
    gbT_d = dscr("gbT_d", [4, 128, N], BF16); sT_d = dscr("sT_d", [4, 128, N], BF16)
    aT_d = dscr("aT_d", [4, 128, N], BF16)
    h_d = dscr("h_d", [N, D], BF16); aff_d = dscr("aff_d", [N, E], F32)

    es = ExitStack()
    with es:
        P = Prog(nc, es)

        uid = [0]

        def sb(st, name, shape, dt=F32):
            uid[0] += 1
            return st.enter_context(nc.sbuf_tensor("s%d_%s" % (uid[0], name), list(shape), dt))

        def ps(st, name, shape, dt=F32):
            uid[0] += 1
            return st.enter_context(nc.psum_tensor("p%d_%s" % (uid[0], name), list(shape), dt))

        ident_f = sb(es, "ident_f", [128, 128]); ident_b = sb(es, "ident_b", [128, 128], BF16)
        ones_b = sb(es, "ones_b", [128, 128], BF16); tri_b = sb(es, "tri_b", [128, 128], BF16)
        iota_j = sb(es, "iota_j", [128, 512]); idcols = sb(es, "idcols", [128, 32, 2], BF16)
        modb = sb(es, "modb", [128, 6, D])
        aff = sb(es, "aff", [128, NT, E])
        idx_i = sb(es, "idx_i", [128, E * 4], I32)
        gall = sb(es, "gall", [128, E * 4])

        P.dma('sp', lambda e: e.dma_start(out=ident_f[:], in_=ident_in.ap()), (), ('ident_f',))
        P.dma('pool', lambda e: e.dma_start(out=ident_b[:], in_=ident_in.ap()), (), ('ident_b',))
        P.dma('pool', lambda e: e.dma_start(out=tri_b[:], in_=tri_in.ap()), (), ('tri_b',))
        P.dma('sp', lambda e: e.dma_start(out=iota_j[:], in_=iota_in.ap()), (), ('iota_j',))
        iota_h = sb(es, "iota_h", [128, 512], mybir.dt.float16)
        P.op('dve', lambda e: e.tensor_copy(out=iota_h[:], in_=iota_j[:]), ('iota_j',), ('iota_h',))
        P.dma('pool', lambda e: e.dma_start(out=idcols[:].rearrange("p t c -> p (t c)"), in_=idcols_in.ap()), (), ('idcols',))
        P.op('dve', lambda e: e.memset(ones_b[:], 1.0), (), ('ones_b',))
        epsc = sb(es, "epsc", [128, 1])
        P.op('dve', lambda e: e.memset(epsc[:], EPS), (), ('epsc',))

        def compute_mod(st, layer, col_in, dst, slots, tag):
            cc = sb(st, "cc" + tag, [128, 8]); sc = sb(st, "sc" + tag, [128, 8])
            scb = sb(st, "scb" + tag, [128, 8, 128])
            awt = [sb(st, "awt%d" % i + tag, [128, 8, 512]) for i in range(2)]
            abt = sb(st, "abt" + tag, [128, 512])
            pm = [ps(st, "pm%d" % i + tag, [128, 512]) for i in range(2)]
            P.dma('sp', lambda e: e.dma_start(out=cc[:], in_=col_in.ap()), (), ('cc',))
            P.op('act', lambda e: e.activation(out=sc[:], in_=cc[:], func=AF.Silu), ('cc',), ('sc',))
            P.op('dve', lambda e: e.tensor_copy(out=scb[:], in_=sc[:].unsqueeze(2).to_broadcast([128, 8, 128])), ('sc',), ('scb',))
            it = 0
            for si, slot in enumerate(slots):
                for hh in range(2):
                    c0 = slot * D + hh * 512
                    b = it % 2
                    P.dma('sp', lambda e, b=b, c0=c0: e.dma_start(
                        out=awt[b][:], in_=ada_w[layer, :, c0:c0 + 512].rearrange("(k p) f -> p k f", p=128)),
                        (), ('awt%d' % b,))
                    P.dma('sp', lambda e, c0=c0: e.dma_start(
                        out=abt[:], in_=ada_b[layer:layer + 1, c0:c0 + 512].to_broadcast([128, 512])), (), ('abt',))
                    for k in range(8):
                        P.op('pe', lambda e, b=b, k=k: e.matmul(pm[b][:], lhsT=scb[:, k, :], rhs=awt[b][:, k, :],
                                                               start=(k == 0), stop=(k == 7)),
                             ('scb', 'awt%d' % b), ('pm%d' % b,))
                    P.op('dve', lambda e, b=b, si=si, hh=hh: e.tensor_tensor(
                        out=dst[:, si, hh * 512:(hh + 1) * 512], in0=pm[b][:], in1=abt[:], op=ALU.add),
                        ('pm%d' % b, 'abt'), ('mod',))
                    it += 1

        def rms_mod(xt, rx, shift, onepsc, outb, ro, tmp, junk, ssq, rstd, kx=""):
            if kx:
                P.op('act', lambda e: e.activation(out=junk[:], in_=xt, func=AF.Square, accum_out=ssq[:]), (rx, 'mod'), ('junk', 'ssq' + kx))
                P.op('act', lambda e: e.activation(out=rstd[:], in_=ssq[:], func=AF.Sqrt, scale=1.0 / D, bias=epsc[:, 0:1]), ('ssq' + kx, 'epsc'), ('rstd' + kx,))
                P.op('dve', lambda e: e.reciprocal(out=rstd[:], in_=rstd[:]), ('rstd' + kx,), ('rstd' + kx,))
                P.op('dve', lambda e: e.scalar_tensor_tensor(out=tmp[:], in0=xt, scalar=rstd[:, 0:1], in1=onepsc, op0=ALU.mult, op1=ALU.mult),
                     (rx, 'rstd' + kx, 'mod'), ('tmp' + kx,))
                P.op('dve', lambda e: e.tensor_tensor(out=outb, in0=tmp[:], in1=shift, op=ALU.add), ('tmp' + kx, 'mod'), (ro,))
                return
            P.op('act', lambda e: e.activation(out=junk[:], in_=xt, func=AF.Square, accum_out=ssq[:]), (rx, 'mod'), ('junk', 'ssq'))
            P.op('act', lambda e: e.activation(out=rstd[:], in_=ssq[:], func=AF.Sqrt, scale=1.0 / D, bias=epsc[:, 0:1]), ('ssq', 'epsc'), ('rstd',))
            P.op('dve', lambda e: e.reciprocal(out=rstd[:], in_=rstd[:]), ('rstd',), ('rstd',))
            P.op('dve', lambda e: e.scalar_tensor_tensor(out=tmp[:], in0=xt, scalar=rstd[:, 0:1], in1=onepsc, op0=ALU.mult, op1=ALU.mult),
                 (rx, 'rstd', 'mod'), ('tmp',))
            P.op('dve', lambda e: e.tensor_tensor(out=outb, in0=tmp[:], in1=shift, op=ALU.add), ('tmp', 'mod'), (ro,))

        class Banks:
            def __init__(self, st, tag):
                self.t = [ps(st, "bk%s%d" % (tag, i), [128, 1024]) for i in range(4)]
                self.i = 0

            def one(self):
                i = self.i
                self.i = (i + 1) % 8
                return self.t[i // 2][:, (i % 2) * 512:(i % 2 + 1) * 512], ('bk', i)

            def two(self):
                if self.i % 2:
                    self.i = (self.i + 1) % 8
                i = self.i
                self.i = (i + 2) % 8
                return self.t[i // 2][:, :], (('bk', i), ('bk', i + 1))

        def interleave(gens, G, stagger=0):
            it = iter(gens)
            active = []
            if stagger:
                g0 = next(it)
                for _ in range(stagger):
                    next(g0)
                active.append(g0)
            while True:
                while len(active) < G:
                    try:
                        active.append(next(it))
                    except StopIteration:
                        break
                if not active:
                    break
                for g in list(active):
                    try:
                        next(g)
                    except StopIteration:
                        active.remove(g)

        def transpose_mm(bk, src, rsrc):
            pt, kk = bk.two()
            for k in range(8):
                P.op('pe', lambda e, k=k: e.matmul(pt[:, k * 128:(k + 1) * 128], lhsT=src[:, k * 128:(k + 1) * 128], rhs=ident_b[:],
                                                   start=True, stop=True), (rsrc, 'ident_b'), kk)
            return pt, kk

        def ffn_prep_bufs(st, G):
            L = []
            for g in range(G):
                d = {}
                sfx = "_%d" % g
                d['sfx'] = sfx
                d['ssq'] = sb(st, "fq_ssq" + sfx, [128, 1]); d['rstd'] = sb(st, "fq_rstd" + sfx, [128, 1])
                d['tmp'] = sb(st, "fq_tmp" + sfx, [128, D]); d['h2'] = sb(st, "fq_h2" + sfx, [128, D]); d['h2b'] = sb(st, "fq_h2b" + sfx, [128, D], BF16)
                d['h2T'] = sb(st, "fq_h2T" + sfx, [128, 8, 128])
                d['ex'] = sb(st, "fq_ex" + sfx, [128, E]); d['esum'] = sb(st, "fq_esum" + sfx, [128, 1]); d['erec'] = sb(st, "fq_erec" + sfx, [128, 1])
                L.append(d)
            return L

        def ffn_prep_g(bk, d, rw, junk, x1, rx1, T):
            sfx = d['sfx']
            P.op('act', lambda e: e.activation(out=junk[:], in_=x1, func=AF.Square, accum_out=d['ssq'][:]), (rx1,), ('junk', 'ssq' + sfx))
            P.op('act', lambda e: e.activation(out=d['rstd'][:], in_=d['ssq'][:], func=AF.Sqrt, scale=1.0 / D, bias=epsc[:, 0:1]), ('ssq' + sfx, 'epsc'), ('rstd' + sfx,))
            P.op('dve', lambda e: e.reciprocal(out=d['rstd'][:], in_=d['rstd'][:]), ('rstd' + sfx,), ('rstd' + sfx,))
            P.op('dve', lambda e: e.scalar_tensor_tensor(out=d['tmp'][:], in0=x1, scalar=d['rstd'][:, 0:1], in1=modb[:, 4, :], op0=ALU.mult, op1=ALU.mult),
                 (rx1, 'rstd' + sfx, 'mod'), ('tmp' + sfx,))
            P.op('dve', lambda e: e.tensor_tensor(out=d['h2'][:], in0=d['tmp'][:], in1=modb[:, 3, :], op=ALU.add), ('tmp' + sfx, 'mod'), ('h2' + sfx,))
            P.op('act', lambda e: e.activation(out=d['h2b'][:], in_=d['h2'][:], func=AF.Copy), ('h2' + sfx,), ('h2b' + sfx,))
            P.dma('pool', lambda e: e.dma_start(out=h_d[T * 128:(T + 1) * 128, :], in_=d['h2b'][:]), ('h2b' + sfx,), (('h_d', T),))
            pt32, kk = bk.two()
            for k in range(8):
                P.op('pe', lambda e, k=k: e.transpose(pt32[:, k * 128:(k + 1) * 128], d['h2'][:, k * 128:(k + 1) * 128], ident_f[:]),
                     ('h2' + sfx, 'ident_f'), kk)
            yield
            P.op('dve', lambda e: e.tensor_copy(out=d['h2T'][:].rearrange("p k t -> p (k t)"), in_=pt32), kk, ('h2T' + sfx,))
            pr, kr = bk.one()
            for k in range(8):
                P.op('pe', lambda e, k=k: e.matmul(pr[:, 0:E], lhsT=d['h2T'][:, k, :], rhs=rw[:, k, :], start=(k == 0), stop=(k == 7)),
                     ('h2T' + sfx, 'fp_rw'), (kr,))
            yield
            P.op('act', lambda e: e.activation(out=d['ex'][:], in_=pr[:, 0:E], func=AF.Exp, accum_out=d['esum'][:]), (kr,), ('ex' + sfx, 'esum' + sfx))
            P.op('dve', lambda e: e.reciprocal(out=d['erec'][:], in_=d['esum'][:]), ('esum' + sfx,), ('erec' + sfx,))
            P.op('dve', lambda e: e.tensor_scalar(out=aff[:, T, :], in0=d['ex'][:], scalar1=d['erec'][:, 0:1], scalar2=None, op0=ALU.mult),
                 ('ex' + sfx, 'erec' + sfx), (('aff', T),))

        def ffn_prep_alloc(st):
            d = {}
            d['junk'] = sb(st, "fp_junk", [128, D]); d['ssq'] = sb(st, "fp_ssq", [128, 1]); d['rstd'] = sb(st, "fp_rstd", [128, 1])
            d['tmp'] = sb(st, "fp_tmp", [128, D]); d['h2'] = sb(st, "fp_h2", [128, D]); d['h2b'] = sb(st, "fp_h2b", [128, D], BF16)
            d['h2T'] = sb(st, "fp_h2T", [128, 8, 128]); d['rw'] = sb(st, "fp_rw", [128, 8, E])
            d['ex'] = sb(st, "fp_ex", [128, E]); d['esum'] = sb(st, "fp_esum", [128, 1]); d['erec'] = sb(st, "fp_erec", [128, 1])
            d['pt32'] = ps(st, "fp_pt32", [128, 1024]); d['pr'] = ps(st, "fp_pr", [128, E])
            return d

        def ffn_prep(d, layer, x1, rx1, T):
            rms_mod(x1, rx1, modb[:, 3, :], modb[:, 4, :], d['h2'][:], 'fp_h2', d['tmp'], d['junk'], d['ssq'], d['rstd'])
            P.op('act', lambda e: e.activation(out=d['h2b'][:], in_=d['h2'][:], func=AF.Copy), ('fp_h2',), ('fp_h2b',))
            P.dma('sp', lambda e: e.dma_start(out=h_d[T * 128:(T + 1) * 128, :], in_=d['h2b'][:]), ('fp_h2b',), (('h_d', T),))
            for k in range(8):
                P.op('pe', lambda e, k=k: e.transpose(d['pt32'][:, k * 128:(k + 1) * 128], d['h2'][:, k * 128:(k + 1) * 128], ident_f[:]),
                     ('fp_h2', 'ident_f'), ('fp_pt32',))
            P.op('dve', lambda e: e.tensor_copy(out=d['h2T'][:].rearrange("p k t -> p (k t)"), in_=d['pt32'][:]), ('fp_pt32',), ('fp_h2T',))
            for k in range(8):
                P.op('pe', lambda e, k=k: e.matmul(d['pr'][:], lhsT=d['h2T'][:, k, :], rhs=d['rw'][:, k, :], start=(k == 0), stop=(k == 7)),
                     ('fp_h2T', 'fp_rw'), ('fp_pr',))
            P.op('act', lambda e: e.activation(out=d['ex'][:], in_=d['pr'][:], func=AF.Exp, accum_out=d['esum'][:]), ('fp_pr',), ('fp_ex', 'fp_esum'))
            P.op('dve', lambda e: e.reciprocal(out=d['erec'][:], in_=d['esum'][:]), ('fp_esum',), ('fp_erec',))
            P.op('dve', lambda e: e.tensor_scalar(out=aff[:, T, :], in0=d['ex'][:], scalar1=d['erec'][:, 0:1], scalar2=None, op0=ALU.mult),
                 ('fp_ex', 'fp_erec'), (('aff', T),))

        def load_router(d, layer):
            P.dma('sp', lambda e: e.dma_start(out=d['rw'][:], in_=router_w[layer].rearrange("(k p) e -> p k e", p=128)), (), ('fp_rw',))

        def mixer0():
            st = ExitStack()
            with st:
                modc = sb(st, "modc", [128, 2, D])
                with ExitStack() as s2:
                    compute_mod(s2, 0, ccol, modb, [0, 1, 2, 3, 4, 5], "a")
                    P.barrier()
                with ExitStack() as s2:
                    compute_mod(s2, 0, cccol, modc, [0, 1], "b")
                    P.barrier()
                P.op('dve', lambda e: e.tensor_scalar(out=modb[:, 1, :], in0=modb[:, 1, :], scalar1=1.0, scalar2=None, op0=ALU.add), ('mod',), ('mod',))
                P.op('dve', lambda e: e.tensor_scalar(out=modb[:, 4, :], in0=modb[:, 4, :], scalar1=1.0, scalar2=None, op0=ALU.add), ('mod',), ('mod',))
                P.op('dve', lambda e: e.tensor_scalar(out=modc[:, 1, :], in0=modc[:, 1, :], scalar1=1.0, scalar2=None, op0=ALU.add), ('mod',), ('mod',))
                P.barrier()
                with ExitStack() as sa:
                    wi = sb(sa, "wi", [128, 8, 3328], BF16)
                    cosb = sb(sa, "cosb", [128, N]); sinb = sb(sa, "sinb", [128, N])
                    gn = sb(sa, "gn", [128, 4])
                    xt = [sb(sa, "xt%d" % i, [128, D]) for i in range(2)]
                    junk = sb(sa, "junk", [128, D])
                    tmp = [sb(sa, "tmp%d" % i, [128, D]) for i in range(2)]
                    ssq = [sb(sa, "ssq%d" % i, [128, 1]) for i in range(2)]; rstd = [sb(sa, "rstd%d" % i, [128, 1]) for i in range(2)]
                    hb = [sb(sa, "hb%d" % i, [128, D], BF16) for i in range(2)]
                    hT = [sb(sa, "hT%d" % i, [128, 8, 512], BF16) for i in range(2)]
                    sqb = [sb(sa, "sqb%d" % i, [128, 512], BF16) for i in range(2)]
                    r1 = [sb(sa, "r1_%d" % i, [128, 512]) for i in range(2)]
                    qn = [sb(sa, "qn%d" % i, [128, 512]) for i in range(2)]; qsn = [sb(sa, "qsn%d" % i, [128, 512]) for i in range(2)]
                    qf = [sb(sa, "qf%d" % i, [128, 512], BF16) for i in range(2)]
                    vb = [sb(sa, "vb%d" % i, [128, 256], BF16) for i in range(2)]
                    gcs = [sb(sa, "gcs%d" % i, [128, 512]) for i in range(2)]
                    ub = [sb(sa, "ub%d" % i, [128, 512], BF16) for i in range(2)]
                    gbb = [sb(sa, "gbb%d" % i, [128, 512], BF16) for i in range(2)]
                    bk = Banks(sa, "pa")

                    for hh in range(2):
                        P.dma('pool', lambda e, hh=hh: e.dma_start(
                            out=wi[:, :, hh * 1664:(hh + 1) * 1664],
                            in_=w_in[:, hh * 1664:(hh + 1) * 1664].rearrange("(k p) f -> p k f", p=128)), (), ('wi',))
                    P.dma('sp', lambda e: e.dma_start(out=cosb[:], in_=cos_t.ap()), (), ('cosb',))
                    P.dma('sp', lambda e: e.dma_start(out=sinb[:], in_=sin_t.ap()), (), ('sinb',))
                    P.dma('sp', lambda e: e.dma_start(out=gn[:], in_=gains.ap()), (), ('gn',))

                    def tiles_stage(s):
                        isctx = s < 0
                        sp = (s + 1) % 2
                        for t in range(2 if isctx else 4):
                            b = t % 2
                            src = ctx_in[t * 128:(t + 1) * 128, :] if isctx else x_in[(s * 4 + t) * 128:(s * 4 + t + 1) * 128, :]
                            P.dma('sp', lambda e: e.dma_start(out=xt[b][:], in_=src), (), ('xt%d' % b,))
                            shift = modc[:, 0, :] if isctx else modb[:, 0, :]
                            onep = modc[:, 1, :] if isctx else modb[:, 1, :]
                            rms_mod(xt[b][:], 'xt%d' % b, shift, onep, hb[b][:], 'hb%d' % b, tmp[b], junk, ssq[b], rstd[b], kx="_a%d" % b)
                            pt, kk = transpose_mm(bk, hb[b], 'hb%d' % b)
                            P.op('act', lambda e: e.activation(out=hT[sp][:, :, t * 128:(t + 1) * 128],
                                                               in_=pt.rearrange("p (k t) -> p k t", k=8), func=AF.Copy), kk, ('hT%d' % sp,))

                    def make_chains(s):
                        isctx = s < 0
                        sp = (s + 1) % 2
                        ntile = 2 if isctx else 4
                        ntok = ntile * 128
                        hTk = 'hT%d' % sp
                        chains = []

                        def proj(c0, n=ntok):
                            pt, kp = bk.one()
                            for k in range(8):
                                P.op('pe', lambda e, k=k: e.matmul(pt[:, 0:n], lhsT=wi[:, k, c0:c0 + 128], rhs=hT[sp][:, k, 0:n],
                                                                   start=(k == 0), stop=(k == 7)), ('wi', hTk), (kp,))
                            return pt, kp

                        def qk_chain(ci, c0, c1, gc0, kind, hidx):
                            st_ = {}
                            i2 = ci % 2

                            def do_proj():
                                st_['pq'] = proj(c0)
                                if not isctx:
                                    st_['pqs'] = proj(c1)

                            def do_post():
                                pq, kq = st_['pq']
                                P.op('act', lambda e: e.activation(out=sqb[i2][:, 0:ntok], in_=pq[:, 0:ntok], func=AF.Square), (kq,), ('sqb%d' % i2,))
                                pss, kss = bk.one()
                                P.op('pe', lambda e: e.matmul(pss[:, 0:ntok], lhsT=ones_b[:], rhs=sqb[i2][:, 0:ntok], start=True, stop=True),
                                     ('sqb%d' % i2, 'ones_b'), (kss,))
                                P.op('act', lambda e: e.activation(out=r1[i2][:, 0:ntok], in_=pss[:, 0:ntok], func=AF.Ln, scale=1.0 / 128, bias=epsc[:, 0:1]),
                                     (kss, 'epsc'), ('r1_%d' % i2,))
                                P.op('act', lambda e: e.activation(out=r1[i2][:, 0:ntok], in_=r1[i2][:, 0:ntok], func=AF.Exp, scale=-0.5), ('r1_%d' % i2,), ('r1_%d' % i2,))
                                if isctx:
                                    P.op('dve', lambda e: e.scalar_tensor_tensor(
                                        out=qf[i2][:, 0:ntok], in0=pq[:, 0:ntok], scalar=gn[:, gc0:gc0 + 1], in1=r1[i2][:, 0:ntok],
                                        op0=ALU.mult, op1=ALU.mult), (kq, 'gn', 'r1_%d' % i2), ('qf%d' % i2,))
                                    P.dma('pool', lambda e: e.dma_start(out=kT_d[hidx, :, 0:CTX], in_=qf[i2][:, 0:CTX]), ('qf%d' % i2,), ())
                                    return
                                pqs, kqs = st_['pqs']
                                P.op('dve', lambda e: e.scalar_tensor_tensor(
                                    out=qn[i2][:], in0=pq, scalar=gn[:, gc0:gc0 + 1], in1=r1[i2][:], op0=ALU.mult, op1=ALU.mult),
                                    (kq, 'gn', 'r1_%d' % i2), ('qn%d' % i2,))
                                P.op('dve', lambda e: e.scalar_tensor_tensor(
                                    out=qsn[i2][:], in0=pqs, scalar=gn[:, gc0 + 1:gc0 + 2], in1=r1[i2][:], op0=ALU.mult, op1=ALU.mult),
                                    (kqs, 'gn', 'r1_%d' % i2), ('qsn%d' % i2,))
                                P.op('dve', lambda e: e.tensor_tensor(out=qn[i2][:], in0=qn[i2][:], in1=cosb[:, s * 512:(s + 1) * 512], op=ALU.mult),
                                     ('qn%d' % i2, 'cosb'), ('qn%d' % i2,))
                                P.op('dve', lambda e: e.tensor_tensor(out=qsn[i2][:], in0=qsn[i2][:], in1=sinb[:, s * 512:(s + 1) * 512], op=ALU.mult),
                                     ('qsn%d' % i2, 'sinb'), ('qsn%d' % i2,))
                                P.op('dve', lambda e: e.tensor_tensor(out=qf[i2][:], in0=qn[i2][:], in1=qsn[i2][:], op=ALU.add),
                                     ('qn%d' % i2, 'qsn%d' % i2), ('qf%d' % i2,))
                                if kind == 'q':
                                    P.dma('pool', lambda e: e.dma_start(out=qT_d[hidx, :, s * 512:(s + 1) * 512], in_=qf[i2][:]), ('qf%d' % i2,), ())
                                else:
                                    P.dma('pool', lambda e: e.dma_start(out=kT_d[hidx, :, CTX + s * 512:CTX + (s + 1) * 512], in_=qf[i2][:]),
                                          ('qf%d' % i2,), ())
                            return do_proj, do_post

                        def v_chain(t):
                            st_ = {}
                            b = t % 2

                            def do_proj():
                                pv, kv_ = bk.one()
                                for k in range(8):
                                    P.op('pe', lambda e, k=k: e.matmul(pv[:, 0:256], lhsT=hT[sp][:, k, t * 128:(t + 1) * 128], rhs=wi[:, k, 768:1024],
                                                                       start=(k == 0), stop=(k == 7)), ('wi', hTk), (kv_,))
                                st_['pv'] = (pv, kv_)

                            def do_post():
                                pv, kv_ = st_['pv']
                                P.op('act', lambda e: e.activation(out=vb[b][:], in_=pv[:, 0:256], func=AF.Copy), (kv_,), ('vb%d' % b,))
                                row0 = t * 128 if isctx else CTX + (s * 4 + t) * 128
                                P.dma('pool', lambda e: e.dma_start(out=v_d[row0:row0 + 128, :], in_=vb[b][:]), ('vb%d' % b,), ())
                            return do_proj, do_post

                        def conv_chain(c):
                            st_ = {}
                            b = c % 2

                            def do_proj():
                                st_['gb'] = proj(1024 + c * 128); st_['gc'] = proj(1536 + c * 128); st_['hx'] = proj(2048 + c * 128)

                            def do_post():
                                (pgb, kgb), (pgc, kgc), (phx, khx) = st_['gb'], st_['gc'], st_['hx']
                                P.op('act', lambda e: e.activation(out=gcs[b][:], in_=pgc, func=AF.Copy), (kgc,), ('gcs%d' % b,))
                                P.op('dve', lambda e: e.tensor_tensor(out=ub[b][:], in0=phx, in1=gcs[b][:], op=ALU.mult), (khx, 'gcs%d' % b), ('ub%d' % b,))
                                P.op('act', lambda e: e.activation(out=gbb[b][:], in_=pgb, func=AF.Copy), (kgb,), ('gbb%d' % b,))
                                P.dma('pool', lambda e: e.dma_start(out=uT_d[c, :, s * 512:(s + 1) * 512], in_=ub[b][:]), ('ub%d' % b,), ())
                                P.dma('pool', lambda e: e.dma_start(out=gbT_d[c, :, s * 512:(s + 1) * 512], in_=gbb[b][:]), ('gbb%d' % b,), ())
                            return do_proj, do_post

                        ci = 0
                        if not isctx:
                            for h in range(4):
                                chains.append(qk_chain(ci, h * 128, 2560 + h * 128, 0, 'q', h)); ci += 1
                                chains.append(conv_chain(h))
                        for kv in range(2):
                            chains.append(qk_chain(ci, 512 + kv * 128, 3072 + kv * 128, 2, 'k', kv)); ci += 1
                            for t in range(kv * ntile // 2, (kv + 1) * ntile // 2):
                                chains.append(v_chain(t))
                        return chains

                    tiles_stage(-1)
                    for s in range(-1, 8):
                        if s + 1 < 8:
                            tiles_stage(s + 1)
                        pend = None
                        for (pj, po) in make_chains(s):
                            pj()
                            if pend is not None:
                                pend()
                            pend = po
                        pend()
                    P.barrier()
                with ExitStack() as sbk:
                    cw = sb(sbk, "cw", [128, 12])
                    u = sb(sbk, "u", [128, N + 2], BF16); acc = sb(sbk, "acc", [128, N])
                    gbt = sb(sbk, "gbt", [128, N], BF16); so = sb(sbk, "so", [128, N], BF16)
                    P.dma('sp', lambda e: e.dma_start(out=cw[:], in_=convw.ap()), (), ('cw',))
                    for c in range(4):
                        P.op('dve', lambda e: e.memset(u[:, 0:1], 0.0), (), ('u',))
                        P.op('dve', lambda e: e.memset(u[:, N + 1:N + 2], 0.0), (), ('u',))
                        P.dma('sp', lambda e, c=c: e.dma_start(out=u[:, 1:N + 1], in_=uT_d[c]), (), ('u',))
                        P.dma('sp', lambda e, c=c: e.dma_start(out=gbt[:], in_=gbT_d[c]), (), ('gbt',))
                        P.op('dve', lambda e, c=c: e.tensor_scalar(out=acc[:], in0=u[:, 1:N + 1], scalar1=cw[:, c * 3 + 1:c * 3 + 2], scalar2=None,
                                                                   op0=ALU.mult), ('u', 'cw'), ('acc',))
                        P.op('dve', lambda e, c=c: e.scalar_tensor_tensor(out=acc[:], in0=u[:, 0:N], scalar=cw[:, c * 3:c * 3 + 1], in1=acc[:],
                                                                          op0=ALU.mult, op1=ALU.add), ('u', 'cw', 'acc'), ('acc',))
                        P.op('dve', lambda e, c=c: e.scalar_tensor_tensor(out=acc[:], in0=u[:, 2:N + 2], scalar=cw[:, c * 3 + 2:c * 3 + 3], in1=acc[:],
                                                                          op0=ALU.mult, op1=ALU.add), ('u', 'cw', 'acc'), ('acc',))
                        P.op('dve', lambda e: e.tensor_tensor(out=so[:], in0=acc[:], in1=gbt[:], op=ALU.mult), ('acc', 'gbt'), ('so',))
                        P.dma('sp', lambda e, c=c: e.dma_start(out=sT_d[c], in_=so[:]), ('so',), ())
                    P.barrier()
                with ExitStack() as sc:
                    kT = sb(sc, "kT", [128, 2, NK], BF16); vv = sb(sc, "vv", [128, 34, 256], BF16)
                    qs = [sb(sc, "qs%d" % i, [128, 512], BF16) for i in range(2)]
                    pb = [sb(sc, "pb%d" % i, [128, 512], BF16) for i in range(3)]
                    rec = sb(sc, "rec", [128, 512]); ab = [sb(sc, "ab%d" % i, [128, 512], BF16) for i in range(2)]
                    pS = [ps(sc, "pS%d" % i, [128, 512]) for i in range(3)]
                    pO = [ps(sc, "pO%d" % i, [128, 512]) for i in range(2)]
                    pD = [ps(sc, "pD%d" % i, [128, 512]) for i in range(2)]
                    P.dma('sp', lambda e: e.dma_start(out=kT[:], in_=kT_d.ap().rearrange("k p n -> p k n")), (), ('kT',))
                    P.dma('sp', lambda e: e.dma_start(out=vv[:], in_=v_d.ap().rearrange("(c p) f -> p c f", p=128)), (), ('vv',))
                    groups = [(h, s_) for h in range(4) for s_ in range(8)]
                    iters = [(g, c) for g in range(len(groups)) for c in range(34)]

                    def load_q(g):
                        h, s_ = groups[g]
                        b = g % 2
                        P.dma('sp', lambda e: e.dma_start(out=qs[b][:], in_=qT_d[h, :, s_ * 512:(s_ + 1) * 512]), (), ('qs%d' % b,))

                    def qk_exp(i):
                        g, c = iters[i]
                        h, s_ = groups[g]
                        kv = h // 2
                        b = g % 2
                        j = i % 3
                        if c == 0 and g + 1 < len(groups):
                            load_q(g + 1)
                        P.op('pe', lambda e: e.matmul(pS[j][:], lhsT=kT[:, kv, c * 128:(c + 1) * 128], rhs=qs[b][:],
                                                      start=True, stop=True), ('kT', 'qs%d' % b), ('pS%d' % j,))
                        P.op('act', lambda e: e.activation(out=pb[j][:], in_=pS[j][:], func=AF.Exp, scale=float(128 ** -0.5)),
                             ('pS%d' % j,), ('pb%d' % j,))

                    load_q(0)
                    qk_exp(0)
                    qk_exp(1)
                    for i, (g, c) in enumerate(iters):
                        h, s_ = groups[g]
                        kv = h // 2
                        b = g % 2
                        j = i % 3
                        if i + 2 < len(iters):
                            qk_exp(i + 2)
                        P.op('pe', lambda e: e.matmul(pO[b][:], lhsT=vv[:, c, kv * 128:(kv + 1) * 128], rhs=pb[j][:],
                                                      start=(c == 0), stop=(c == 33)), ('vv', 'pb%d' % j), ('pO%d' % b,))
                        P.op('pe', lambda e: e.matmul(pD[b][:], lhsT=ones_b[:], rhs=pb[j][:],
                                                      start=(c == 0), stop=(c == 33)), ('ones_b', 'pb%d' % j), ('pD%d' % b,))
                        if c == 33:
                            P.op('dve', lambda e: e.reciprocal(out=rec[:], in_=pD[b][:]), ('pD%d' % b,), ('rec',))
                            P.op('dve', lambda e: e.tensor_tensor(out=ab[b][:], in0=pO[b][:], in1=rec[:], op=ALU.mult),
                                 ('pO%d' % b, 'rec'), ('ab%d' % b,))
                            P.dma('pool', lambda e: e.dma_start(out=aT_d[h, :, s_ * 512:(s_ + 1) * 512], in_=ab[b][:]), ('ab%d' % b,), ())
                    P.barrier()
                with ExitStack() as sd:
                    G = 2
                    wo = sb(sd, "wo", [128, 8, D], BF16)
                    asl = [sb(sd, "asl%d" % i, [128, 8, 512], BF16) for i in range(2)]
                    xt = [sb(sd, "dxt%d" % i, [128, D]) for i in range(G)]
                    x1 = [sb(sd, "x1_%d" % i, [128, D]) for i in range(G)]
                    rw = sb(sd, "drw", [128, 8, E]); junk = sb(sd, "djunk", [128, D])
                    fpb = ffn_prep_bufs(sd, G)
                    bk = Banks(sd, "pd")
                    P.dma('sp', lambda e: e.dma_start(out=rw[:], in_=router_w[0].rearrange("(k p) e -> p k e", p=128)), (), ('fp_rw',))
                    P.dma('pool', lambda e: e.dma_start(out=wo[:], in_=w_out0.ap().rearrange("(k p) f -> p k f", p=128)), (), ('wo',))

                    def load_slab(s):
                        b = s % 2
                        P.dma('sp', lambda e: e.dma_start(out=asl[b][:, 0:4, :], in_=aT_d[:, :, s * 512:(s + 1) * 512].rearrange("h p t -> p h t")),
                              (), ('asl%d' % b,))
                        P.dma('sp', lambda e: e.dma_start(out=asl[b][:, 4:8, :], in_=sT_d[:, :, s * 512:(s + 1) * 512].rearrange("h p t -> p h t")),
                              (), ('asl%d' % b,))

                    def tile_gen(T):
                        s, t = T // 4, T % 4
                        b = s % 2
                        g = T % G
                        sx = "_%d" % g
                        if t == 0 and s + 1 < 8:
                            load_slab(s + 1)
                        P.dma('sp', lambda e: e.dma_start(out=xt[g][:], in_=x_in[T * 128:(T + 1) * 128, :]), (), ('dxt' + sx,))
                        pys = []
                        for hh in range(2):
                            py_, ky = bk.one()
                            pys.append((py_, ky))
                            for k in range(8):
                                P.op('pe', lambda e, k=k, hh=hh, py_=py_: e.matmul(
                                    py_, lhsT=asl[b][:, k, t * 128:(t + 1) * 128], rhs=wo[:, k, hh * 512:(hh + 1) * 512],
                                    start=(k == 0), stop=(k == 7)), ('asl%d' % b, 'wo'), (ky,))
                        yield
                        for hh in range(2):
                            py_, ky = pys[hh]
                            P.op('dve', lambda e, hh=hh, py_=py_: e.tensor_tensor(out=x1[g][:, hh * 512:(hh + 1) * 512], in0=py_,
                                                                                 in1=modb[:, 2, hh * 512:(hh + 1) * 512], op=ALU.mult),
                                 (ky, 'mod'), ('x1' + sx,))
                        P.op('dve', lambda e: e.tensor_tensor(out=x1[g][:], in0=x1[g][:], in1=xt[g][:], op=ALU.add), ('x1' + sx, 'dxt' + sx), ('x1' + sx,))
                        P.dma('pool', lambda e: e.dma_start(out=out_d[T * 128:(T + 1) * 128, :], in_=x1[g][:]), ('x1' + sx,), (('xd', T),))
                        yield from ffn_prep_g(bk, fpb[g], rw, junk, x1[g][:], 'x1' + sx, T)

                    load_slab(0)
                    interleave([tile_gen(T) for T in range(NT)], G, stagger=2)
                    P.barrier()

        def mixer1():
            G = 2
            with ExitStack() as st:
                with ExitStack() as s2:
                    compute_mod(s2, 1, ccol, modb, [0, 1, 2, 3, 4, 5], "c")
                    P.barrier()
                P.op('dve', lambda e: e.tensor_scalar(out=modb[:, 1, :], in0=modb[:, 1, :], scalar1=1.0, scalar2=None, op0=ALU.add), ('mod',), ('mod',))
                P.op('dve', lambda e: e.tensor_scalar(out=modb[:, 4, :], in0=modb[:, 4, :], scalar1=1.0, scalar2=None, op0=ALU.add), ('mod',), ('mod',))
                P.barrier()
                wi = sb(st, "gwi", [128, 8, 2 * D], BF16); wo = sb(st, "gwo", [128, 8, D], BF16)
                swT = sb(st, "swT", [128, 8, 128], BF16); sbq = sb(st, "sbq", [128, 8]); snb = sb(st, "snb", [128, D])
                rw = sb(st, "grw", [128, 8, E]); junk = sb(st, "gjunk", [128, D])
                xt = [sb(st, "gxt%d" % i, [128, D]) for i in range(G)]
                tmp = [sb(st, "gtmp%d" % i, [128, D]) for i in range(G)]
                ssq = [sb(st, "gssq%d" % i, [128, 1]) for i in range(G)]; rstd = [sb(st, "grstd%d" % i, [128, 1]) for i in range(G)]
                hb = [sb(st, "ghb%d" % i, [128, D], BF16) for i in range(G)]; hT = [sb(st, "ghT%d" % i, [128, 8, 128], BF16) for i in range(G)]
                z = [sb(st, "gz%d" % i, [128, 2 * D]) for i in range(G)]; vnb = [sb(st, "gvnb%d" % i, [128, D], BF16) for i in range(G)]
                mb = [sb(st, "gmb%d" % i, [128, D], BF16) for i in range(G)]; mT = [sb(st, "gmT%d" % i, [128, 8, 128], BF16) for i in range(G)]
                x3 = [sb(st, "x3_%d" % i, [128, D]) for i in range(G)]
                fpb = ffn_prep_bufs(st, G)
                bk = Banks(st, "m1")
                P.dma('sp', lambda e: e.dma_start(out=rw[:], in_=router_w[1].rearrange("(k p) e -> p k e", p=128)), (), ('fp_rw',))
                for hh in range(2):
                    P.dma('pool', lambda e, hh=hh: e.dma_start(out=wi[:, :, hh * D:(hh + 1) * D],
                                                               in_=sg_w_in[:, hh * D:(hh + 1) * D].rearrange("(k p) f -> p k f", p=128)), (), ('gwi',))
                P.dma('pool', lambda e: e.dma_start(out=wo[:], in_=sg_w_out.ap().rearrange("(k p) f -> p k f", p=128)), (), ('gwo',))
                P.dma('pool', lambda e: e.dma_start(out=swT[:].rearrange("p g q -> p (g q)"), in_=sgwT.ap()), (), ('swT',))
                P.dma('sp', lambda e: e.dma_start(out=sbq[:], in_=sgb.ap()), (), ('sbq',))
                P.dma('sp', lambda e: e.dma_start(out=snb[:], in_=sg_norm.ap().to_broadcast([128, D])), (), ('snb',))

                def tile_gen(T):
                    g = T % G
                    sx = "_%d" % g
                    P.dma('sp', lambda e: e.dma_start(out=xt[g][:], in_=out_d[T * 128:(T + 1) * 128, :]), (('xd', T),), ('gxt' + sx,))
                    P.op('act', lambda e: e.activation(out=junk[:], in_=xt[g][:], func=AF.Square, accum_out=ssq[g][:]), ('gxt' + sx,), ('junk', 'gssq' + sx))
                    P.op('act', lambda e: e.activation(out=rstd[g][:], in_=ssq[g][:], func=AF.Sqrt, scale=1.0 / D, bias=epsc[:, 0:1]), ('gssq' + sx, 'epsc'), ('grstd' + sx,))
                    P.op('dve', lambda e: e.reciprocal(out=rstd[g][:], in_=rstd[g][:]), ('grstd' + sx,), ('grstd' + sx,))
                    P.op('dve', lambda e: e.scalar_tensor_tensor(out=tmp[g][:], in0=xt[g][:], scalar=rstd[g][:, 0:1], in1=modb[:, 1, :], op0=ALU.mult, op1=ALU.mult),
                         ('gxt' + sx, 'grstd' + sx, 'mod'), ('gtmp' + sx,))
                    P.op('dve', lambda e: e.tensor_tensor(out=hb[g][:], in0=tmp[g][:], in1=modb[:, 0, :], op=ALU.add), ('gtmp' + sx, 'mod'), ('ghb' + sx,))
                    pt, kk = transpose_mm(bk, hb[g], 'ghb' + sx)
                    yield
                    P.op('act', lambda e: e.activation(out=hT[g][:].rearrange("p k t -> p (k t)"), in_=pt, func=AF.Copy), kk, ('ghT' + sx,))
                    for n in range(4):
                        pz, kz = bk.one()
                        for k in range(8):
                            P.op('pe', lambda e, k=k, n=n, pz=pz: e.matmul(pz, lhsT=hT[g][:, k, :], rhs=wi[:, k, n * 512:(n + 1) * 512],
                                                                          start=(k == 0), stop=(k == 7)), ('ghT' + sx, 'gwi'), (kz,))
                        P.op('act', lambda e, n=n, pz=pz: e.activation(out=z[g][:, n * 512:(n + 1) * 512], in_=pz, func=AF.Gelu_apprx_tanh),
                             (kz,), (('gz' + sx, n),))
                        if n == 1:
                            yield
                    yield
                    zv = (('gz' + sx, 2), ('gz' + sx, 3))
                    P.op('act', lambda e: e.activation(out=junk[:], in_=z[g][:, D:2 * D], func=AF.Square, accum_out=ssq[g][:]), zv, ('junk', 'gssq' + sx))
                    P.op('act', lambda e: e.activation(out=rstd[g][:], in_=ssq[g][:], func=AF.Sqrt, scale=1.0 / D, bias=epsc[:, 0:1]), ('gssq' + sx, 'epsc'), ('grstd' + sx,))
                    P.op('dve', lambda e: e.reciprocal(out=rstd[g][:], in_=rstd[g][:]), ('grstd' + sx,), ('grstd' + sx,))
                    P.op('dve', lambda e: e.scalar_tensor_tensor(out=vnb[g][:], in0=z[g][:, D:2 * D], scalar=rstd[g][:, 0:1], in1=snb[:], op0=ALU.mult, op1=ALU.mult),
                         zv + ('grstd' + sx, 'snb'), ('gvnb' + sx,))
                    pm, km = bk.two()
                    for gg in range(8):
                        P.op('pe', lambda e, gg=gg: e.matmul(pm[:, gg * 128:(gg + 1) * 128], lhsT=swT[:, gg, :], rhs=vnb[g][:, gg * 128:(gg + 1) * 128],
                                                             start=True, stop=True), ('swT', 'gvnb' + sx), km)
                    yield
                    for gg in range(8):
                        P.op('dve', lambda e, gg=gg: e.scalar_tensor_tensor(out=mb[g][:, gg * 128:(gg + 1) * 128], in0=pm[:, gg * 128:(gg + 1) * 128],
                                                                            scalar=sbq[:, gg:gg + 1], in1=z[g][:, gg * 128:(gg + 1) * 128],
                                                                            op0=ALU.add, op1=ALU.mult),
                             km + ('sbq', ('gz' + sx, 0), ('gz' + sx, 1)), ('gmb' + sx,))
                    pt2, kk2 = transpose_mm(bk, mb[g], 'gmb' + sx)
                    yield
                    P.op('act', lambda e: e.activation(out=mT[g][:].rearrange("p k t -> p (k t)"), in_=pt2, func=AF.Copy), kk2, ('gmT' + sx,))
                    pys = []
                    for hh in range(2):
                        py_, ky = bk.one()
                        pys.append((py_, ky))
                        for k in range(8):
                            P.op('pe', lambda e, k=k, hh=hh, py_=py_: e.matmul(py_, lhsT=mT[g][:, k, :], rhs=wo[:, k, hh * 512:(hh + 1) * 512],
                                                                              start=(k == 0), stop=(k == 7)), ('gmT' + sx, 'gwo'), (ky,))
                    yield
                    for hh in range(2):
                        py_, ky = pys[hh]
                        P.op('dve', lambda e, hh=hh, py_=py_: e.tensor_tensor(out=x3[g][:, hh * 512:(hh + 1) * 512], in0=py_,
                                                                             in1=modb[:, 2, hh * 512:(hh + 1) * 512], op=ALU.mult),
                             (ky, 'mod'), ('x3' + sx,))
                    P.op('dve', lambda e: e.tensor_tensor(out=x3[g][:], in0=x3[g][:], in1=xt[g][:], op=ALU.add), ('x3' + sx, 'gxt' + sx), ('x3' + sx,))
                    P.dma('pool', lambda e: e.dma_start(out=out_d[T * 128:(T + 1) * 128, :], in_=x3[g][:]), ('x3' + sx,), (('xd', T),))
                    yield from ffn_prep_g(bk, fpb[g], rw, junk, x3[g][:], 'x3' + sx, T)

                interleave([tile_gen(T) for T in range(NT)], G, stagger=4)
                P.barrier()

        def ffn(layer):
            with ExitStack() as se:
                lo = sb(se, "lo", [128, E]); hi = sb(se, "hi", [128, E]); mid = sb(se, "mid", [128, E])
                cmpb = sb(se, "cmpb", [128, NT, E], BF16); cnt = sb(se, "cnt", [128, E]); ge = sb(se, "ge", [128, E])
                t1 = sb(se, "t1", [128, E]); t2 = sb(se, "t2", [128, E])
                mf = sb(se, "mf", [128, NT, E]); offs = sb(se, "offs", [128, NT, E]); tot = sb(se, "tot", [128, NT, E])
                gpos = sb(se, "gpos", [128, NT, E])
                ohb = [sb(se, "ohb%d" % i, [128, 512], BF16) for i in range(4)]
                idf = sb(se, "idf", [128, E * 4])
                pc = ps(se, "pc", [128, 512]); pA = ps(se, "pA", [128, 512]); pB = ps(se, "pB", [128, 512])
                pid = ps(se, "pid", [128, E * 4 * 4])
                idg = sb(se, "idg", [128, NT, E, 4], BF16); hif = sb(se, "hif", [128, NT, E])
                affr = tuple(('aff', T) for T in range(NT))
                P.op('dve', lambda e: e.memset(lo[:], 0.0), (), ('lo',))
                P.op('dve', lambda e: e.memset(hi[:], 1.0), (), ('hi',))
                P.op('dve', lambda e: e.memset(mid[:], 0.5), (), ('mid',))
                for it in range(NBIS):
                    P.op('dve', lambda e: e.tensor_tensor(out=cmpb[:], in0=aff[:], in1=mid[:].unsqueeze(1).to_broadcast([128, NT, E]), op=ALU.is_ge),
                         affr + ('mid',), ('cmpb',))
                    P.op('pe', lambda e: e.matmul(pc[:], lhsT=ones_b[:], rhs=cmpb[:].rearrange("p t e -> p (t e)"), start=True, stop=True),
                         ('cmpb', 'ones_b'), ('pc',))
                    P.op('dve', lambda e: e.tensor_reduce(out=cnt[:], in_=pc[:].rearrange("p (t e) -> p e t", e=E), axis=AX.X, op=ALU.add),
                         ('pc',), ('cnt',))
                    P.op('dve', lambda e: e.tensor_scalar(out=ge[:], in0=cnt[:], scalar1=float(CAP), scalar2=None, op0=ALU.is_ge), ('cnt',), ('ge',))
                    P.op('dve', lambda e: e.tensor_tensor(out=t1[:], in0=ge[:], in1=mid[:], op=ALU.mult), ('ge', 'mid'), ('t1',))
                    P.op('dve', lambda e: e.tensor_tensor(out=lo[:], in0=lo[:], in1=t1[:], op=ALU.max), ('lo', 't1'), ('lo',))
                    P.op('dve', lambda e: e.scalar_tensor_tensor(out=t2[:], in0=ge[:], scalar=2.0, in1=mid[:], op0=ALU.mult, op1=ALU.add),
                         ('ge', 'mid'), ('t2',))
                    P.op('dve', lambda e: e.tensor_tensor(out=hi[:], in0=hi[:], in1=t2[:], op=ALU.min), ('hi', 't2'), ('hi',))
                    P.op('dve', lambda e: e.tensor_tensor(out=mid[:], in0=lo[:], in1=hi[:], op=ALU.add), ('lo', 'hi'), ('mid',))
                    P.op('dve', lambda e: e.tensor_scalar(out=mid[:], in0=mid[:], scalar1=0.5, scalar2=None, op0=ALU.mult), ('mid',), ('mid',))
                P.op('dve', lambda e: e.tensor_tensor(out=mf[:], in0=aff[:], in1=lo[:].unsqueeze(1).to_broadcast([128, NT, E]), op=ALU.is_ge),
                     affr + ('lo',), ('mf',))
                P.op('dve', lambda e: e.tensor_copy(out=cmpb[:], in_=mf[:]), ('mf',), ('cmpb',))
                P.op('pe', lambda e: e.matmul(pA[:], lhsT=tri_b[:], rhs=cmpb[:].rearrange("p t e -> p (t e)"), start=True, stop=True),
                     ('cmpb', 'tri_b'), ('pA',))
                P.op('pe', lambda e: e.matmul(pB[:], lhsT=ones_b[:], rhs=cmpb[:].rearrange("p t e -> p (t e)"), start=True, stop=True),
                     ('cmpb', 'ones_b'), ('pB',))
                P.op('dve', lambda e: e.tensor_copy(out=tot[:].rearrange("p t e -> p (t e)"), in_=pB[:]), ('pB',), ('tot',))
                P.op('dve', lambda e: e.memset(offs[:, 0, :], 0.0), (), ('offs',))
                for T in range(1, NT):
                    P.op('dve', lambda e, T=T: e.tensor_tensor(out=offs[:, T, :], in0=offs[:, T - 1, :], in1=tot[:, T - 1, :], op=ALU.add),
                         ('offs', 'tot'), ('offs',))
                P.op('dve', lambda e: e.tensor_tensor(out=gpos[:].rearrange("p t e -> p (t e)"), in0=pA[:], in1=offs[:].rearrange("p t e -> p (t e)"), op=ALU.add),
                     ('pA', 'offs'), ('gpos',))
                P.op('dve', lambda e: e.tensor_tensor(out=gpos[:], in0=gpos[:], in1=mf[:], op=ALU.mult), ('gpos', 'mf'), ('gpos',))
                P.op('dve', lambda e: e.tensor_scalar(out=gpos[:], in0=gpos[:], scalar1=-1.0, scalar2=None, op0=ALU.add), ('gpos',), ('gpos',))
                P.op('dve', lambda e: e.tensor_copy(out=idg[:, :, :, 0:2], in_=idcols[:].unsqueeze(2).to_broadcast([128, NT, E, 2])), ('idcols',), ('idg',))
                P.op('dve', lambda e: e.tensor_copy(out=idg[:, :, :, 2], in_=aff[:]), affr, ('idg',))
                P.op('dve', lambda e: e.tensor_copy(out=hif[:], in_=idg[:, :, :, 2]), ('idg',), ('hif',))
                P.op('dve', lambda e: e.tensor_tensor(out=idg[:, :, :, 3], in0=aff[:], in1=hif[:], op=ALU.subtract), affr + ('hif',), ('idg',))
                first = True
                n = 0
                for T in range(NT):
                    for ex in range(E):
                        b = n % 4
                        eng = 'dve'
                        P.op(eng, lambda e, b=b, T=T, ex=ex: e.tensor_scalar(out=ohb[b][:], in0=iota_h[:], scalar1=gpos[:, T, ex:ex + 1], scalar2=None,
                                                                             op0=ALU.is_equal), ('iota_h', 'gpos'), ('ohb%d' % b,))
                        for jt in range(4):
                            col = (ex * 4 + jt) * 4
                            P.op('pe', lambda e, b=b, jt=jt, col=col, T=T, ex=ex, first=first: e.matmul(
                                pid[:, col:col + 4], lhsT=ohb[b][:, jt * 128:(jt + 1) * 128], rhs=idg[:, T, ex, :],
                                start=first, stop=(T == NT - 1), skip_group_check=True), ('ohb%d' % b, 'idg'), ('pid',))
                            first = False
                        n += 1
                P.op('dve', lambda e: e.tensor_reduce(out=idf[:], in_=pid[:].rearrange("p (n c) -> p n c", c=4)[:, :, 0:2], axis=AX.X, op=ALU.add), ('pid',), ('idf',))
                P.op('dve', lambda e: e.tensor_reduce(out=gall[:], in_=pid[:].rearrange("p (n c) -> p n c", c=4)[:, :, 2:4], axis=AX.X, op=ALU.add), ('pid',), ('gall',))
                P.op('dve', lambda e: e.tensor_copy(out=idx_i[:], in_=idf[:]), ('idf',), ('idx_i',))
                P.barrier()
            with ExitStack() as sf:
                US = [sb(sf, "US%d" % i, [128, 2, 8, 512], BF16) for i in range(3)]
                VS = [sb(sf, "VS%d" % i, [128, 4, D], BF16) for i in range(NG)]
                xs = [sb(sf, "xs%d" % i, [128, 4, D], BF16) for i in range(2)]
                xsT = [sb(sf, "xsT%d" % i, [128, 8, 512], BF16) for i in range(2)]
                hid = sb(sf, "hid", [128, NF, 512], BF16)
                sg = [sb(sf, "sg%d" % i, [128, 512]) for i in range(2)]
                ysb = [sb(sf, "ysb%d" % i, [128, D]) for i in range(4)]
                pst = ps(sf, "fpst", [128, 1024], BF16)
                ph1 = [ps(sf, "ph1_%d" % i, [128, 512]) for i in range(2)]
                ph3 = [ps(sf, "ph3_%d" % i, [128, 512]) for i in range(2)]
                py = [ps(sf, "fpy%d" % i, [128, 512]) for i in range(2)]
                allxd = tuple(('xd', T) for T in range(NT))

                def load_U(ex, q):
                    u = ex * NG + q
                    slot = u % 3
                    f0, nf = FG[q]
                    for wi_, W in enumerate((exp_w1, exp_w3)):
                        P.dma('pool', lambda e, W=W, wi_=wi_, slot=slot, f0=f0, nf=nf, ex=ex: e.dma_start(
                            out=US[slot][:, wi_, :, 0:nf * 128],
                            in_=W[layer, ex, :, f0 * 128:(f0 + nf) * 128].rearrange("(k p) f -> p k f", p=128)), (), ('US%d' % slot,))

                def load_V(ex, q):
                    f0, nf = FG[q]
                    P.dma('pool', lambda e, q=q, f0=f0, nf=nf, ex=ex: e.dma_start(
                        out=VS[q][:, 0:nf, :], in_=exp_w2[layer, ex, f0 * 128:(f0 + nf) * 128, :].rearrange("(f p) d -> p f d", p=128)),
                        (), ('VS%d' % q,))

                def gathers(ex):
                    b = ex % 2
                    for jt in range(4):
                        col = ex * 4 + jt
                        P.dma('pool', lambda e, b=b, jt=jt, col=col: e.indirect_dma_start(
                            out=xs[b][:, jt, :], out_offset=None, in_=h_d[:, :],
                            in_offset=bass.IndirectOffsetOnAxis(ap=idx_i[:, col:col + 1], axis=0)),
                            ('idx_i',), (('xs', b, jt),))

                def transposes(ex, jt):
                    b = ex % 2
                    for k in range(8):
                        P.op('pe', lambda e, k=k: e.transpose(pst[:, k * 128:(k + 1) * 128], xs[b][:, jt, k * 128:(k + 1) * 128], ident_b[:]),
                             (('xs', b, jt), 'ident_b'), ('fpst',))
                    P.op('act', lambda e: e.activation(out=xsT[b][:, :, jt * 128:(jt + 1) * 128],
                                                       in_=pst[:].rearrange("p (k t) -> p k t", k=8), func=AF.Copy), ('fpst',), ('xsT%d' % b,))

                gathers(0)
                for q in range(3):
                    load_U(0, q)
                for q in range(NG):
                    load_V(0, q)
                for jt in range(4):
                    transposes(0, jt)
                for ex in range(E):
                    b = ex % 2
                    for q in range(NG):
                        u = ex * NG + q
                        slot = u % 3
                        f0, nf = FG[q]
                        for fl in range(nf):
                            f = f0 + fl
                            pb_ = f % 2
                            for k in range(8):
                                P.op('pe', lambda e, slot=slot, fl=fl, k=k, pb_=pb_: e.matmul(
                                    ph1[pb_][:], lhsT=US[slot][:, 0, k, fl * 128:(fl + 1) * 128], rhs=xsT[b][:, k, :], start=(k == 0), stop=(k == 7)),
                                    ('US%d' % slot, 'xsT%d' % b), ('ph1_%d' % pb_,))
                            for k in range(8):
                                P.op('pe', lambda e, slot=slot, fl=fl, k=k, pb_=pb_: e.matmul(
                                    ph3[pb_][:], lhsT=US[slot][:, 1, k, fl * 128:(fl + 1) * 128], rhs=xsT[b][:, k, :], start=(k == 0), stop=(k == 7)),
                                    ('US%d' % slot, 'xsT%d' % b), ('ph3_%d' % pb_,))
                            P.op('act', lambda e, pb_=pb_: e.activation(out=sg[pb_][:], in_=ph1[pb_][:], func=AF.Silu), ('ph1_%d' % pb_,), ('sg%d' % pb_,))
                            P.op('dve', lambda e, pb_=pb_, f=f: e.tensor_tensor(out=hid[:, f, :], in0=ph3[pb_][:], in1=sg[pb_][:], op=ALU.mult),
                                 ('ph3_%d' % pb_, 'sg%d' % pb_), (('hid', f),))
                        un = u + 3
                        if un < E * NG:
                            load_U(un // NG, un % NG)
                        if q == 0 and ex + 1 < E:
                            gathers(ex + 1)
                        if q == 2 and ex > 0:
                            for qq in range(NG):
                                load_V(ex, qq)
                    gi = 0
                    for jt in range(4):
                        for hh in range(2):
                            for f in range(NF):
                                q = FQ[f]
                                fl = f - FG[q][0]
                                P.op('pe', lambda e, jt=jt, hh=hh, f=f, q=q, fl=fl: e.matmul(
                                    py[hh][:], lhsT=hid[:, f, jt * 128:(jt + 1) * 128], rhs=VS[q][:, fl, hh * 512:(hh + 1) * 512],
                                    start=(f == 0), stop=(f == NF - 1)), (('hid', f), 'VS%d' % q), ('fpy%d' % hh,))
                            P.op('dve', lambda e, jt=jt, hh=hh, ex=ex: e.scalar_tensor_tensor(
                                out=ysb[jt][:, hh * 512:(hh + 1) * 512], in0=py[hh][:], scalar=gall[:, ex * 4 + jt:ex * 4 + jt + 1],
                                in1=modb[:, 5, hh * 512:(hh + 1) * 512], op0=ALU.mult, op1=ALU.mult),
                                ('fpy%d' % hh, 'gall', 'mod'), ('ysb%d' % jt,))
                            if ex + 1 < E and gi >= 4:
                                transposes(ex + 1, gi - 4)
                            gi += 1
                        col = ex * 4 + jt
                        P.dma('pool', lambda e, jt=jt, col=col: e.indirect_dma_start(
                            out=out_d[:, :], out_offset=bass.IndirectOffsetOnAxis(ap=idx_i[:, col:col + 1], axis=0),
                            in_=ysb[jt][:, :], in_offset=None, compute_op=ALU.add),
                            ('ysb%d' % jt, 'idx_i'), allxd)
                P.barrier()

        if 'm0' in phases:
            mixer0()
        if 'f0' in phases:
            ffn(0)
        if 'm1' in phases:
            mixer1()
        if 'f1' in phases:
            ffn(1)
        P.barrier()
    return nc


def _consts():
    n = N
    rows = n // 64
    row_idx = np.repeat(np.arange(rows, dtype=np.float32), 64)
    col_idx = np.tile(np.arange(64, dtype=np.float32), rows)
    inv_freq = (np.float32(10000.0) ** (-np.arange(32, dtype=np.float32) / np.float32(32))).astype(np.float32)
    ang_r = (row_idx[None, :] * inv_freq[:, None]).astype(np.float32)
    ang_c = (col_idx[None, :] * inv_freq[:, None]).astype(np.float32)
    cos_t = np.concatenate([np.cos(ang_r), np.cos(ang_r), np.cos(ang_c), np.cos(ang_c)], 0).astype(np.float32)
    sin_t = np.concatenate([-np.sin(ang_r), np.sin(ang_r), -np.sin(ang_c), np.sin(ang_c)], 0).astype(np.float32)
    ident = np.eye(128, dtype=np.float32)
    tri = np.triu(np.ones((128, 128), dtype=np.float32))
    iota = np.tile(np.arange(512, dtype=np.float32)[None, :], (128, 1))
    idcols = np.zeros((128, 32, 2), dtype=np.float32)
    idcols[:, :, 0] = np.arange(128, dtype=np.float32)[:, None]
    idcols[:, :, 1] = (128.0 * np.arange(32, dtype=np.float32))[None, :]
    return dict(cos_t=np.ascontiguousarray(cos_t), sin_t=np.ascontiguousarray(sin_t), ident=ident, tri=tri, iota=iota,
                idcols=np.ascontiguousarray(idcols.reshape(128, 64)))


def _swap_perm():
    p = np.arange(128)
    blk = p // 32
    return (blk ^ 1) * 32 + (p % 32)


def prepare_shared(inputs):
    f = lambda a: np.ascontiguousarray(np.asarray(a, dtype=np.float32))
    perm = _swap_perm()
    w = np.asarray(inputs['ab_w_in'][0], dtype=np.float32)
    qsw = np.concatenate([w[:, h * 128:(h + 1) * 128][:, perm] for h in range(4)], 1)
    ksw = np.concatenate([w[:, 512 + h * 128:512 + (h + 1) * 128][:, perm] for h in range(2)], 1)
    w_in_ext = np.concatenate([w, qsw, ksw], 1)
    qn = np.asarray(inputs['ab_q_norm'][0], dtype=np.float32); kn = np.asarray(inputs['ab_k_norm'][0], dtype=np.float32)
    gains = np.stack([qn, qn[perm], kn, kn[perm]], 1)
    cw = np.asarray(inputs['ab_conv_w'][0], dtype=np.float32)
    convw = cw.reshape(3, 4, 128).transpose(2, 1, 0).reshape(128, 12)
    sgw = np.asarray(inputs['sg_w'][0], dtype=np.float32)
    sgwT = sgw.transpose(2, 0, 1).reshape(128, 8 * 128)
    sgb = np.asarray(inputs['sg_b'][0], dtype=np.float32).T
    cctx = np.asarray(inputs['c_ctx'], dtype=np.float32)
    sh = dict(
        cccol=f(cctx.reshape(8, 128).T), ada_w=f(inputs['ada_w']), ada_b=f(inputs['ada_b']),
        w_in=f(w_in_ext), gains=f(gains), convw=f(convw), w_out0=f(inputs['ab_w_out'][0]),
        sg_w_in=f(inputs['sg_w_in'][0]), sg_norm=f(np.asarray(inputs['sg_norm'][0]).reshape(1, D)),
        sgwT=f(sgwT), sgb=f(sgb), sg_w_out=f(inputs['sg_w_out'][0]), router_w=f(inputs['router_w']),
        exp_w1=f(inputs['exp_w1']), exp_w3=f(inputs['exp_w3']), exp_w2=f(inputs['exp_w2']),
    )
    sh.update(_consts())
    return sh


def core_inputs(inputs, shared, b):
    m = dict(shared)
    m['x'] = np.ascontiguousarray(np.asarray(inputs['x'][b], dtype=np.float32))
    m['ctx'] = np.ascontiguousarray(np.asarray(inputs['ctx'][b], dtype=np.float32))
    m['ccol'] = np.ascontiguousarray(np.asarray(inputs['c'][b], dtype=np.float32).reshape(8, 128).T)
    return m


def kernel(**inputs):
    nb = inputs['x'].shape[0]
    shared = prepare_shared(inputs)
    nc = build()
    in_maps = [core_inputs(inputs, shared, b) for b in range(nb)]
    res = run_bass_kernel_spmd(nc, in_maps, core_ids=list(range(nb)))
    return np.stack([np.asarray(r['out'], dtype=np.float32) for r in res.results], 0)
```

```python
import numpy as np
from contextlib import ExitStack
import concourse.bass as bass
import concourse.mybir as mybir
from concourse.bass_utils import run_bass_kernel_spmd

F32 = mybir.dt.float32
BF16 = mybir.dt.bfloat16
I32 = mybir.dt.int32
AF = mybir.ActivationFunctionType
ALU = mybir.AluOpType
AX = mybir.AxisListType

N = 4096
D = 1024
NT = 32
CTX = 256
NK = N + CTX
E = 16
CAP = 512
DFF = 2816
NF = 22
EPS = 1e-6
FG = [(0, 4), (4, 4), (8, 4), (12, 4), (16, 3), (19, 3)]
NG = len(FG)
FQ = [q for q, (f0, nf) in enumerate(FG) for _ in range(nf)]
NBIS = 30


class Prog:
    def __init__(self, nc, es, nds=40):
        self.nc = nc
        self.eng = {'pe': nc.tensor, 'act': nc.scalar, 'dve': nc.vector, 'pool': nc.gpsimd, 'sp': nc.sync}
        self.sem = {k: es.enter_context(nc.semaphore('S' + k)) for k in self.eng}
        self.cnt = {k: 0 for k in self.eng}
        self.waited = {k: {} for k in self.eng}
        self.dsem = [es.enter_context(nc.semaphore('Q%d' % i)) for i in range(nds)]
        self.dcnt = [0] * nds
        self.dn = 0
        self.lastw = {}
        self.readers = {}

    def _wait(self, e, tok):
        key, sem, val = tok
        if self.waited[e].get(key, 0) >= val:
            return
        self.eng[e].wait_ge(sem, val)
        self.waited[e][key] = val

    def _deps(self, e, reads, writes):
        for r in reads:
            t = self.lastw.get(r)
            if t is not None and not (t[0] == e and e == 'pe'):
                self._wait(e, t)
        for w in writes:
            t = self.lastw.get(w)
            if t is not None and t[0] != e:
                self._wait(e, t)
            for t in self.readers.get(w, {}).values():
                if t[0] != e:
                    self._wait(e, t)

    def _record(self, tok, reads, writes):
        for w in writes:
            self.lastw[w] = tok
            self.readers[w] = {}
        for r in reads:
            self.readers.setdefault(r, {})[tok[0]] = tok

    def op(self, e, fn, reads=(), writes=()):
        self._deps(e, reads, writes)
        self.cnt[e] += 1
        tok = (e, self.sem[e], self.cnt[e])
        fn(self.eng[e]).then_inc(self.sem[e], 1)
        self._record(tok, reads, writes)

    def dma(self, e, fn, reads=(), writes=()):
        self._deps(e, reads, writes)
        i = self.dn
        self.dn = (self.dn + 1) % len(self.dsem)
        if self.dcnt[i] > 0:
            self._wait(e, ('Q%d' % i, self.dsem[i], self.dcnt[i]))
        self.dcnt[i] += 16
        tok = ('Q%d' % i, self.dsem[i], self.dcnt[i])
        fn(self.eng[e]).then_inc(self.dsem[i], 16)
        self._record(tok, reads, writes)

    def barrier(self):
        for e in self.eng:
            for f in self.eng:
                if f != e and self.cnt[f] > 0:
                    self._wait(e, (f, self.sem[f], self.cnt[f]))
            for i, c in enumerate(self.dcnt):
                if c > 0:
                    self._wait(e, ('Q%d' % i, self.dsem[i], c))
        self.lastw.clear()
        self.readers.clear()


def build(phases=('m0', 'f0', 'm1', 'f1'), dbg=False):
    nc = bass.Bass("TRN2", target_bir_lowering=False)

    def din(name, shape, dt=F32):
        return nc.dram_tensor(name, list(shape), dt, kind="ExternalInput")

    x_in = din("x", [N, D]); ctx_in = din("ctx", [CTX, D])
    ccol = din("ccol", [128, 8]); cccol = din("cccol", [128, 8])
    ada_w = din("ada_w", [2, D, 6 * D]); ada_b = din("ada_b", [2, 6 * D])
    w_in = din("w_in", [D, 3328]); gains = din("gains", [128, 4]); convw = din("convw", [128, 12])
    w_out0 = din("w_out0", [D, D])
    sg_w_in = din("sg_w_in", [D, 2 * D]); sg_norm = din("sg_norm", [1, D])
    sgwT = din("sgwT", [128, 8 * 128]); sgb = din("sgb", [128, 8]); sg_w_out = din("sg_w_out", [D, D])
    router_w = din("router_w", [2, D, E])
    exp_w1 = din("exp_w1", [2, E, D, DFF]); exp_w3 = din("exp_w3", [2, E, D, DFF]); exp_w2 = din("exp_w2", [2, E, DFF, D])
    cos_t = din("cos_t", [128, N]); sin_t = din("sin_t", [128, N])
    ident_in = din("ident", [128, 128]); tri_in = din("tri", [128, 128]); iota_in = din("iota", [128, 512])
    idcols_in = din("idcols", [128, 64])
    out_d = nc.dram_tensor("out", [N, D], F32, kind="ExternalOutput")

    def dscr(name, shape, dt):
        return nc.dram_tensor(name, list(shape), dt, kind="Internal")

    qT_d = dscr("qT_d", [4, 128, N], BF16); kT_d = dscr("kT_d", [2, 128, NK], BF16)
    v_d = dscr("v_d", [NK, 256], BF16); uT_d = dscr("uT_d", [4, 128, N], BF16)
    gbT_d = dscr("gbT_d", [4, 128, N], BF16); sT_d = dscr("sT_d", [4, 128, N], BF16)
    aT_d = dscr("aT_d", [4, 128, N], BF16)
    h_d = dscr("h_d", [N, D], BF16); aff_d = dscr("aff_d", [N, E], F32)

    es = ExitStack()
    with es:
        P = Prog(nc, es)

        uid = [0]

        def sb(st, name, shape, dt=F32):
            uid[0] += 1
            return st.enter_context(nc.sbuf_tensor("s%d_%s" % (uid[0], name), list(shape), dt))

        def ps(st, name, shape, dt=F32):
            uid[0] += 1
            return st.enter_context(nc.psum_tensor("p%d_%s" % (uid[0], name), list(shape), dt))

        ident_f = sb(es, "ident_f", [128, 128]); ident_b = sb(es, "ident_b", [128, 128], BF16)
        ones_b = sb(es, "ones_b", [128, 128], BF16); tri_b = sb(es, "tri_b", [128, 128], BF16)
        iota_j = sb(es, "iota_j", [128, 512]); idcols = sb(es, "idcols", [128, 32, 2], BF16)
        modb = sb(es, "modb", [128, 6, D])
        aff = sb(es, "aff", [128, NT, E])
        idx_i = sb(es, "idx_i", [128, E * 4], I32)
        gall = sb(es, "gall", [128, E * 4])

        P.dma('sp', lambda e: e.dma_start(out=ident_f[:], in_=ident_in.ap()), (), ('ident_f',))
        P.dma('pool', lambda e: e.dma_start(out=ident_b[:], in_=ident_in.ap()), (), ('ident_b',))
        P.dma('pool', lambda e: e.dma_start(out=tri_b[:], in_=tri_in.ap()), (), ('tri_b',))
        P.dma('sp', lambda e: e.dma_start(out=iota_j[:], in_=iota_in.ap()), (), ('iota_j',))
        iota_h = sb(es, "iota_h", [128, 512], mybir.dt.float16)
        P.op('dve', lambda e: e.tensor_copy(out=iota_h[:], in_=iota_j[:]), ('iota_j',), ('iota_h',))
        P.dma('pool', lambda e: e.dma_start(out=idcols[:].rearrange("p t c -> p (t c)"), in_=idcols_in.ap()), (), ('idcols',))
        P.op('dve', lambda e: e.memset(ones_b[:], 1.0), (), ('ones_b',))
        epsc = sb(es, "epsc", [128, 1])
        P.op('dve', lambda e: e.memset(epsc[:], EPS), (), ('epsc',))

        def compute_mod(st, layer, col_in, dst, slots, tag):
            cc = sb(st, "cc" + tag, [128, 8]); sc = sb(st, "sc" + tag, [128, 8])
            scb = sb(st, "scb" + tag, [128, 8, 128])
            awt = [sb(st, "awt%d" % i + tag, [128, 8, 512]) for i in range(2)]
            abt = sb(st, "abt" + tag, [128, 512])
            pm = [ps(st, "pm%d" % i + tag, [128, 512]) for i in range(2)]
            P.dma('sp', lambda e: e.dma_start(out=cc[:], in_=col_in.ap()), (), ('cc',))
            P.op('act', lambda e: e.activation(out=sc[:], in_=cc[:], func=AF.Silu), ('cc',), ('sc',))
            P.op('dve', lambda e: e.tensor_copy(out=scb[:], in_=sc[:].unsqueeze(2).to_broadcast([128, 8, 128])), ('sc',), ('scb',))
            it = 0
            for si, slot in enumerate(slots):
                for hh in range(2):
                    c0 = slot * D + hh * 512
                    b = it % 2
                    P.dma('sp', lambda e, b=b, c0=c0: e.dma_start(
                        out=awt[b][:], in_=ada_w[layer, :, c0:c0 + 512].rearrange("(k p) f -> p k f", p=128)),
                        (), ('awt%d' % b,))
                    P.dma('sp', lambda e, c0=c0: e.dma_start(
                        out=abt[:], in_=ada_b[layer:layer + 1, c0:c0 + 512].to_broadcast([128, 512])), (), ('abt',))
                    for k in range(8):
                        P.op('pe', lambda e, b=b, k=k: e.matmul(pm[b][:], lhsT=scb[:, k, :], rhs=awt[b][:, k, :],
                                                               start=(k == 0), stop=(k == 7)),
                             ('scb', 'awt%d' % b), ('pm%d' % b,))
                    P.op('dve', lambda e, b=b, si=si, hh=hh: e.tensor_tensor(
                        out=dst[:, si, hh * 512:(hh + 1) * 512], in0=pm[b][:], in1=abt[:], op=ALU.add),
                        ('pm%d' % b, 'abt'), ('mod',))
                    it += 1

        def rms_mod(xt, rx, shift, onepsc, outb, ro, tmp, junk, ssq, rstd, kx=""):
            if kx:
                P.op('act', lambda e: e.activation(out=junk[:], in_=xt, func=AF.Square, accum_out=ssq[:]), (rx, 'mod'), ('junk', 'ssq' + kx))
                P.op('act', lambda e: e.activation(out=rstd[:], in_=ssq[:], func=AF.Sqrt, scale=1.0 / D, bias=epsc[:, 0:1]), ('ssq' + kx, 'epsc'), ('rstd' + kx,))
                P.op('dve', lambda e: e.reciprocal(out=rstd[:], in_=rstd[:]), ('rstd' + kx,), ('rstd' + kx,))
                P.op('dve', lambda e: e.scalar_tensor_tensor(out=tmp[:], in0=xt, scalar=rstd[:, 0:1], in1=onepsc, op0=ALU.mult, op1=ALU.mult),
                     (rx, 'rstd' + kx, 'mod'), ('tmp' + kx,))
                P.op('dve', lambda e: e.tensor_tensor(out=outb, in0=tmp[:], in1=shift, op=ALU.add), ('tmp' + kx, 'mod'), (ro,))
                return
            P.op('act', lambda e: e.activation(out=junk[:], in_=xt, func=AF.Square, accum_out=ssq[:]), (rx, 'mod'), ('junk', 'ssq'))
            P.op('act', lambda e: e.activation(out=rstd[:], in_=ssq[:], func=AF.Sqrt, scale=1.0 / D, bias=epsc[:, 0:1]), ('ssq', 'epsc'), ('rstd',))
            P.op('dve', lambda e: e.reciprocal(out=rstd[:], in_=rstd[:]), ('rstd',), ('rstd',))
            P.op('dve', lambda e: e.scalar_tensor_tensor(out=tmp[:], in0=xt, scalar=rstd[:, 0:1], in1=onepsc, op0=ALU.mult, op1=ALU.mult),
                 (rx, 'rstd', 'mod'), ('tmp',))
            P.op('dve', lambda e: e.tensor_tensor(out=outb, in0=tmp[:], in1=shift, op=ALU.add), ('tmp', 'mod'), (ro,))

        class Banks:
            def __init__(self, st, tag):
                self.t = [ps(st, "bk%s%d" % (tag, i), [128, 1024]) for i in range(4)]
                self.i = 0

            def one(self):
                i = self.i
                self.i = (i + 1) % 8
                return self.t[i // 2][:, (i % 2) * 512:(i % 2 + 1) * 512], ('bk', i)

            def two(self):
                if self.i % 2:
                    self.i = (self.i + 1) % 8
                i = self.i
                self.i = (i + 2) % 8
                return self.t[i // 2][:, :], (('bk', i), ('bk', i + 1))

        def interleave(gens, G):
            it = iter(gens)
            active = []
            while True:
                while len(active) < G:
                    try:
                        active.append(next(it))
                    except StopIteration:
                        break
                if not active:
                    break
                for g in list(active):
                    try:
                        next(g)
                    except StopIteration:
                        active.remove(g)

        def transpose_mm(bk, src, rsrc):
            pt, kk = bk.two()
            for k in range(8):
                P.op('pe', lambda e, k=k: e.matmul(pt[:, k * 128:(k + 1) * 128], lhsT=src[:, k * 128:(k + 1) * 128], rhs=ident_b[:],
                                                   start=True, stop=True), (rsrc, 'ident_b'), kk)
            return pt, kk

        def ffn_prep_bufs(st, G):
            L = []
            for g in range(G):
                d = {}
                sfx = "_%d" % g
                d['sfx'] = sfx
                d['ssq'] = sb(st, "fq_ssq" + sfx, [128, 1]); d['rstd'] = sb(st, "fq_rstd" + sfx, [128, 1])
                d['tmp'] = sb(st, "fq_tmp" + sfx, [128, D]); d['h2'] = sb(st, "fq_h2" + sfx, [128, D]); d['h2b'] = sb(st, "fq_h2b" + sfx, [128, D], BF16)
                d['h2T'] = sb(st, "fq_h2T" + sfx, [128, 8, 128])
                d['ex'] = sb(st, "fq_ex" + sfx, [128, E]); d['esum'] = sb(st, "fq_esum" + sfx, [128, 1]); d['erec'] = sb(st, "fq_erec" + sfx, [128, 1])
                L.append(d)
            return L

        def ffn_prep_g(bk, d, rw, junk, x1, rx1, T):
            sfx = d['sfx']
            P.op('act', lambda e: e.activation(out=junk[:], in_=x1, func=AF.Square, accum_out=d['ssq'][:]), (rx1,), ('junk', 'ssq' + sfx))
            P.op('act', lambda e: e.activation(out=d['rstd'][:], in_=d['ssq'][:], func=AF.Sqrt, scale=1.0 / D, bias=epsc[:, 0:1]), ('ssq' + sfx, 'epsc'), ('rstd' + sfx,))
            P.op('dve', lambda e: e.reciprocal(out=d['rstd'][:], in_=d['rstd'][:]), ('rstd' + sfx,), ('rstd' + sfx,))
            P.op('dve', lambda e: e.scalar_tensor_tensor(out=d['tmp'][:], in0=x1, scalar=d['rstd'][:, 0:1], in1=modb[:, 4, :], op0=ALU.mult, op1=ALU.mult),
                 (rx1, 'rstd' + sfx, 'mod'), ('tmp' + sfx,))
            P.op('dve', lambda e: e.tensor_tensor(out=d['h2'][:], in0=d['tmp'][:], in1=modb[:, 3, :], op=ALU.add), ('tmp' + sfx, 'mod'), ('h2' + sfx,))
            P.op('act', lambda e: e.activation(out=d['h2b'][:], in_=d['h2'][:], func=AF.Copy), ('h2' + sfx,), ('h2b' + sfx,))
            P.dma('sp', lambda e: e.dma_start(out=h_d[T * 128:(T + 1) * 128, :], in_=d['h2b'][:]), ('h2b' + sfx,), (('h_d', T),))
            pt32, kk = bk.two()
            for k in range(8):
                P.op('pe', lambda e, k=k: e.transpose(pt32[:, k * 128:(k + 1) * 128], d['h2'][:, k * 128:(k + 1) * 128], ident_f[:]),
                     ('h2' + sfx, 'ident_f'), kk)
            yield
            P.op('dve', lambda e: e.tensor_copy(out=d['h2T'][:].rearrange("p k t -> p (k t)"), in_=pt32), kk, ('h2T' + sfx,))
            pr, kr = bk.one()
            for k in range(8):
                P.op('pe', lambda e, k=k: e.matmul(pr[:, 0:E], lhsT=d['h2T'][:, k, :], rhs=rw[:, k, :], start=(k == 0), stop=(k == 7)),
                     ('h2T' + sfx, 'fp_rw'), (kr,))
            yield
            P.op('act', lambda e: e.activation(out=d['ex'][:], in_=pr[:, 0:E], func=AF.Exp, accum_out=d['esum'][:]), (kr,), ('ex' + sfx, 'esum' + sfx))
            P.op('dve', lambda e: e.reciprocal(out=d['erec'][:], in_=d['esum'][:]), ('esum' + sfx,), ('erec' + sfx,))
            P.op('dve', lambda e: e.tensor_scalar(out=aff[:, T, :], in0=d['ex'][:], scalar1=d['erec'][:, 0:1], scalar2=None, op0=ALU.mult),
                 ('ex' + sfx, 'erec' + sfx), (('aff', T),))

        def ffn_prep_alloc(st):
            d = {}
            d['junk'] = sb(st, "fp_junk", [128, D]); d['ssq'] = sb(st, "fp_ssq", [128, 1]); d['rstd'] = sb(st, "fp_rstd", [128, 1])
            d['tmp'] = sb(st, "fp_tmp", [128, D]); d['h2'] = sb(st, "fp_h2", [128, D]); d['h2b'] = sb(st, "fp_h2b", [128, D], BF16)
            d['h2T'] = sb(st, "fp_h2T", [128, 8, 128]); d['rw'] = sb(st, "fp_rw", [128, 8, E])
            d['ex'] = sb(st, "fp_ex", [128, E]); d['esum'] = sb(st, "fp_esum", [128, 1]); d['erec'] = sb(st, "fp_erec", [128, 1])
            d['pt32'] = ps(st, "fp_pt32", [128, 1024]); d['pr'] = ps(st, "fp_pr", [128, E])
            return d

        def ffn_prep(d, layer, x1, rx1, T):
            rms_mod(x1, rx1, modb[:, 3, :], modb[:, 4, :], d['h2'][:], 'fp_h2', d['tmp'], d['junk'], d['ssq'], d['rstd'])
            P.op('act', lambda e: e.activation(out=d['h2b'][:], in_=d['h2'][:], func=AF.Copy), ('fp_h2',), ('fp_h2b',))
            P.dma('sp', lambda e: e.dma_start(out=h_d[T * 128:(T + 1) * 128, :], in_=d['h2b'][:]), ('fp_h2b',), (('h_d', T),))
            for k in range(8):
                P.op('pe', lambda e, k=k: e.transpose(d['pt32'][:, k * 128:(k + 1) * 128], d['h2'][:, k * 128:(k + 1) * 128], ident_f[:]),
                     ('fp_h2', 'ident_f'), ('fp_pt32',))
            P.op('dve', lambda e: e.tensor_copy(out=d['h2T'][:].rearrange("p k t -> p (k t)"), in_=d['pt32'][:]), ('fp_pt32',), ('fp_h2T',))
            for k in range(8):
                P.op('pe', lambda e, k=k: e.matmul(d['pr'][:], lhsT=d['h2T'][:, k, :], rhs=d['rw'][:, k, :], start=(k == 0), stop=(k == 7)),
                     ('fp_h2T', 'fp_rw'), ('fp_pr',))
            P.op('act', lambda e: e.activation(out=d['ex'][:], in_=d['pr'][:], func=AF.Exp, accum_out=d['esum'][:]), ('fp_pr',), ('fp_ex', 'fp_esum'))
            P.op('dve', lambda e: e.reciprocal(out=d['erec'][:], in_=d['esum'][:]), ('fp_esum',), ('fp_erec',))
            P.op('dve', lambda e: e.tensor_scalar(out=aff[:, T, :], in0=d['ex'][:], scalar1=d['erec'][:, 0:1], scalar2=None, op0=ALU.mult),
                 ('fp_ex', 'fp_erec'), (('aff', T),))

        def load_router(d, layer):
            P.dma('sp', lambda e: e.dma_start(out=d['rw'][:], in_=router_w[layer].rearrange("(k p) e -> p k e", p=128)), (), ('fp_rw',))

        def mixer0():
            st = ExitStack()
            with st:
                modc = sb(st, "modc", [128, 2, D])
                with ExitStack() as s2:
                    compute_mod(s2, 0, ccol, modb, [0, 1, 2, 3, 4, 5], "a")
                    P.barrier()
                with ExitStack() as s2:
                    compute_mod(s2, 0, cccol, modc, [0, 1], "b")
                    P.barrier()
                P.op('dve', lambda e: e.tensor_scalar(out=modb[:, 1, :], in0=modb[:, 1, :], scalar1=1.0, scalar2=None, op0=ALU.add), ('mod',), ('mod',))
                P.op('dve', lambda e: e.tensor_scalar(out=modb[:, 4, :], in0=modb[:, 4, :], scalar1=1.0, scalar2=None, op0=ALU.add), ('mod',), ('mod',))
                P.op('dve', lambda e: e.tensor_scalar(out=modc[:, 1, :], in0=modc[:, 1, :], scalar1=1.0, scalar2=None, op0=ALU.add), ('mod',), ('mod',))
                P.barrier()
                with ExitStack() as sa:
                    wi = sb(sa, "wi", [128, 8, 3328], BF16)
                    cosb = sb(sa, "cosb", [128, N]); sinb = sb(sa, "sinb", [128, N])
                    gn = sb(sa, "gn", [128, 4])
                    xt = [sb(sa, "xt%d" % i, [128, D]) for i in range(2)]
                    junk = sb(sa, "junk", [128, D])
                    tmp = [sb(sa, "tmp%d" % i, [128, D]) for i in range(2)]
                    ssq = [sb(sa, "ssq%d" % i, [128, 1]) for i in range(2)]; rstd = [sb(sa, "rstd%d" % i, [128, 1]) for i in range(2)]
                    hb = [sb(sa, "hb%d" % i, [128, D], BF16) for i in range(2)]
                    hT = [sb(sa, "hT%d" % i, [128, 8, 512], BF16) for i in range(2)]
                    sqb = [sb(sa, "sqb%d" % i, [128, 512], BF16) for i in range(2)]
                    r1 = [sb(sa, "r1_%d" % i, [128, 512]) for i in range(2)]
                    qn = [sb(sa, "qn%d" % i, [128, 512]) for i in range(2)]; qsn = [sb(sa, "qsn%d" % i, [128, 512]) for i in range(2)]
                    qf = [sb(sa, "qf%d" % i, [128, 512], BF16) for i in range(2)]
                    vb = [sb(sa, "vb%d" % i, [128, 256], BF16) for i in range(2)]
                    gcs = [sb(sa, "gcs%d" % i, [128, 512]) for i in range(2)]
                    ub = [sb(sa, "ub%d" % i, [128, 512], BF16) for i in range(2)]
                    gbb = [sb(sa, "gbb%d" % i, [128, 512], BF16) for i in range(2)]
                    bk = Banks(sa, "pa")

                    for hh in range(2):
                        P.dma('pool', lambda e, hh=hh: e.dma_start(
                            out=wi[:, :, hh * 1664:(hh + 1) * 1664],
                            in_=w_in[:, hh * 1664:(hh + 1) * 1664].rearrange("(k p) f -> p k f", p=128)), (), ('wi',))
                    P.dma('sp', lambda e: e.dma_start(out=cosb[:], in_=cos_t.ap()), (), ('cosb',))
                    P.dma('sp', lambda e: e.dma_start(out=sinb[:], in_=sin_t.ap()), (), ('sinb',))
                    P.dma('sp', lambda e: e.dma_start(out=gn[:], in_=gains.ap()), (), ('gn',))

                    def tiles_stage(s):
                        isctx = s < 0
                        sp = (s + 1) % 2
                        for t in range(2 if isctx else 4):
                            b = t % 2
                            src = ctx_in[t * 128:(t + 1) * 128, :] if isctx else x_in[(s * 4 + t) * 128:(s * 4 + t + 1) * 128, :]
                            P.dma('sp', lambda e: e.dma_start(out=xt[b][:], in_=src), (), ('xt%d' % b,))
                            shift = modc[:, 0, :] if isctx else modb[:, 0, :]
                            onep = modc[:, 1, :] if isctx else modb[:, 1, :]
                            rms_mod(xt[b][:], 'xt%d' % b, shift, onep, hb[b][:], 'hb%d' % b, tmp[b], junk, ssq[b], rstd[b], kx="_a%d" % b)
                            pt, kk = transpose_mm(bk, hb[b], 'hb%d' % b)
                            P.op('act', lambda e: e.activation(out=hT[sp][:, :, t * 128:(t + 1) * 128],
                                                               in_=pt.rearrange("p (k t) -> p k t", k=8), func=AF.Copy), kk, ('hT%d' % sp,))

                    def make_chains(s):
                        isctx = s < 0
                        sp = (s + 1) % 2
                        ntile = 2 if isctx else 4
                        ntok = ntile * 128
                        hTk = 'hT%d' % sp
                        chains = []

                        def proj(c0, n=ntok):
                            pt, kp = bk.one()
                            for k in range(8):
                                P.op('pe', lambda e, k=k: e.matmul(pt[:, 0:n], lhsT=wi[:, k, c0:c0 + 128], rhs=hT[sp][:, k, 0:n],
                                                                   start=(k == 0), stop=(k == 7)), ('wi', hTk), (kp,))
                            return pt, kp

                        def qk_chain(ci, c0, c1, gc0, kind, hidx):
                            st_ = {}
                            i2 = ci % 2

                            def do_proj():
                                st_['pq'] = proj(c0)
                                if not isctx:
                                    st_['pqs'] = proj(c1)

                            def do_post():
                                pq, kq = st_['pq']
                                P.op('act', lambda e: e.activation(out=sqb[i2][:, 0:ntok], in_=pq[:, 0:ntok], func=AF.Square), (kq,), ('sqb%d' % i2,))
                                pss, kss = bk.one()
                                P.op('pe', lambda e: e.matmul(pss[:, 0:ntok], lhsT=ones_b[:], rhs=sqb[i2][:, 0:ntok], start=True, stop=True),
                                     ('sqb%d' % i2, 'ones_b'), (kss,))
                                P.op('act', lambda e: e.activation(out=r1[i2][:, 0:ntok], in_=pss[:, 0:ntok], func=AF.Ln, scale=1.0 / 128, bias=epsc[:, 0:1]),
                                     (kss, 'epsc'), ('r1_%d' % i2,))
                                P.op('act', lambda e: e.activation(out=r1[i2][:, 0:ntok], in_=r1[i2][:, 0:ntok], func=AF.Exp, scale=-0.5), ('r1_%d' % i2,), ('r1_%d' % i2,))
                                if isctx:
                                    P.op('dve', lambda e: e.scalar_tensor_tensor(
                                        out=qf[i2][:, 0:ntok], in0=pq[:, 0:ntok], scalar=gn[:, gc0:gc0 + 1], in1=r1[i2][:, 0:ntok],
                                        op0=ALU.mult, op1=ALU.mult), (kq, 'gn', 'r1_%d' % i2), ('qf%d' % i2,))
                                    P.dma('sp', lambda e: e.dma_start(out=kT_d[hidx, :, 0:CTX], in_=qf[i2][:, 0:CTX]), ('qf%d' % i2,), ())
                                    return
                                pqs, kqs = st_['pqs']
                                P.op('dve', lambda e: e.scalar_tensor_tensor(
                                    out=qn[i2][:], in0=pq, scalar=gn[:, gc0:gc0 + 1], in1=r1[i2][:], op0=ALU.mult, op1=ALU.mult),
                                    (kq, 'gn', 'r1_%d' % i2), ('qn%d' % i2,))
                                P.op('dve', lambda e: e.scalar_tensor_tensor(
                                    out=qsn[i2][:], in0=pqs, scalar=gn[:, gc0 + 1:gc0 + 2], in1=r1[i2][:], op0=ALU.mult, op1=ALU.mult),
                                    (kqs, 'gn', 'r1_%d' % i2), ('qsn%d' % i2,))
                                P.op('dve', lambda e: e.tensor_tensor(out=qn[i2][:], in0=qn[i2][:], in1=cosb[:, s * 512:(s + 1) * 512], op=ALU.mult),
                                     ('qn%d' % i2, 'cosb'), ('qn%d' % i2,))
                                P.op('dve', lambda e: e.tensor_tensor(out=qsn[i2][:], in0=qsn[i2][:], in1=sinb[:, s * 512:(s + 1) * 512], op=ALU.mult),
                                     ('qsn%d' % i2, 'sinb'), ('qsn%d' % i2,))
                                P.op('dve', lambda e: e.tensor_tensor(out=qf[i2][:], in0=qn[i2][:], in1=qsn[i2][:], op=ALU.add),
                                     ('qn%d' % i2, 'qsn%d' % i2), ('qf%d' % i2,))
                                if kind == 'q':
                                    P.dma('sp', lambda e: e.dma_start(out=qT_d[hidx, :, s * 512:(s + 1) * 512], in_=qf[i2][:]), ('qf%d' % i2,), ())
                                else:
                                    P.dma('sp', lambda e: e.dma_start(out=kT_d[hidx, :, CTX + s * 512:CTX + (s + 1) * 512], in_=qf[i2][:]),
                                          ('qf%d' % i2,), ())
                            return do_proj, do_post

                        def v_chain(t):
                            st_ = {}
                            b = t % 2

                            def do_proj():
                                pv, kv_ = bk.one()
                                for k in range(8):
                                    P.op('pe', lambda e, k=k: e.matmul(pv[:, 0:256], lhsT=hT[sp][:, k, t * 128:(t + 1) * 128], rhs=wi[:, k, 768:1024],
                                                                       start=(k == 0), stop=(k == 7)), ('wi', hTk), (kv_,))
                                st_['pv'] = (pv, kv_)

                            def do_post():
                                pv, kv_ = st_['pv']
                                P.op('act', lambda e: e.activation(out=vb[b][:], in_=pv[:, 0:256], func=AF.Copy), (kv_,), ('vb%d' % b,))
                                row0 = t * 128 if isctx else CTX + (s * 4 + t) * 128
                                P.dma('sp', lambda e: e.dma_start(out=v_d[row0:row0 + 128, :], in_=vb[b][:]), ('vb%d' % b,), ())
                            return do_proj, do_post

                        def conv_chain(c):
                            st_ = {}
                            b = c % 2

                            def do_proj():
                                st_['gb'] = proj(1024 + c * 128); st_['gc'] = proj(1536 + c * 128); st_['hx'] = proj(2048 + c * 128)

                            def do_post():
                                (pgb, kgb), (pgc, kgc), (phx, khx) = st_['gb'], st_['gc'], st_['hx']
                                P.op('act', lambda e: e.activation(out=gcs[b][:], in_=pgc, func=AF.Copy), (kgc,), ('gcs%d' % b,))
                                P.op('dve', lambda e: e.tensor_tensor(out=ub[b][:], in0=phx, in1=gcs[b][:], op=ALU.mult), (khx, 'gcs%d' % b), ('ub%d' % b,))
                                P.op('act', lambda e: e.activation(out=gbb[b][:], in_=pgb, func=AF.Copy), (kgb,), ('gbb%d' % b,))
                                P.dma('sp', lambda e: e.dma_start(out=uT_d[c, :, s * 512:(s + 1) * 512], in_=ub[b][:]), ('ub%d' % b,), ())
                                P.dma('sp', lambda e: e.dma_start(out=gbT_d[c, :, s * 512:(s + 1) * 512], in_=gbb[b][:]), ('gbb%d' % b,), ())
                            return do_proj, do_post

                        ci = 0
                        if not isctx:
                            for h in range(4):
                                chains.append(qk_chain(ci, h * 128, 2560 + h * 128, 0, 'q', h)); ci += 1
                                chains.append(conv_chain(h))
                        for kv in range(2):
                            chains.append(qk_chain(ci, 512 + kv * 128, 3072 + kv * 128, 2, 'k', kv)); ci += 1
                            for t in range(kv * ntile // 2, (kv + 1) * ntile // 2):
                                chains.append(v_chain(t))
                        return chains

                    tiles_stage(-1)
                    for s in range(-1, 8):
                        if s + 1 < 8:
                            tiles_stage(s + 1)
                        pend = None
                        for (pj, po) in make_chains(s):
                            pj()
                            if pend is not None:
                                pend()
                            pend = po
                        pend()
                    P.barrier()
                with ExitStack() as sbk:
                    cw = sb(sbk, "cw", [128, 12])
                    u = sb(sbk, "u", [128, N + 2], BF16); acc = sb(sbk, "acc", [128, N])
                    gbt = sb(sbk, "gbt", [128, N], BF16); so = sb(sbk, "so", [128, N], BF16)
                    P.dma('sp', lambda e: e.dma_start(out=cw[:], in_=convw.ap()), (), ('cw',))
                    for c in range(4):
                        P.op('dve', lambda e: e.memset(u[:, 0:1], 0.0), (), ('u',))
                        P.op('dve', lambda e: e.memset(u[:, N + 1:N + 2], 0.0), (), ('u',))
                        P.dma('sp', lambda e, c=c: e.dma_start(out=u[:, 1:N + 1], in_=uT_d[c]), (), ('u',))
                        P.dma('sp', lambda e, c=c: e.dma_start(out=gbt[:], in_=gbT_d[c]), (), ('gbt',))
                        P.op('dve', lambda e, c=c: e.tensor_scalar(out=acc[:], in0=u[:, 1:N + 1], scalar1=cw[:, c * 3 + 1:c * 3 + 2], scalar2=None,
                                                                   op0=ALU.mult), ('u', 'cw'), ('acc',))
                        P.op('dve', lambda e, c=c: e.scalar_tensor_tensor(out=acc[:], in0=u[:, 0:N], scalar=cw[:, c * 3:c * 3 + 1], in1=acc[:],
                                                                          op0=ALU.mult, op1=ALU.add), ('u', 'cw', 'acc'), ('acc',))
                        P.op('dve', lambda e, c=c: e.scalar_tensor_tensor(out=acc[:], in0=u[:, 2:N + 2], scalar=cw[:, c * 3 + 2:c * 3 + 3], in1=acc[:],
                                                                          op0=ALU.mult, op1=ALU.add), ('u', 'cw', 'acc'), ('acc',))
                        P.op('dve', lambda e: e.tensor_tensor(out=so[:], in0=acc[:], in1=gbt[:], op=ALU.mult), ('acc', 'gbt'), ('so',))
                        P.dma('sp', lambda e, c=c: e.dma_start(out=sT_d[c], in_=so[:]), ('so',), ())
                    P.barrier()
                with ExitStack() as sc:
                    kT = sb(sc, "kT", [128, 2, NK], BF16); vv = sb(sc, "vv", [128, 34, 256], BF16)
                    qs = [sb(sc, "qs%d" % i, [128, 512], BF16) for i in range(2)]
                    pb = [sb(sc, "pb%d" % i, [128, 512], BF16) for i in range(3)]
                    rec = sb(sc, "rec", [128, 512]); ab = [sb(sc, "ab%d" % i, [128, 512], BF16) for i in range(2)]
                    pS = [ps(sc, "pS%d" % i, [128, 512]) for i in range(3)]
                    pO = [ps(sc, "pO%d" % i, [128, 512]) for i in range(2)]
                    pD = [ps(sc, "pD%d" % i, [128, 512]) for i in range(2)]
                    P.dma('sp', lambda e: e.dma_start(out=kT[:], in_=kT_d.ap().rearrange("k p n -> p k n")), (), ('kT',))
                    P.dma('sp', lambda e: e.dma_start(out=vv[:], in_=v_d.ap().rearrange("(c p) f -> p c f", p=128)), (), ('vv',))
                    groups = [(h, s_) for h in range(4) for s_ in range(8)]
                    iters = [(g, c) for g in range(len(groups)) for c in range(34)]

                    def load_q(g):
                        h, s_ = groups[g]
                        b = g % 2
                        P.dma('sp', lambda e: e.dma_start(out=qs[b][:], in_=qT_d[h, :, s_ * 512:(s_ + 1) * 512]), (), ('qs%d' % b,))

                    def qk_exp(i):
                        g, c = iters[i]
                        h, s_ = groups[g]
                        kv = h // 2
                        b = g % 2
                        j = i % 3
                        if c == 0 and g + 1 < len(groups):
                            load_q(g + 1)
                        P.op('pe', lambda e: e.matmul(pS[j][:], lhsT=kT[:, kv, c * 128:(c + 1) * 128], rhs=qs[b][:],
                                                      start=True, stop=True), ('kT', 'qs%d' % b), ('pS%d' % j,))
                        P.op('act', lambda e: e.activation(out=pb[j][:], in_=pS[j][:], func=AF.Exp, scale=float(128 ** -0.5)),
                             ('pS%d' % j,), ('pb%d' % j,))

                    load_q(0)
                    qk_exp(0)
                    qk_exp(1)
                    for i, (g, c) in enumerate(iters):
                        h, s_ = groups[g]
                        kv = h // 2
                        b = g % 2
                        j = i % 3
                        if i + 2 < len(iters):
                            qk_exp(i + 2)
                        P.op('pe', lambda e: e.matmul(pO[b][:], lhsT=vv[:, c, kv * 128:(kv + 1) * 128], rhs=pb[j][:],
                                                      start=(c == 0), stop=(c == 33)), ('vv', 'pb%d' % j), ('pO%d' % b,))
                        P.op('pe', lambda e: e.matmul(pD[b][:], lhsT=ones_b[:], rhs=pb[j][:],
                                                      start=(c == 0), stop=(c == 33)), ('ones_b', 'pb%d' % j), ('pD%d' % b,))
                        if c == 33:
                            P.op('dve', lambda e: e.reciprocal(out=rec[:], in_=pD[b][:]), ('pD%d' % b,), ('rec',))
                            P.op('dve', lambda e: e.tensor_tensor(out=ab[b][:], in0=pO[b][:], in1=rec[:], op=ALU.mult),
                                 ('pO%d' % b, 'rec'), ('ab%d' % b,))
                            P.dma('sp', lambda e: e.dma_start(out=aT_d[h, :, s_ * 512:(s_ + 1) * 512], in_=ab[b][:]), ('ab%d' % b,), ())
                    P.barrier()
                with ExitStack() as sd:
                    G = 2
                    wo = sb(sd, "wo", [128, 8, D], BF16)
                    asl = [sb(sd, "asl%d" % i, [128, 8, 512], BF16) for i in range(2)]
                    xt = [sb(sd, "dxt%d" % i, [128, D]) for i in range(G)]
                    x1 = [sb(sd, "x1_%d" % i, [128, D]) for i in range(G)]
                    rw = sb(sd, "drw", [128, 8, E]); junk = sb(sd, "djunk", [128, D])
                    fpb = ffn_prep_bufs(sd, G)
                    bk = Banks(sd, "pd")
                    P.dma('sp', lambda e: e.dma_start(out=rw[:], in_=router_w[0].rearrange("(k p) e -> p k e", p=128)), (), ('fp_rw',))
                    P.dma('pool', lambda e: e.dma_start(out=wo[:], in_=w_out0.ap().rearrange("(k p) f -> p k f", p=128)), (), ('wo',))

                    def load_slab(s):
                        b = s % 2
                        P.dma('sp', lambda e: e.dma_start(out=asl[b][:, 0:4, :], in_=aT_d[:, :, s * 512:(s + 1) * 512].rearrange("h p t -> p h t")),
                              (), ('asl%d' % b,))
                        P.dma('sp', lambda e: e.dma_start(out=asl[b][:, 4:8, :], in_=sT_d[:, :, s * 512:(s + 1) * 512].rearrange("h p t -> p h t")),
                              (), ('asl%d' % b,))

                    def tile_gen(T):
                        s, t = T // 4, T % 4
                        b = s % 2
                        g = T % G
                        sx = "_%d" % g
                        if t == 0 and s + 1 < 8:
                            load_slab(s + 1)
                        P.dma('sp', lambda e: e.dma_start(out=xt[g][:], in_=x_in[T * 128:(T + 1) * 128, :]), (), ('dxt' + sx,))
                        pys = []
                        for hh in range(2):
                            py_, ky = bk.one()
                            pys.append((py_, ky))
                            for k in range(8):
                                P.op('pe', lambda e, k=k, hh=hh, py_=py_: e.matmul(
                                    py_, lhsT=asl[b][:, k, t * 128:(t + 1) * 128], rhs=wo[:, k, hh * 512:(hh + 1) * 512],
                                    start=(k == 0), stop=(k == 7)), ('asl%d' % b, 'wo'), (ky,))
                        yield
                        for hh in range(2):
                            py_, ky = pys[hh]
                            P.op('dve', lambda e, hh=hh, py_=py_: e.tensor_tensor(out=x1[g][:, hh * 512:(hh + 1) * 512], in0=py_,
                                                                                 in1=modb[:, 2, hh * 512:(hh + 1) * 512], op=ALU.mult),
                                 (ky, 'mod'), ('x1' + sx,))
                        P.op('dve', lambda e: e.tensor_tensor(out=x1[g][:], in0=x1[g][:], in1=xt[g][:], op=ALU.add), ('x1' + sx, 'dxt' + sx), ('x1' + sx,))
                        P.dma('sp', lambda e: e.dma_start(out=out_d[T * 128:(T + 1) * 128, :], in_=x1[g][:]), ('x1' + sx,), (('xd', T),))
                        yield from ffn_prep_g(bk, fpb[g], rw, junk, x1[g][:], 'x1' + sx, T)

                    load_slab(0)
                    interleave([tile_gen(T) for T in range(NT)], G)
                    P.barrier()

        def mixer1():
            G = 2
            with ExitStack() as st:
                with ExitStack() as s2:
                    compute_mod(s2, 1, ccol, modb, [0, 1, 2, 3, 4, 5], "c")
                    P.barrier()
                P.op('dve', lambda e: e.tensor_scalar(out=modb[:, 1, :], in0=modb[:, 1, :], scalar1=1.0, scalar2=None, op0=ALU.add), ('mod',), ('mod',))
                P.op('dve', lambda e: e.tensor_scalar(out=modb[:, 4, :], in0=modb[:, 4, :], scalar1=1.0, scalar2=None, op0=ALU.add), ('mod',), ('mod',))
                P.barrier()
                wi = sb(st, "gwi", [128, 8, 2 * D], BF16); wo = sb(st, "gwo", [128, 8, D], BF16)
                swT = sb(st, "swT", [128, 8, 128], BF16); sbq = sb(st, "sbq", [128, 8]); snb = sb(st, "snb", [128, D])
                rw = sb(st, "grw", [128, 8, E]); junk = sb(st, "gjunk", [128, D])
                xt = [sb(st, "gxt%d" % i, [128, D]) for i in range(G)]
                tmp = [sb(st, "gtmp%d" % i, [128, D]) for i in range(G)]
                ssq = [sb(st, "gssq%d" % i, [128, 1]) for i in range(G)]; rstd = [sb(st, "grstd%d" % i, [128, 1]) for i in range(G)]
                hb = [sb(st, "ghb%d" % i, [128, D], BF16) for i in range(G)]; hT = [sb(st, "ghT%d" % i, [128, 8, 128], BF16) for i in range(G)]
                z = [sb(st, "gz%d" % i, [128, 2 * D]) for i in range(G)]; vnb = [sb(st, "gvnb%d" % i, [128, D], BF16) for i in range(G)]
                mb = [sb(st, "gmb%d" % i, [128, D], BF16) for i in range(G)]; mT = [sb(st, "gmT%d" % i, [128, 8, 128], BF16) for i in range(G)]
                x3 = [sb(st, "x3_%d" % i, [128, D]) for i in range(G)]
                fpb = ffn_prep_bufs(st, G)
                bk = Banks(st, "m1")
                P.dma('sp', lambda e: e.dma_start(out=rw[:], in_=router_w[1].rearrange("(k p) e -> p k e", p=128)), (), ('fp_rw',))
                for hh in range(2):
                    P.dma('pool', lambda e, hh=hh: e.dma_start(out=wi[:, :, hh * D:(hh + 1) * D],
                                                               in_=sg_w_in[:, hh * D:(hh + 1) * D].rearrange("(k p) f -> p k f", p=128)), (), ('gwi',))
                P.dma('pool', lambda e: e.dma_start(out=wo[:], in_=sg_w_out.ap().rearrange("(k p) f -> p k f", p=128)), (), ('gwo',))
                P.dma('pool', lambda e: e.dma_start(out=swT[:].rearrange("p g q -> p (g q)"), in_=sgwT.ap()), (), ('swT',))
                P.dma('sp', lambda e: e.dma_start(out=sbq[:], in_=sgb.ap()), (), ('sbq',))
                P.dma('sp', lambda e: e.dma_start(out=snb[:], in_=sg_norm.ap().to_broadcast([128, D])), (), ('snb',))

                def tile_gen(T):
                    g = T % G
                    sx = "_%d" % g
                    P.dma('sp', lambda e: e.dma_start(out=xt[g][:], in_=out_d[T * 128:(T + 1) * 128, :]), (('xd', T),), ('gxt' + sx,))
                    P.op('act', lambda e: e.activation(out=junk[:], in_=xt[g][:], func=AF.Square, accum_out=ssq[g][:]), ('gxt' + sx,), ('junk', 'gssq' + sx))
                    P.op('act', lambda e: e.activation(out=rstd[g][:], in_=ssq[g][:], func=AF.Sqrt, scale=1.0 / D, bias=epsc[:, 0:1]), ('gssq' + sx, 'epsc'), ('grstd' + sx,))
                    P.op('dve', lambda e: e.reciprocal(out=rstd[g][:], in_=rstd[g][:]), ('grstd' + sx,), ('grstd' + sx,))
                    P.op('dve', lambda e: e.scalar_tensor_tensor(out=tmp[g][:], in0=xt[g][:], scalar=rstd[g][:, 0:1], in1=modb[:, 1, :], op0=ALU.mult, op1=ALU.mult),
                         ('gxt' + sx, 'grstd' + sx, 'mod'), ('gtmp' + sx,))
                    P.op('dve', lambda e: e.tensor_tensor(out=hb[g][:], in0=tmp[g][:], in1=modb[:, 0, :], op=ALU.add), ('gtmp' + sx, 'mod'), ('ghb' + sx,))
                    pt, kk = transpose_mm(bk, hb[g], 'ghb' + sx)
                    yield
                    P.op('act', lambda e: e.activation(out=hT[g][:].rearrange("p k t -> p (k t)"), in_=pt, func=AF.Copy), kk, ('ghT' + sx,))
                    for n in range(4):
                        pz, kz = bk.one()
                        for k in range(8):
                            P.op('pe', lambda e, k=k, n=n, pz=pz: e.matmul(pz, lhsT=hT[g][:, k, :], rhs=wi[:, k, n * 512:(n + 1) * 512],
                                                                          start=(k == 0), stop=(k == 7)), ('ghT' + sx, 'gwi'), (kz,))
                        P.op('act', lambda e, n=n, pz=pz: e.activation(out=z[g][:, n * 512:(n + 1) * 512], in_=pz, func=AF.Gelu_apprx_tanh),
                             (kz,), (('gz' + sx, n),))
                        if n == 1:
                            yield
                    yield
                    zv = (('gz' + sx, 2), ('gz' + sx, 3))
                    P.op('act', lambda e: e.activation(out=junk[:], in_=z[g][:, D:2 * D], func=AF.Square, accum_out=ssq[g][:]), zv, ('junk', 'gssq' + sx))
                    P.op('act', lambda e: e.activation(out=rstd[g][:], in_=ssq[g][:], func=AF.Sqrt, scale=1.0 / D, bias=epsc[:, 0:1]), ('gssq' + sx, 'epsc'), ('grstd' + sx,))
                    P.op('dve', lambda e: e.reciprocal(out=rstd[g][:], in_=rstd[g][:]), ('grstd' + sx,), ('grstd' + sx,))
                    P.op('dve', lambda e: e.scalar_tensor_tensor(out=vnb[g][:], in0=z[g][:, D:2 * D], scalar=rstd[g][:, 0:1], in1=snb[:], op0=ALU.mult, op1=ALU.mult),
                         zv + ('grstd' + sx, 'snb'), ('gvnb' + sx,))
                    pm, km = bk.two()
                    for gg in range(8):
                        P.op('pe', lambda e, gg=gg: e.matmul(pm[:, gg * 128:(gg + 1) * 128], lhsT=swT[:, gg, :], rhs=vnb[g][:, gg * 128:(gg + 1) * 128],
                                                             start=True, stop=True), ('swT', 'gvnb' + sx), km)
                    yield
                    for gg in range(8):
                        P.op('dve', lambda e, gg=gg: e.scalar_tensor_tensor(out=mb[g][:, gg * 128:(gg + 1) * 128], in0=pm[:, gg * 128:(gg + 1) * 128],
                                                                            scalar=sbq[:, gg:gg + 1], in1=z[g][:, gg * 128:(gg + 1) * 128],
                                                                            op0=ALU.add, op1=ALU.mult),
                             km + ('sbq', ('gz' + sx, 0), ('gz' + sx, 1)), ('gmb' + sx,))
                    pt2, kk2 = transpose_mm(bk, mb[g], 'gmb' + sx)
                    yield
                    P.op('act', lambda e: e.activation(out=mT[g][:].rearrange("p k t -> p (k t)"), in_=pt2, func=AF.Copy), kk2, ('gmT' + sx,))
                    pys = []
                    for hh in range(2):
                        py_, ky = bk.one()
                        pys.append((py_, ky))
                        for k in range(8):
                            P.op('pe', lambda e, k=k, hh=hh, py_=py_: e.matmul(py_, lhsT=mT[g][:, k, :], rhs=wo[:, k, hh * 512:(hh + 1) * 512],
                                                                              start=(k == 0), stop=(k == 7)), ('gmT' + sx, 'gwo'), (ky,))
                    yield
                    for hh in range(2):
                        py_, ky = pys[hh]
                        P.op('dve', lambda e, hh=hh, py_=py_: e.tensor_tensor(out=x3[g][:, hh * 512:(hh + 1) * 512], in0=py_,
                                                                             in1=modb[:, 2, hh * 512:(hh + 1) * 512], op=ALU.mult),
                             (ky, 'mod'), ('x3' + sx,))
                    P.op('dve', lambda e: e.tensor_tensor(out=x3[g][:], in0=x3[g][:], in1=xt[g][:], op=ALU.add), ('x3' + sx, 'gxt' + sx), ('x3' + sx,))
                    P.dma('sp', lambda e: e.dma_start(out=out_d[T * 128:(T + 1) * 128, :], in_=x3[g][:]), ('x3' + sx,), (('xd', T),))
                    yield from ffn_prep_g(bk, fpb[g], rw, junk, x3[g][:], 'x3' + sx, T)

                interleave([tile_gen(T) for T in range(NT)], G)
                P.barrier()

        def ffn(layer):
            with ExitStack() as se:
                lo = sb(se, "lo", [128, E]); hi = sb(se, "hi", [128, E]); mid = sb(se, "mid", [128, E])
                cmpb = sb(se, "cmpb", [128, NT, E], BF16); cnt = sb(se, "cnt", [128, E]); ge = sb(se, "ge", [128, E])
                t1 = sb(se, "t1", [128, E]); t2 = sb(se, "t2", [128, E])
                mf = sb(se, "mf", [128, NT, E]); offs = sb(se, "offs", [128, NT, E]); tot = sb(se, "tot", [128, NT, E])
                gpos = sb(se, "gpos", [128, NT, E])
                ohb = [sb(se, "ohb%d" % i, [128, 512], BF16) for i in range(4)]
                idf = sb(se, "idf", [128, E * 4])
                pc = ps(se, "pc", [128, 512]); pA = ps(se, "pA", [128, 512]); pB = ps(se, "pB", [128, 512])
                pid = ps(se, "pid", [128, E * 4 * 4])
                idg = sb(se, "idg", [128, NT, E, 4], BF16); hif = sb(se, "hif", [128, NT, E])
                affr = tuple(('aff', T) for T in range(NT))
                P.op('dve', lambda e: e.memset(lo[:], 0.0), (), ('lo',))
                P.op('dve', lambda e: e.memset(mid[:], 0.5), (), ('mid',))
                for it in range(NBIS):
                    w = 2.0 ** -(it + 1)
                    P.op('dve', lambda e: e.tensor_tensor(out=cmpb[:], in0=aff[:], in1=mid[:].unsqueeze(1).to_broadcast([128, NT, E]), op=ALU.is_ge),
                         affr + ('mid',), ('cmpb',))
                    P.op('pe', lambda e: e.matmul(pc[:], lhsT=ones_b[:], rhs=cmpb[:].rearrange("p t e -> p (t e)"), start=True, stop=True),
                         ('cmpb', 'ones_b'), ('pc',))
                    P.op('dve', lambda e: e.tensor_reduce(out=cnt[:], in_=pc[:].rearrange("p (t e) -> p e t", e=E), axis=AX.X, op=ALU.add),
                         ('pc',), ('cnt',))
                    P.op('dve', lambda e, w=w: e.tensor_scalar(out=ge[:], in0=cnt[:], scalar1=float(CAP), scalar2=w, op0=ALU.is_ge, op1=ALU.mult),
                         ('cnt',), ('ge',))
                    P.op('dve', lambda e: e.tensor_tensor(out=lo[:], in0=lo[:], in1=ge[:], op=ALU.add), ('lo', 'ge'), ('lo',))
                    P.op('dve', lambda e, w=w: e.tensor_scalar(out=mid[:], in0=lo[:], scalar1=0.5 * w, scalar2=None, op0=ALU.add), ('lo',), ('mid',))
                P.op('dve', lambda e: e.tensor_tensor(out=mf[:], in0=aff[:], in1=lo[:].unsqueeze(1).to_broadcast([128, NT, E]), op=ALU.is_ge),
                     affr + ('lo',), ('mf',))
                P.op('dve', lambda e: e.tensor_copy(out=cmpb[:], in_=mf[:]), ('mf',), ('cmpb',))
                P.op('pe', lambda e: e.matmul(pA[:], lhsT=tri_b[:], rhs=cmpb[:].rearrange("p t e -> p (t e)"), start=True, stop=True),
                     ('cmpb', 'tri_b'), ('pA',))
                P.op('pe', lambda e: e.matmul(pB[:], lhsT=ones_b[:], rhs=cmpb[:].rearrange("p t e -> p (t e)"), start=True, stop=True),
                     ('cmpb', 'ones_b'), ('pB',))
                P.op('dve', lambda e: e.tensor_copy(out=tot[:].rearrange("p t e -> p (t e)"), in_=pB[:]), ('pB',), ('tot',))
                P.op('dve', lambda e: e.memset(offs[:, 0, :], 0.0), (), ('offs',))
                for T in range(1, NT):
                    P.op('dve', lambda e, T=T: e.tensor_tensor(out=offs[:, T, :], in0=offs[:, T - 1, :], in1=tot[:, T - 1, :], op=ALU.add),
                         ('offs', 'tot'), ('offs',))
                P.op('dve', lambda e: e.tensor_tensor(out=gpos[:].rearrange("p t e -> p (t e)"), in0=pA[:], in1=offs[:].rearrange("p t e -> p (t e)"), op=ALU.add),
                     ('pA', 'offs'), ('gpos',))
                P.op('dve', lambda e: e.tensor_tensor(out=gpos[:], in0=gpos[:], in1=mf[:], op=ALU.mult), ('gpos', 'mf'), ('gpos',))
                P.op('dve', lambda e: e.tensor_scalar(out=gpos[:], in0=gpos[:], scalar1=-1.0, scalar2=None, op0=ALU.add), ('gpos',), ('gpos',))
                P.op('dve', lambda e: e.tensor_copy(out=idg[:, :, :, 0:2], in_=idcols[:].unsqueeze(2).to_broadcast([128, NT, E, 2])), ('idcols',), ('idg',))
                P.op('dve', lambda e: e.tensor_copy(out=idg[:, :, :, 2], in_=aff[:]), affr, ('idg',))
                P.op('dve', lambda e: e.tensor_copy(out=hif[:], in_=idg[:, :, :, 2]), ('idg',), ('hif',))
                P.op('dve', lambda e: e.tensor_tensor(out=idg[:, :, :, 3], in0=aff[:], in1=hif[:], op=ALU.subtract), affr + ('hif',), ('idg',))
                first = True
                n = 0
                for T in range(NT):
                    for ex in range(E):
                        b = n % 4
                        eng = 'dve'
                        P.op(eng, lambda e, b=b, T=T, ex=ex: e.tensor_scalar(out=ohb[b][:], in0=iota_h[:], scalar1=gpos[:, T, ex:ex + 1], scalar2=None,
                                                                             op0=ALU.is_equal), ('iota_h', 'gpos'), ('ohb%d' % b,))
                        for jt in range(4):
                            col = (ex * 4 + jt) * 4
                            P.op('pe', lambda e, b=b, jt=jt, col=col, T=T, ex=ex, first=first: e.matmul(
                                pid[:, col:col + 4], lhsT=ohb[b][:, jt * 128:(jt + 1) * 128], rhs=idg[:, T, ex, :],
                                start=first, stop=(T == NT - 1), skip_group_check=True), ('ohb%d' % b, 'idg'), ('pid',))
                            first = False
                        n += 1
                P.op('dve', lambda e: e.tensor_reduce(out=idf[:], in_=pid[:].rearrange("p (n c) -> p n c", c=4)[:, :, 0:2], axis=AX.X, op=ALU.add), ('pid',), ('idf',))
                P.op('dve', lambda e: e.tensor_reduce(out=gall[:], in_=pid[:].rearrange("p (n c) -> p n c", c=4)[:, :, 2:4], axis=AX.X, op=ALU.add), ('pid',), ('gall',))
                P.op('dve', lambda e: e.tensor_copy(out=idx_i[:], in_=idf[:]), ('idf',), ('idx_i',))
                P.barrier()
            with ExitStack() as sf:
                US = [sb(sf, "US%d" % i, [128, 2, 8, 512], BF16) for i in range(3)]
                VS = [sb(sf, "VS%d" % i, [128, 4, D], BF16) for i in range(NG)]
                xs = [sb(sf, "xs%d" % i, [128, 4, D], BF16) for i in range(2)]
                xsT = [sb(sf, "xsT%d" % i, [128, 8, 512], BF16) for i in range(2)]
                hid = sb(sf, "hid", [128, NF, 512], BF16)
                sg = [sb(sf, "sg%d" % i, [128, 512]) for i in range(2)]
                ysb = [sb(sf, "ysb%d" % i, [128, D]) for i in range(4)]
                pst = ps(sf, "fpst", [128, 1024], BF16)
                ph1 = [ps(sf, "ph1_%d" % i, [128, 512]) for i in range(2)]
                ph3 = [ps(sf, "ph3_%d" % i, [128, 512]) for i in range(2)]
                py = [ps(sf, "fpy%d" % i, [128, 512]) for i in range(2)]
                allxd = tuple(('xd', T) for T in range(NT))

                def load_U(ex, q):
                    u = ex * NG + q
                    slot = u % 3
                    f0, nf = FG[q]
                    for wi_, W in enumerate((exp_w1, exp_w3)):
                        P.dma('pool', lambda e, W=W, wi_=wi_, slot=slot, f0=f0, nf=nf, ex=ex: e.dma_start(
                            out=US[slot][:, wi_, :, 0:nf * 128],
                            in_=W[layer, ex, :, f0 * 128:(f0 + nf) * 128].rearrange("(k p) f -> p k f", p=128)), (), ('US%d' % slot,))

                def load_V(ex, q):
                    f0, nf = FG[q]
                    P.dma('pool', lambda e, q=q, f0=f0, nf=nf, ex=ex: e.dma_start(
                        out=VS[q][:, 0:nf, :], in_=exp_w2[layer, ex, f0 * 128:(f0 + nf) * 128, :].rearrange("(f p) d -> p f d", p=128)),
                        (), ('VS%d' % q,))

                def gathers(ex):
                    b = ex % 2
                    for jt in range(4):
                        col = ex * 4 + jt
                        P.dma('pool', lambda e, b=b, jt=jt, col=col: e.indirect_dma_start(
                            out=xs[b][:, jt, :], out_offset=None, in_=h_d[:, :],
                            in_offset=bass.IndirectOffsetOnAxis(ap=idx_i[:, col:col + 1], axis=0)),
                            ('idx_i',), (('xs', b, jt),))

                def transposes(ex, jt):
                    b = ex % 2
                    for k in range(8):
                        P.op('pe', lambda e, k=k: e.transpose(pst[:, k * 128:(k + 1) * 128], xs[b][:, jt, k * 128:(k + 1) * 128], ident_b[:]),
                             (('xs', b, jt), 'ident_b'), ('fpst',))
                    P.op('act', lambda e: e.activation(out=xsT[b][:, :, jt * 128:(jt + 1) * 128],
                                                       in_=pst[:].rearrange("p (k t) -> p k t", k=8), func=AF.Copy), ('fpst',), ('xsT%d' % b,))

                gathers(0)
                for q in range(3):
                    load_U(0, q)
                for q in range(NG):
                    load_V(0, q)
                for jt in range(4):
                    transposes(0, jt)
                for ex in range(E):
                    b = ex % 2
                    for q in range(NG):
                        u = ex * NG + q
                        slot = u % 3
                        f0, nf = FG[q]
                        for fl in range(nf):
                            f = f0 + fl
                            pb_ = f % 2
                            for k in range(8):
                                P.op('pe', lambda e, slot=slot, fl=fl, k=k, pb_=pb_: e.matmul(
                                    ph1[pb_][:], lhsT=US[slot][:, 0, k, fl * 128:(fl + 1) * 128], rhs=xsT[b][:, k, :], start=(k == 0), stop=(k == 7)),
                                    ('US%d' % slot, 'xsT%d' % b), ('ph1_%d' % pb_,))
                            for k in range(8):
                                P.op('pe', lambda e, slot=slot, fl=fl, k=k, pb_=pb_: e.matmul(
                                    ph3[pb_][:], lhsT=US[slot][:, 1, k, fl * 128:(fl + 1) * 128], rhs=xsT[b][:, k, :], start=(k == 0), stop=(k == 7)),
                                    ('US%d' % slot, 'xsT%d' % b), ('ph3_%d' % pb_,))
                            P.op('act', lambda e, pb_=pb_: e.activation(out=sg[pb_][:], in_=ph1[pb_][:], func=AF.Silu), ('ph1_%d' % pb_,), ('sg%d' % pb_,))
                            P.op('dve', lambda e, pb_=pb_, f=f: e.tensor_tensor(out=hid[:, f, :], in0=ph3[pb_][:], in1=sg[pb_][:], op=ALU.mult),
                                 ('ph3_%d' % pb_, 'sg%d' % pb_), (('hid', f),))
                        un = u + 3
                        if un < E * NG:
                            load_U(un // NG, un % NG)
                        if q == 0 and ex + 1 < E:
                            gathers(ex + 1)
                        if q == 2 and ex > 0:
                            for qq in range(NG):
                                load_V(ex, qq)
                    gi = 0
                    for jt in range(4):
                        for hh in range(2):
                            for f in range(NF):
                                q = FQ[f]
                                fl = f - FG[q][0]
                                P.op('pe', lambda e, jt=jt, hh=hh, f=f, q=q, fl=fl: e.matmul(
                                    py[hh][:], lhsT=hid[:, f, jt * 128:(jt + 1) * 128], rhs=VS[q][:, fl, hh * 512:(hh + 1) * 512],
                                    start=(f == 0), stop=(f == NF - 1)), (('hid', f), 'VS%d' % q), ('fpy%d' % hh,))
                            P.op('dve', lambda e, jt=jt, hh=hh, ex=ex: e.scalar_tensor_tensor(
                                out=ysb[jt][:, hh * 512:(hh + 1) * 512], in0=py[hh][:], scalar=gall[:, ex * 4 + jt:ex * 4 + jt + 1],
                                in1=modb[:, 5, hh * 512:(hh + 1) * 512], op0=ALU.mult, op1=ALU.mult),
                                ('fpy%d' % hh, 'gall', 'mod'), ('ysb%d' % jt,))
                            if ex + 1 < E and gi >= 4:
                                transposes(ex + 1, gi - 4)
                            gi += 1
                        col = ex * 4 + jt
                        P.dma('pool', lambda e, jt=jt, col=col: e.indirect_dma_start(
                            out=out_d[:, :], out_offset=bass.IndirectOffsetOnAxis(ap=idx_i[:, col:col + 1], axis=0),
                            in_=ysb[jt][:, :], in_offset=None, compute_op=ALU.add),
                            ('ysb%d' % jt, 'idx_i'), allxd)
                P.barrier()

        if 'm0' in phases:
            mixer0()
        if 'f0' in phases:
            ffn(0)
        if 'm1' in phases:
            mixer1()
        if 'f1' in phases:
            ffn(1)
        P.barrier()
    return nc


def _consts():
    n = N
    rows = n // 64
    row_idx = np.repeat(np.arange(rows, dtype=np.float32), 64)
    col_idx = np.tile(np.arange(64, dtype=np.float32), rows)
    inv_freq = (np.float32(10000.0) ** (-np.arange(32, dtype=np.float32) / np.float32(32))).astype(np.float32)
    ang_r = (row_idx[None, :] * inv_freq[:, None]).astype(np.float32)
    ang_c = (col_idx[None, :] * inv_freq[:, None]).astype(np.float32)
    cos_t = np.concatenate([np.cos(ang_r), np.cos(ang_r), np.cos(ang_c), np.cos(ang_c)], 0).astype(np.float32)
    sin_t = np.concatenate([-np.sin(ang_r), np.sin(ang_r), -np.sin(ang_c), np.sin(ang_c)], 0).astype(np.float32)
    ident = np.eye(128, dtype=np.float32)
    tri = np.triu(np.ones((128, 128), dtype=np.float32))
    iota = np.tile(np.arange(512, dtype=np.float32)[None, :], (128, 1))
    idcols = np.zeros((128, 32, 2), dtype=np.float32)
    idcols[:, :, 0] = np.arange(128, dtype=np.float32)[:, None]
    idcols[:, :, 1] = (128.0 * np.arange(32, dtype=np.float32))[None, :]
    return dict(cos_t=np.ascontiguousarray(cos_t), sin_t=np.ascontiguousarray(sin_t), ident=ident, tri=tri, iota=iota,
                idcols=np.ascontiguousarray(idcols.reshape(128, 64)))


def _swap_perm():
    p = np.arange(128)
    blk = p // 32
    return (blk ^ 1) * 32 + (p % 32)


def prepare_shared(inputs):
    f = lambda a: np.ascontiguousarray(np.asarray(a, dtype=np.float32))
    perm = _swap_perm()
    w = np.asarray(inputs['ab_w_in'][0], dtype=np.float32)
    qsw = np.concatenate([w[:, h * 128:(h + 1) * 128][:, perm] for h in range(4)], 1)
    ksw = np.concatenate([w[:, 512 + h * 128:512 + (h + 1) * 128][:, perm] for h in range(2)], 1)
    w_in_ext = np.concatenate([w, qsw, ksw], 1)
    qn = np.asarray(inputs['ab_q_norm'][0], dtype=np.float32); kn = np.asarray(inputs['ab_k_norm'][0], dtype=np.float32)
    gains = np.stack([qn, qn[perm], kn, kn[perm]], 1)
    cw = np.asarray(inputs['ab_conv_w'][0], dtype=np.float32)
    convw = cw.reshape(3, 4, 128).transpose(2, 1, 0).reshape(128, 12)
    sgw = np.asarray(inputs['sg_w'][0], dtype=np.float32)
    sgwT = sgw.transpose(2, 0, 1).reshape(128, 8 * 128)
    sgb = np.asarray(inputs['sg_b'][0], dtype=np.float32).T
    cctx = np.asarray(inputs['c_ctx'], dtype=np.float32)
    sh = dict(
        cccol=f(cctx.reshape(8, 128).T), ada_w=f(inputs['ada_w']), ada_b=f(inputs['ada_b']),
        w_in=f(w_in_ext), gains=f(gains), convw=f(convw), w_out0=f(inputs['ab_w_out'][0]),
        sg_w_in=f(inputs['sg_w_in'][0]), sg_norm=f(np.asarray(inputs['sg_norm'][0]).reshape(1, D)),
        sgwT=f(sgwT), sgb=f(sgb), sg_w_out=f(inputs['sg_w_out'][0]), router_w=f(inputs['router_w']),
        exp_w1=f(inputs['exp_w1']), exp_w3=f(inputs['exp_w3']), exp_w2=f(inputs['exp_w2']),
    )
    sh.update(_consts())
    return sh


def core_inputs(inputs, shared, b):
    m = dict(shared)
    m['x'] = np.ascontiguousarray(np.asarray(inputs['x'][b], dtype=np.float32))
    m['ctx'] = np.ascontiguousarray(np.asarray(inputs['ctx'][b], dtype=np.float32))
    m['ccol'] = np.ascontiguousarray(np.asarray(inputs['c'][b], dtype=np.float32).reshape(8, 128).T)
    return m


def kernel(**inputs):
    nb = inputs['x'].shape[0]
    shared = prepare_shared(inputs)
    nc = build()
    in_maps = [core_inputs(inputs, shared, b) for b in range(nb)]
    res = run_bass_kernel_spmd(nc, in_maps, core_ids=list(range(nb)))
    return np.stack([np.asarray(r['out'], dtype=np.float32) for r in res.results], 0)
```

```python
import numpy as np
from contextlib import ExitStack
import concourse.bass as bass
import concourse.mybir as mybir
from concourse.bass_utils import run_bass_kernel_spmd

F32 = mybir.dt.float32
BF16 = mybir.dt.bfloat16
I32 = mybir.dt.int32
AF = mybir.ActivationFunctionType
ALU = mybir.AluOpType
AX = mybir.AxisListType

N = 4096
D = 1024
NT = 32
CTX = 256
NK = N + CTX
E = 16
CAP = 512
DFF = 2816
NF = 22
EPS = 1e-6
FG = [(0, 4), (4, 4), (8, 4), (12, 4), (16, 3), (19, 3)]
NG = len(FG)
FQ = [q for q, (f0, nf) in enumerate(FG) for _ in range(nf)]
NBIS = 30


class Prog:
    def __init__(self, nc, es, nds=40):
        self.nc = nc
        self.eng = {'pe': nc.tensor, 'act': nc.scalar, 'dve': nc.vector, 'pool': nc.gpsimd, 'sp': nc.sync}
        self.sem = {k: es.enter_context(nc.semaphore('S' + k)) for k in self.eng}
        self.cnt = {k: 0 for k in self.eng}
        self.waited = {k: {} for k in self.eng}
        self.dsem = [es.enter_context(nc.semaphore('Q%d' % i)) for i in range(nds)]
        self.dcnt = [0] * nds
        self.dn = 0
        self.lastw = {}
        self.readers = {}

    def _wait(self, e, tok):
        key, sem, val = tok
        if self.waited[e].get(key, 0) >= val:
            return
        self.eng[e].wait_ge(sem, val)
        self.waited[e][key] = val

    def _deps(self, e, reads, writes):
        for r in reads:
            t = self.lastw.get(r)
            if t is not None and not (t[0] == e and e == 'pe'):
                self._wait(e, t)
        for w in writes:
            t = self.lastw.get(w)
            if t is not None and t[0] != e:
                self._wait(e, t)
            for t in self.readers.get(w, {}).values():
                if t[0] != e:
                    self._wait(e, t)

    def _record(self, tok, reads, writes):
        for w in writes:
            self.lastw[w] = tok
            self.readers[w] = {}
        for r in reads:
            self.readers.setdefault(r, {})[tok[0]] = tok

    def op(self, e, fn, reads=(), writes=()):
        self._deps(e, reads, writes)
        self.cnt[e] += 1
        tok = (e, self.sem[e], self.cnt[e])
        fn(self.eng[e]).then_inc(self.sem[e], 1)
        self._record(tok, reads, writes)

    def dma(self, e, fn, reads=(), writes=()):
        self._deps(e, reads, writes)
        i = self.dn
        self.dn = (self.dn + 1) % len(self.dsem)
        if self.dcnt[i] > 0:
            self._wait(e, ('Q%d' % i, self.dsem[i], self.dcnt[i]))
        self.dcnt[i] += 16
        tok = ('Q%d' % i, self.dsem[i], self.dcnt[i])
        fn(self.eng[e]).then_inc(self.dsem[i], 16)
        self._record(tok, reads, writes)

    def barrier(self):
        for e in self.eng:
            for f in self.eng:
                if f != e and self.cnt[f] > 0:
                    self._wait(e, (f, self.sem[f], self.cnt[f]))
            for i, c in enumerate(self.dcnt):
                if c > 0:
                    self._wait(e, ('Q%d' % i, self.dsem[i], c))
        self.lastw.clear()
        self.readers.clear()


def build(phases=('m0', 'f0', 'm1', 'f1'), dbg=False):
    nc = bass.Bass("TRN2", target_bir_lowering=False)

    def din(name, shape, dt=F32):
        return nc.dram_tensor(name, list(shape), dt, kind="ExternalInput")

    x_in = din("x", [N, D]); ctx_in = din("ctx", [CTX, D])
    ccol = din("ccol", [128, 8]); cccol = din("cccol", [128, 8])
    ada_w = din("ada_w", [2, D, 6 * D]); ada_b = din("ada_b", [2, 6 * D])
    w_in = din("w_in", [D, 3328]); gains = din("gains", [128, 4]); convw = din("convw", [128, 12])
    w_out0 = din("w_out0", [D, D])
    sg_w_in = din("sg_w_in", [D, 2 * D]); sg_norm = din("sg_norm", [1, D])
    sgwT = din("sgwT", [128, 8 * 128]); sgb = din("sgb", [128, 8]); sg_w_out = din("sg_w_out", [D, D])
    router_w = din("router_w", [2, D, E])
    exp_w1 = din("exp_w1", [2, E, D, DFF]); exp_w3 = din("exp_w3", [2, E, D, DFF]); exp_w2 = din("exp_w2", [2, E, DFF, D])
    cos_t = din("cos_t", [128, N]); sin_t = din("sin_t", [128, N])
    ident_in = din("ident", [128, 128]); tri_in = din("tri", [128, 128]); iota_in = din("iota", [128, 512])
    idcols_in = din("idcols", [128, 64])
    out_d = nc.dram_tensor("out", [N, D], F32, kind="ExternalOutput")

    def dscr(name, shape, dt):
        return nc.dram_tensor(name, list(shape), dt, kind="Internal")

    qT_d = dscr("qT_d", [4, 128, N], BF16); kT_d = dscr("kT_d", [2, 128, NK], BF16)
    v_d = dscr("v_d", [NK, 256], BF16); uT_d = dscr("uT_d", [4, 128, N], BF16)
    gbT_d = dscr("gbT_d", [4, 128, N], BF16); sT_d = dscr("sT_d", [4, 128, N], BF16)
    aT_d = dscr("aT_d", [4, 128, N], BF16)
    h_d = dscr("h_d", [N, D], BF16); aff_d = dscr("aff_d", [N, E], F32)

    es = ExitStack()
    with es:
        P = Prog(nc, es)

        uid = [0]

        def sb(st, name, shape, dt=F32):
            uid[0] += 1
            return st.enter_context(nc.sbuf_tensor("s%d_%s" % (uid[0], name), list(shape), dt))

        def ps(st, name, shape, dt=F32):
            uid[0] += 1
            return st.enter_context(nc.psum_tensor("p%d_%s" % (uid[0], name), list(shape), dt))

        ident_f = sb(es, "ident_f", [128, 128]); ident_b = sb(es, "ident_b", [128, 128], BF16)
        ones_b = sb(es, "ones_b", [128, 128], BF16); tri_b = sb(es, "tri_b", [128, 128], BF16)
        iota_j = sb(es, "iota_j", [128, 512]); idcols = sb(es, "idcols", [128, 32, 2], BF16)
        modb = sb(es, "modb", [128, 6, D])
        aff = sb(es, "aff", [128, NT, E])
        idx_i = sb(es, "idx_i", [128, E * 4], I32)
        gall = sb(es, "gall", [128, E * 4])

        P.dma('sp', lambda e: e.dma_start(out=ident_f[:], in_=ident_in.ap()), (), ('ident_f',))
        P.dma('pool', lambda e: e.dma_start(out=ident_b[:], in_=ident_in.ap()), (), ('ident_b',))
        P.dma('pool', lambda e: e.dma_start(out=tri_b[:], in_=tri_in.ap()), (), ('tri_b',))
        P.dma('sp', lambda e: e.dma_start(out=iota_j[:], in_=iota_in.ap()), (), ('iota_j',))
        iota_h = sb(es, "iota_h", [128, 512], mybir.dt.float16)
        P.op('dve', lambda e: e.tensor_copy(out=iota_h[:], in_=iota_j[:]), ('iota_j',), ('iota_h',))
        P.dma('pool', lambda e: e.dma_start(out=idcols[:].rearrange("p t c -> p (t c)"), in_=idcols_in.ap()), (), ('idcols',))
        P.op('dve', lambda e: e.memset(ones_b[:], 1.0), (), ('ones_b',))
        epsc = sb(es, "epsc", [128, 1])
        P.op('dve', lambda e: e.memset(epsc[:], EPS), (), ('epsc',))

        def compute_mod(st, layer, col_in, dst, slots, tag):
            cc = sb(st, "cc" + tag, [128, 8]); sc = sb(st, "sc" + tag, [128, 8])
            scb = sb(st, "scb" + tag, [128, 8, 128])
            awt = [sb(st, "awt%d" % i + tag, [128, 8, 512]) for i in range(2)]
            abt = sb(st, "abt" + tag, [128, 512])
            pm = [ps(st, "pm%d" % i + tag, [128, 512]) for i in range(2)]
            P.dma('sp', lambda e: e.dma_start(out=cc[:], in_=col_in.ap()), (), ('cc',))
            P.op('act', lambda e: e.activation(out=sc[:], in_=cc[:], func=AF.Silu), ('cc',), ('sc',))
            P.op('dve', lambda e: e.tensor_copy(out=scb[:], in_=sc[:].unsqueeze(2).to_broadcast([128, 8, 128])), ('sc',), ('scb',))
            it = 0
            for si, slot in enumerate(slots):
                for hh in range(2):
                    c0 = slot * D + hh * 512
                    b = it % 2
                    P.dma('sp', lambda e, b=b, c0=c0: e.dma_start(
                        out=awt[b][:], in_=ada_w[layer, :, c0:c0 + 512].rearrange("(k p) f -> p k f", p=128)),
                        (), ('awt%d' % b,))
                    P.dma('sp', lambda e, c0=c0: e.dma_start(
                        out=abt[:], in_=ada_b[layer:layer + 1, c0:c0 + 512].to_broadcast([128, 512])), (), ('abt',))
                    for k in range(8):
                        P.op('pe', lambda e, b=b, k=k: e.matmul(pm[b][:], lhsT=scb[:, k, :], rhs=awt[b][:, k, :],
                                                               start=(k == 0), stop=(k == 7)),
                             ('scb', 'awt%d' % b), ('pm%d' % b,))
                    P.op('dve', lambda e, b=b, si=si, hh=hh: e.tensor_tensor(
                        out=dst[:, si, hh * 512:(hh + 1) * 512], in0=pm[b][:], in1=abt[:], op=ALU.add),
                        ('pm%d' % b, 'abt'), ('mod',))
                    it += 1

        def rms_mod(xt, rx, shift, onepsc, outb, ro, tmp, junk, ssq, rstd, kx=""):
            if kx:
                P.op('act', lambda e: e.activation(out=junk[:], in_=xt, func=AF.Square, accum_out=ssq[:]), (rx, 'mod'), ('junk', 'ssq' + kx))
                P.op('act', lambda e: e.activation(out=rstd[:], in_=ssq[:], func=AF.Sqrt, scale=1.0 / D, bias=epsc[:, 0:1]), ('ssq' + kx, 'epsc'), ('rstd' + kx,))
                P.op('dve', lambda e: e.reciprocal(out=rstd[:], in_=rstd[:]), ('rstd' + kx,), ('rstd' + kx,))
                P.op('dve', lambda e: e.scalar_tensor_tensor(out=tmp[:], in0=xt, scalar=rstd[:, 0:1], in1=onepsc, op0=ALU.mult, op1=ALU.mult),
                     (rx, 'rstd' + kx, 'mod'), ('tmp' + kx,))
                P.op('dve', lambda e: e.tensor_tensor(out=outb, in0=tmp[:], in1=shift, op=ALU.add), ('tmp' + kx, 'mod'), (ro,))
                return
            P.op('act', lambda e: e.activation(out=junk[:], in_=xt, func=AF.Square, accum_out=ssq[:]), (rx, 'mod'), ('junk', 'ssq'))
            P.op('act', lambda e: e.activation(out=rstd[:], in_=ssq[:], func=AF.Sqrt, scale=1.0 / D, bias=epsc[:, 0:1]), ('ssq', 'epsc'), ('rstd',))
            P.op('dve', lambda e: e.reciprocal(out=rstd[:], in_=rstd[:]), ('rstd',), ('rstd',))
            P.op('dve', lambda e: e.scalar_tensor_tensor(out=tmp[:], in0=xt, scalar=rstd[:, 0:1], in1=onepsc, op0=ALU.mult, op1=ALU.mult),
                 (rx, 'rstd', 'mod'), ('tmp',))
            P.op('dve', lambda e: e.tensor_tensor(out=outb, in0=tmp[:], in1=shift, op=ALU.add), ('tmp', 'mod'), (ro,))

        class Banks:
            def __init__(self, st, tag):
                self.t = [ps(st, "bk%s%d" % (tag, i), [128, 1024]) for i in range(4)]
                self.i = 0

            def one(self):
                i = self.i
                self.i = (i + 1) % 8
                return self.t[i // 2][:, (i % 2) * 512:(i % 2 + 1) * 512], ('bk', i)

            def two(self):
                if self.i % 2:
                    self.i = (self.i + 1) % 8
                i = self.i
                self.i = (i + 2) % 8
                return self.t[i // 2][:, :], (('bk', i), ('bk', i + 1))

        def interleave(gens, G):
            it = iter(gens)
            active = []
            while True:
                while len(active) < G:
                    try:
                        active.append(next(it))
                    except StopIteration:
                        break
                if not active:
                    break
                for g in list(active):
                    try:
                        next(g)
                    except StopIteration:
                        active.remove(g)

        def transpose_mm(bk, src, rsrc):
            pt, kk = bk.two()
            for k in range(8):
                P.op('pe', lambda e, k=k: e.matmul(pt[:, k * 128:(k + 1) * 128], lhsT=src[:, k * 128:(k + 1) * 128], rhs=ident_b[:],
                                                   start=True, stop=True), (rsrc, 'ident_b'), kk)
            return pt, kk

        def ffn_prep_bufs(st, G):
            L = []
            for g in range(G):
                d = {}
                sfx = "_%d" % g
                d['sfx'] = sfx
                d['ssq'] = sb(st, "fq_ssq" + sfx, [128, 1]); d['rstd'] = sb(st, "fq_rstd" + sfx, [128, 1])
                d['tmp'] = sb(st, "fq_tmp" + sfx, [128, D]); d['h2'] = sb(st, "fq_h2" + sfx, [128, D]); d['h2b'] = sb(st, "fq_h2b" + sfx, [128, D], BF16)
                d['h2T'] = sb(st, "fq_h2T" + sfx, [128, 8, 128])
                d['ex'] = sb(st, "fq_ex" + sfx, [128, E]); d['esum'] = sb(st, "fq_esum" + sfx, [128, 1]); d['erec'] = sb(st, "fq_erec" + sfx, [128, 1])
                L.append(d)
            return L

        def ffn_prep_g(bk, d, rw, junk, x1, rx1, T):
            sfx = d['sfx']
            P.op('act', lambda e: e.activation(out=junk[:], in_=x1, func=AF.Square, accum_out=d['ssq'][:]), (rx1,), ('junk', 'ssq' + sfx))
            P.op('act', lambda e: e.activation(out=d['rstd'][:], in_=d['ssq'][:], func=AF.Sqrt, scale=1.0 / D, bias=epsc[:, 0:1]), ('ssq' + sfx, 'epsc'), ('rstd' + sfx,))
            P.op('dve', lambda e: e.reciprocal(out=d['rstd'][:], in_=d['rstd'][:]), ('rstd' + sfx,), ('rstd' + sfx,))
            P.op('dve', lambda e: e.scalar_tensor_tensor(out=d['tmp'][:], in0=x1, scalar=d['rstd'][:, 0:1], in1=modb[:, 4, :], op0=ALU.mult, op1=ALU.mult),
                 (rx1, 'rstd' + sfx, 'mod'), ('tmp' + sfx,))
            P.op('dve', lambda e: e.tensor_tensor(out=d['h2'][:], in0=d['tmp'][:], in1=modb[:, 3, :], op=ALU.add), ('tmp' + sfx, 'mod'), ('h2' + sfx,))
            P.op('act', lambda e: e.activation(out=d['h2b'][:], in_=d['h2'][:], func=AF.Copy), ('h2' + sfx,), ('h2b' + sfx,))
            P.dma('sp', lambda e: e.dma_start(out=h_d[T * 128:(T + 1) * 128, :], in_=d['h2b'][:]), ('h2b' + sfx,), (('h_d', T),))
            pt32, kk = bk.two()
            for k in range(8):
                P.op('pe', lambda e, k=k: e.transpose(pt32[:, k * 128:(k + 1) * 128], d['h2'][:, k * 128:(k + 1) * 128], ident_f[:]),
                     ('h2' + sfx, 'ident_f'), kk)
            yield
            P.op('dve', lambda e: e.tensor_copy(out=d['h2T'][:].rearrange("p k t -> p (k t)"), in_=pt32), kk, ('h2T' + sfx,))
            pr, kr = bk.one()
            for k in range(8):
                P.op('pe', lambda e, k=k: e.matmul(pr[:, 0:E], lhsT=d['h2T'][:, k, :], rhs=rw[:, k, :], start=(k == 0), stop=(k == 7)),
                     ('h2T' + sfx, 'fp_rw'), (kr,))
            yield
            P.op('act', lambda e: e.activation(out=d['ex'][:], in_=pr[:, 0:E], func=AF.Exp, accum_out=d['esum'][:]), (kr,), ('ex' + sfx, 'esum' + sfx))
            P.op('dve', lambda e: e.reciprocal(out=d['erec'][:], in_=d['esum'][:]), ('esum' + sfx,), ('erec' + sfx,))
            P.op('dve', lambda e: e.tensor_scalar(out=aff[:, T, :], in0=d['ex'][:], scalar1=d['erec'][:, 0:1], scalar2=None, op0=ALU.mult),
                 ('ex' + sfx, 'erec' + sfx), (('aff', T),))

        def ffn_prep_alloc(st):
            d = {}
            d['junk'] = sb(st, "fp_junk", [128, D]); d['ssq'] = sb(st, "fp_ssq", [128, 1]); d['rstd'] = sb(st, "fp_rstd", [128, 1])
            d['tmp'] = sb(st, "fp_tmp", [128, D]); d['h2'] = sb(st, "fp_h2", [128, D]); d['h2b'] = sb(st, "fp_h2b", [128, D], BF16)
            d['h2T'] = sb(st, "fp_h2T", [128, 8, 128]); d['rw'] = sb(st, "fp_rw", [128, 8, E])
            d['ex'] = sb(st, "fp_ex", [128, E]); d['esum'] = sb(st, "fp_esum", [128, 1]); d['erec'] = sb(st, "fp_erec", [128, 1])
            d['pt32'] = ps(st, "fp_pt32", [128, 1024]); d['pr'] = ps(st, "fp_pr", [128, E])
            return d

        def ffn_prep(d, layer, x1, rx1, T):
            rms_mod(x1, rx1, modb[:, 3, :], modb[:, 4, :], d['h2'][:], 'fp_h2', d['tmp'], d['junk'], d['ssq'], d['rstd'])
            P.op('act', lambda e: e.activation(out=d['h2b'][:], in_=d['h2'][:], func=AF.Copy), ('fp_h2',), ('fp_h2b',))
            P.dma('sp', lambda e: e.dma_start(out=h_d[T * 128:(T + 1) * 128, :], in_=d['h2b'][:]), ('fp_h2b',), (('h_d', T),))
            for k in range(8):
                P.op('pe', lambda e, k=k: e.transpose(d['pt32'][:, k * 128:(k + 1) * 128], d['h2'][:, k * 128:(k + 1) * 128], ident_f[:]),
                     ('fp_h2', 'ident_f'), ('fp_pt32',))
            P.op('dve', lambda e: e.tensor_copy(out=d['h2T'][:].rearrange("p k t -> p (k t)"), in_=d['pt32'][:]), ('fp_pt32',), ('fp_h2T',))
            for k in range(8):
                P.op('pe', lambda e, k=k: e.matmul(d['pr'][:], lhsT=d['h2T'][:, k, :], rhs=d['rw'][:, k, :], start=(k == 0), stop=(k == 7)),
                     ('fp_h2T', 'fp_rw'), ('fp_pr',))
            P.op('act', lambda e: e.activation(out=d['ex'][:], in_=d['pr'][:], func=AF.Exp, accum_out=d['esum'][:]), ('fp_pr',), ('fp_ex', 'fp_esum'))
            P.op('dve', lambda e: e.reciprocal(out=d['erec'][:], in_=d['esum'][:]), ('fp_esum',), ('fp_erec',))
            P.op('dve', lambda e: e.tensor_scalar(out=aff[:, T, :], in0=d['ex'][:], scalar1=d['erec'][:, 0:1], scalar2=None, op0=ALU.mult),
                 ('fp_ex', 'fp_erec'), (('aff', T),))

        def load_router(d, layer):
            P.dma('sp', lambda e: e.dma_start(out=d['rw'][:], in_=router_w[layer].rearrange("(k p) e -> p k e", p=128)), (), ('fp_rw',))

        def mixer0():
            st = ExitStack()
            with st:
                modc = sb(st, "modc", [128, 2, D])
                swa = ExitStack()
                wi = sb(swa, "wi", [128, 8, 3328], BF16)
                cosb = sb(swa, "cosb", [128, N]); sinb = sb(swa, "sinb", [128, N])
                gn = sb(swa, "gn", [128, 4])
                for hh in range(2):
                    P.dma('pool', lambda e, hh=hh: e.dma_start(
                        out=wi[:, :, hh * 1664:(hh + 1) * 1664],
                        in_=w_in[:, hh * 1664:(hh + 1) * 1664].rearrange("(k p) f -> p k f", p=128)), (), ('wi',))
                P.dma('sp', lambda e: e.dma_start(out=cosb[:], in_=cos_t.ap()), (), ('cosb',))
                P.dma('sp', lambda e: e.dma_start(out=sinb[:], in_=sin_t.ap()), (), ('sinb',))
                P.dma('sp', lambda e: e.dma_start(out=gn[:], in_=gains.ap()), (), ('gn',))
                with ExitStack() as s2:
                    compute_mod(s2, 0, ccol, modb, [0, 1, 2, 3, 4, 5], "a")
                    P.barrier()
                with ExitStack() as s2:
                    compute_mod(s2, 0, cccol, modc, [0, 1], "b")
                    P.barrier()
                P.op('dve', lambda e: e.tensor_scalar(out=modb[:, 1, :], in0=modb[:, 1, :], scalar1=1.0, scalar2=None, op0=ALU.add), ('mod',), ('mod',))
                P.op('dve', lambda e: e.tensor_scalar(out=modb[:, 4, :], in0=modb[:, 4, :], scalar1=1.0, scalar2=None, op0=ALU.add), ('mod',), ('mod',))
                P.op('dve', lambda e: e.tensor_scalar(out=modc[:, 1, :], in0=modc[:, 1, :], scalar1=1.0, scalar2=None, op0=ALU.add), ('mod',), ('mod',))
                P.barrier()
                with ExitStack() as sa:
                    xt = [sb(sa, "xt%d" % i, [128, D]) for i in range(2)]
                    junk = sb(sa, "junk", [128, D])
                    tmp = [sb(sa, "tmp%d" % i, [128, D]) for i in range(2)]
                    ssq = [sb(sa, "ssq%d" % i, [128, 1]) for i in range(2)]; rstd = [sb(sa, "rstd%d" % i, [128, 1]) for i in range(2)]
                    hb = [sb(sa, "hb%d" % i, [128, D], BF16) for i in range(2)]
                    hT = [sb(sa, "hT%d" % i, [128, 8, 512], BF16) for i in range(2)]
                    sqb = [sb(sa, "sqb%d" % i, [128, 512], BF16) for i in range(2)]
                    r1 = [sb(sa, "r1_%d" % i, [128, 512]) for i in range(2)]
                    qn = [sb(sa, "qn%d" % i, [128, 512]) for i in range(2)]; qsn = [sb(sa, "qsn%d" % i, [128, 512]) for i in range(2)]
                    qf = [sb(sa, "qf%d" % i, [128, 512], BF16) for i in range(2)]
                    vb = [sb(sa, "vb%d" % i, [128, 256], BF16) for i in range(2)]
                    gcs = [sb(sa, "gcs%d" % i, [128, 512]) for i in range(2)]
                    ub = [sb(sa, "ub%d" % i, [128, 512], BF16) for i in range(2)]
                    gbb = [sb(sa, "gbb%d" % i, [128, 512], BF16) for i in range(2)]
                    bk = Banks(sa, "pa")

                    def tiles_stage(s):
                        isctx = s < 0
                        sp = (s + 1) % 2
                        for t in range(2 if isctx else 4):
                            b = t % 2
                            src = ctx_in[t * 128:(t + 1) * 128, :] if isctx else x_in[(s * 4 + t) * 128:(s * 4 + t + 1) * 128, :]
                            P.dma('sp', lambda e: e.dma_start(out=xt[b][:], in_=src), (), ('xt%d' % b,))
                            shift = modc[:, 0, :] if isctx else modb[:, 0, :]
                            onep = modc[:, 1, :] if isctx else modb[:, 1, :]
                            rms_mod(xt[b][:], 'xt%d' % b, shift, onep, hb[b][:], 'hb%d' % b, tmp[b], junk, ssq[b], rstd[b], kx="_a%d" % b)
                            pt, kk = transpose_mm(bk, hb[b], 'hb%d' % b)
                            P.op('act', lambda e: e.activation(out=hT[sp][:, :, t * 128:(t + 1) * 128],
                                                               in_=pt.rearrange("p (k t) -> p k t", k=8), func=AF.Copy), kk, ('hT%d' % sp,))

                    def make_chains(s):
                        isctx = s < 0
                        sp = (s + 1) % 2
                        ntile = 2 if isctx else 4
                        ntok = ntile * 128
                        hTk = 'hT%d' % sp
                        chains = []

                        def proj(c0, n=ntok):
                            pt, kp = bk.one()
                            for k in range(8):
                                P.op('pe', lambda e, k=k: e.matmul(pt[:, 0:n], lhsT=wi[:, k, c0:c0 + 128], rhs=hT[sp][:, k, 0:n],
                                                                   start=(k == 0), stop=(k == 7)), ('wi', hTk), (kp,))
                            return pt, kp

                        def qk_chain(ci, c0, c1, gc0, kind, hidx):
                            st_ = {}
                            i2 = ci % 2

                            def do_proj():
                                st_['pq'] = proj(c0)
                                if not isctx:
                                    st_['pqs'] = proj(c1)

                            def do_post():
                                pq, kq = st_['pq']
                                P.op('act', lambda e: e.activation(out=sqb[i2][:, 0:ntok], in_=pq[:, 0:ntok], func=AF.Square), (kq,), ('sqb%d' % i2,))
                                pss, kss = bk.one()
                                P.op('pe', lambda e: e.matmul(pss[:, 0:ntok], lhsT=ones_b[:], rhs=sqb[i2][:, 0:ntok], start=True, stop=True),
                                     ('sqb%d' % i2, 'ones_b'), (kss,))
                                P.op('act', lambda e: e.activation(out=r1[i2][:, 0:ntok], in_=pss[:, 0:ntok], func=AF.Ln, scale=1.0 / 128, bias=epsc[:, 0:1]),
                                     (kss, 'epsc'), ('r1_%d' % i2,))
                                P.op('act', lambda e: e.activation(out=r1[i2][:, 0:ntok], in_=r1[i2][:, 0:ntok], func=AF.Exp, scale=-0.5), ('r1_%d' % i2,), ('r1_%d' % i2,))
                                if isctx:
                                    P.op('dve', lambda e: e.scalar_tensor_tensor(
                                        out=qf[i2][:, 0:ntok], in0=pq[:, 0:ntok], scalar=gn[:, gc0:gc0 + 1], in1=r1[i2][:, 0:ntok],
                                        op0=ALU.mult, op1=ALU.mult), (kq, 'gn', 'r1_%d' % i2), ('qf%d' % i2,))
                                    P.dma('sp', lambda e: e.dma_start(out=kT_d[hidx, :, 0:CTX], in_=qf[i2][:, 0:CTX]), ('qf%d' % i2,), ())
                                    return
                                pqs, kqs = st_['pqs']
                                P.op('dve', lambda e: e.scalar_tensor_tensor(
                                    out=qn[i2][:], in0=pq, scalar=gn[:, gc0:gc0 + 1], in1=r1[i2][:], op0=ALU.mult, op1=ALU.mult),
                                    (kq, 'gn', 'r1_%d' % i2), ('qn%d' % i2,))
                                P.op('dve', lambda e: e.scalar_tensor_tensor(
                                    out=qsn[i2][:], in0=pqs, scalar=gn[:, gc0 + 1:gc0 + 2], in1=r1[i2][:], op0=ALU.mult, op1=ALU.mult),
                                    (kqs, 'gn', 'r1_%d' % i2), ('qsn%d' % i2,))
                                P.op('dve', lambda e: e.tensor_tensor(out=qn[i2][:], in0=qn[i2][:], in1=cosb[:, s * 512:(s + 1) * 512], op=ALU.mult),
                                     ('qn%d' % i2, 'cosb'), ('qn%d' % i2,))
                                P.op('dve', lambda e: e.tensor_tensor(out=qsn[i2][:], in0=qsn[i2][:], in1=sinb[:, s * 512:(s + 1) * 512], op=ALU.mult),
                                     ('qsn%d' % i2, 'sinb'), ('qsn%d' % i2,))
                                P.op('dve', lambda e: e.tensor_tensor(out=qf[i2][:], in0=qn[i2][:], in1=qsn[i2][:], op=ALU.add),
                                     ('qn%d' % i2, 'qsn%d' % i2), ('qf%d' % i2,))
                                if kind == 'q':
                                    P.dma('sp', lambda e: e.dma_start(out=qT_d[hidx, :, s * 512:(s + 1) * 512], in_=qf[i2][:]), ('qf%d' % i2,), ())
                                else:
                                    P.dma('sp', lambda e: e.dma_start(out=kT_d[hidx, :, CTX + s * 512:CTX + (s + 1) * 512], in_=qf[i2][:]),
                                          ('qf%d' % i2,), ())
                            return do_proj, do_post

                        def v_chain(t):
                            st_ = {}
                            b = t % 2

                            def do_proj():
                                pv, kv_ = bk.one()
                                for k in range(8):
                                    P.op('pe', lambda e, k=k: e.matmul(pv[:, 0:256], lhsT=hT[sp][:, k, t * 128:(t + 1) * 128], rhs=wi[:, k, 768:1024],
                                                                       start=(k == 0), stop=(k == 7)), ('wi', hTk), (kv_,))
                                st_['pv'] = (pv, kv_)

                            def do_post():
                                pv, kv_ = st_['pv']
                                P.op('act', lambda e: e.activation(out=vb[b][:], in_=pv[:, 0:256], func=AF.Copy), (kv_,), ('vb%d' % b,))
                                row0 = t * 128 if isctx else CTX + (s * 4 + t) * 128
                                P.dma('sp', lambda e: e.dma_start(out=v_d[row0:row0 + 128, :], in_=vb[b][:]), ('vb%d' % b,), ())
                            return do_proj, do_post

                        def conv_chain(c):
                            st_ = {}
                            b = c % 2

                            def do_proj():
                                st_['gb'] = proj(1024 + c * 128); st_['gc'] = proj(1536 + c * 128); st_['hx'] = proj(2048 + c * 128)

                            def do_post():
                                (pgb, kgb), (pgc, kgc), (phx, khx) = st_['gb'], st_['gc'], st_['hx']
                                P.op('act', lambda e: e.activation(out=gcs[b][:], in_=pgc, func=AF.Copy), (kgc,), ('gcs%d' % b,))
                                P.op('dve', lambda e: e.tensor_tensor(out=ub[b][:], in0=phx, in1=gcs[b][:], op=ALU.mult), (khx, 'gcs%d' % b), ('ub%d' % b,))
                                P.op('act', lambda e: e.activation(out=gbb[b][:], in_=pgb, func=AF.Copy), (kgb,), ('gbb%d' % b,))
                                P.dma('sp', lambda e: e.dma_start(out=uT_d[c, :, s * 512:(s + 1) * 512], in_=ub[b][:]), ('ub%d' % b,), ())
                                P.dma('sp', lambda e: e.dma_start(out=gbT_d[c, :, s * 512:(s + 1) * 512], in_=gbb[b][:]), ('gbb%d' % b,), ())
                            return do_proj, do_post

                        ci = 0
                        if not isctx:
                            for h in range(4):
                                chains.append(qk_chain(ci, h * 128, 2560 + h * 128, 0, 'q', h)); ci += 1
                                chains.append(conv_chain(h))
                        for kv in range(2):
                            chains.append(qk_chain(ci, 512 + kv * 128, 3072 + kv * 128, 2, 'k', kv)); ci += 1
                            for t in range(kv * ntile // 2, (kv + 1) * ntile // 2):
                                chains.append(v_chain(t))
                        return chains

                    tiles_stage(-1)
                    for s in range(-1, 8):
                        if s + 1 < 8:
                            tiles_stage(s + 1)
                        pend = None
                        for (pj, po) in make_chains(s):
                            pj()
                            if pend is not None:
                                pend()
                            pend = po
                        pend()
                    P.barrier()
                swa.close()
                with ExitStack() as sbk:
                    cw = sb(sbk, "cw", [128, 12])
                    u = sb(sbk, "u", [128, N + 2], BF16); acc = sb(sbk, "acc", [128, N])
                    gbt = sb(sbk, "gbt", [128, N], BF16); so = sb(sbk, "so", [128, N], BF16)
                    P.dma('sp', lambda e: e.dma_start(out=cw[:], in_=convw.ap()), (), ('cw',))
                    for c in range(4):
                        P.op('dve', lambda e: e.memset(u[:, 0:1], 0.0), (), ('u',))
                        P.op('dve', lambda e: e.memset(u[:, N + 1:N + 2], 0.0), (), ('u',))
                        P.dma('sp', lambda e, c=c: e.dma_start(out=u[:, 1:N + 1], in_=uT_d[c]), (), ('u',))
                        P.dma('sp', lambda e, c=c: e.dma_start(out=gbt[:], in_=gbT_d[c]), (), ('gbt',))
                        P.op('dve', lambda e, c=c: e.tensor_scalar(out=acc[:], in0=u[:, 1:N + 1], scalar1=cw[:, c * 3 + 1:c * 3 + 2], scalar2=None,
                                                                   op0=ALU.mult), ('u', 'cw'), ('acc',))
                        P.op('dve', lambda e, c=c: e.scalar_tensor_tensor(out=acc[:], in0=u[:, 0:N], scalar=cw[:, c * 3:c * 3 + 1], in1=acc[:],
                                                                          op0=ALU.mult, op1=ALU.add), ('u', 'cw', 'acc'), ('acc',))
                        P.op('dve', lambda e, c=c: e.scalar_tensor_tensor(out=acc[:], in0=u[:, 2:N + 2], scalar=cw[:, c * 3 + 2:c * 3 + 3], in1=acc[:],
                                                                          op0=ALU.mult, op1=ALU.add), ('u', 'cw', 'acc'), ('acc',))
                        P.op('dve', lambda e: e.tensor_tensor(out=so[:], in0=acc[:], in1=gbt[:], op=ALU.mult), ('acc', 'gbt'), ('so',))
                        P.dma('sp', lambda e, c=c: e.dma_start(out=sT_d[c], in_=so[:]), ('so',), ())
                    P.barrier()
                with ExitStack() as sc:
                    kT = sb(sc, "kT", [128, 2, NK], BF16); vv = sb(sc, "vv", [128, 34, 256], BF16)
                    qs = [sb(sc, "qs%d" % i, [128, 512], BF16) for i in range(2)]
                    pb = [sb(sc, "pb%d" % i, [128, 512], BF16) for i in range(3)]
                    rec = sb(sc, "rec", [128, 512]); ab = [sb(sc, "ab%d" % i, [128, 512], BF16) for i in range(2)]
                    pS = [ps(sc, "pS%d" % i, [128, 512]) for i in range(3)]
                    pO = [ps(sc, "pO%d" % i, [128, 512]) for i in range(2)]
                    pD = [ps(sc, "pD%d" % i, [128, 512]) for i in range(2)]
                    P.dma('sp', lambda e: e.dma_start(out=kT[:], in_=kT_d.ap().rearrange("k p n -> p k n")), (), ('kT',))
                    P.dma('sp', lambda e: e.dma_start(out=vv[:], in_=v_d.ap().rearrange("(c p) f -> p c f", p=128)), (), ('vv',))
                    groups = [(h, s_) for h in range(4) for s_ in range(8)]
                    iters = [(g, c) for g in range(len(groups)) for c in range(34)]

                    def load_q(g):
                        h, s_ = groups[g]
                        b = g % 2
                        P.dma('sp', lambda e: e.dma_start(out=qs[b][:], in_=qT_d[h, :, s_ * 512:(s_ + 1) * 512]), (), ('qs%d' % b,))

                    def qk_exp(i):
                        g, c = iters[i]
                        h, s_ = groups[g]
                        kv = h // 2
                        b = g % 2
                        j = i % 3
                        if c == 0 and g + 1 < len(groups):
                            load_q(g + 1)
                        P.op('pe', lambda e: e.matmul(pS[j][:], lhsT=kT[:, kv, c * 128:(c + 1) * 128], rhs=qs[b][:],
                                                      start=True, stop=True), ('kT', 'qs%d' % b), ('pS%d' % j,))
                        P.op('act', lambda e: e.activation(out=pb[j][:], in_=pS[j][:], func=AF.Exp, scale=float(128 ** -0.5)),
                             ('pS%d' % j,), ('pb%d' % j,))

                    load_q(0)
                    qk_exp(0)
                    qk_exp(1)
                    for i, (g, c) in enumerate(iters):
                        h, s_ = groups[g]
                        kv = h // 2
                        b = g % 2
                        j = i % 3
                        if i + 2 < len(iters):
                            qk_exp(i + 2)
                        P.op('pe', lambda e: e.matmul(pO[b][:], lhsT=vv[:, c, kv * 128:(kv + 1) * 128], rhs=pb[j][:],
                                                      start=(c == 0), stop=(c == 33)), ('vv', 'pb%d' % j), ('pO%d' % b,))
                        P.op('pe', lambda e: e.matmul(pD[b][:], lhsT=ones_b[:], rhs=pb[j][:],
                                                      start=(c == 0), stop=(c == 33)), ('ones_b', 'pb%d' % j), ('pD%d' % b,))
                        if c == 33:
                            P.op('dve', lambda e: e.reciprocal(out=rec[:], in_=pD[b][:]), ('pD%d' % b,), ('rec',))
                            P.op('dve', lambda e: e.tensor_tensor(out=ab[b][:], in0=pO[b][:], in1=rec[:], op=ALU.mult),
                                 ('pO%d' % b, 'rec'), ('ab%d' % b,))
                            P.dma('sp', lambda e: e.dma_start(out=aT_d[h, :, s_ * 512:(s_ + 1) * 512], in_=ab[b][:]), ('ab%d' % b,), ())
                    P.barrier()
                with ExitStack() as sd:
                    G = 2
                    wo = sb(sd, "wo", [128, 8, D], BF16)
                    asl = [sb(sd, "asl%d" % i, [128, 8, 512], BF16) for i in range(2)]
                    xt = [sb(sd, "dxt%d" % i, [128, D]) for i in range(G)]
                    x1 = [sb(sd, "x1_%d" % i, [128, D]) for i in range(G)]
                    rw = sb(sd, "drw", [128, 8, E]); junk = sb(sd, "djunk", [128, D])
                    fpb = ffn_prep_bufs(sd, G)
                    bk = Banks(sd, "pd")
                    P.dma('sp', lambda e: e.dma_start(out=rw[:], in_=router_w[0].rearrange("(k p) e -> p k e", p=128)), (), ('fp_rw',))
                    P.dma('pool', lambda e: e.dma_start(out=wo[:], in_=w_out0.ap().rearrange("(k p) f -> p k f", p=128)), (), ('wo',))

                    def load_slab(s):
                        b = s % 2
                        P.dma('sp', lambda e: e.dma_start(out=asl[b][:, 0:4, :], in_=aT_d[:, :, s * 512:(s + 1) * 512].rearrange("h p t -> p h t")),
                              (), ('asl%d' % b,))
                        P.dma('sp', lambda e: e.dma_start(out=asl[b][:, 4:8, :], in_=sT_d[:, :, s * 512:(s + 1) * 512].rearrange("h p t -> p h t")),
                              (), ('asl%d' % b,))

                    def tile_gen(T):
                        s, t = T // 4, T % 4
                        b = s % 2
                        g = T % G
                        sx = "_%d" % g
                        if t == 0 and s + 1 < 8:
                            load_slab(s + 1)
                        P.dma('sp', lambda e: e.dma_start(out=xt[g][:], in_=x_in[T * 128:(T + 1) * 128, :]), (), ('dxt' + sx,))
                        pys = []
                        for hh in range(2):
                            py_, ky = bk.one()
                            pys.append((py_, ky))
                            for k in range(8):
                                P.op('pe', lambda e, k=k, hh=hh, py_=py_: e.matmul(
                                    py_, lhsT=asl[b][:, k, t * 128:(t + 1) * 128], rhs=wo[:, k, hh * 512:(hh + 1) * 512],
                                    start=(k == 0), stop=(k == 7)), ('asl%d' % b, 'wo'), (ky,))
                        yield
                        for hh in range(2):
                            py_, ky = pys[hh]
                            P.op('dve', lambda e, hh=hh, py_=py_: e.tensor_tensor(out=x1[g][:, hh * 512:(hh + 1) * 512], in0=py_,
                                                                                 in1=modb[:, 2, hh * 512:(hh + 1) * 512], op=ALU.mult),
                                 (ky, 'mod'), ('x1' + sx,))
                        P.op('dve', lambda e: e.tensor_tensor(out=x1[g][:], in0=x1[g][:], in1=xt[g][:], op=ALU.add), ('x1' + sx, 'dxt' + sx), ('x1' + sx,))
                        P.dma('sp', lambda e: e.dma_start(out=out_d[T * 128:(T + 1) * 128, :], in_=x1[g][:]), ('x1' + sx,), (('xd', T),))
                        yield from ffn_prep_g(bk, fpb[g], rw, junk, x1[g][:], 'x1' + sx, T)

                    load_slab(0)
                    interleave([tile_gen(T) for T in range(NT)], G)
                    P.barrier()

        def mixer1():
            G = 2
            with ExitStack() as st:
                with ExitStack() as s2:
                    compute_mod(s2, 1, ccol, modb, [0, 1, 2, 3, 4, 5], "c")
                    P.barrier()
                P.op('dve', lambda e: e.tensor_scalar(out=modb[:, 1, :], in0=modb[:, 1, :], scalar1=1.0, scalar2=None, op0=ALU.add), ('mod',), ('mod',))
                P.op('dve', lambda e: e.tensor_scalar(out=modb[:, 4, :], in0=modb[:, 4, :], scalar1=1.0, scalar2=None, op0=ALU.add), ('mod',), ('mod',))
                P.barrier()
                wi = sb(st, "gwi", [128, 8, 2 * D], BF16); wo = sb(st, "gwo", [128, 8, D], BF16)
                swT = sb(st, "swT", [128, 8, 128], BF16); sbq = sb(st, "sbq", [128, 8]); snb = sb(st, "snb", [128, D])
                rw = sb(st, "grw", [128, 8, E]); junk = sb(st, "gjunk", [128, D])
                xt = [sb(st, "gxt%d" % i, [128, D]) for i in range(G)]
                tmp = [sb(st, "gtmp%d" % i, [128, D]) for i in range(G)]
                ssq = [sb(st, "gssq%d" % i, [128, 1]) for i in range(G)]; rstd = [sb(st, "grstd%d" % i, [128, 1]) for i in range(G)]
                hb = [sb(st, "ghb%d" % i, [128, D], BF16) for i in range(G)]; hT = [sb(st, "ghT%d" % i, [128, 8, 128], BF16) for i in range(G)]
                z = [sb(st, "gz%d" % i, [128, 2 * D]) for i in range(G)]; vnb = [sb(st, "gvnb%d" % i, [128, D], BF16) for i in range(G)]
                mb = [sb(st, "gmb%d" % i, [128, D], BF16) for i in range(G)]; mT = [sb(st, "gmT%d" % i, [128, 8, 128], BF16) for i in range(G)]
                x3 = [sb(st, "x3_%d" % i, [128, D]) for i in range(G)]
                fpb = ffn_prep_bufs(st, G)
                bk = Banks(st, "m1")
                P.dma('sp', lambda e: e.dma_start(out=rw[:], in_=router_w[1].rearrange("(k p) e -> p k e", p=128)), (), ('fp_rw',))
                for hh in range(2):
                    P.dma('pool', lambda e, hh=hh: e.dma_start(out=wi[:, :, hh * D:(hh + 1) * D],
                                                               in_=sg_w_in[:, hh * D:(hh + 1) * D].rearrange("(k p) f -> p k f", p=128)), (), ('gwi',))
                P.dma('pool', lambda e: e.dma_start(out=wo[:], in_=sg_w_out.ap().rearrange("(k p) f -> p k f", p=128)), (), ('gwo',))
                P.dma('pool', lambda e: e.dma_start(out=swT[:].rearrange("p g q -> p (g q)"), in_=sgwT.ap()), (), ('swT',))
                P.dma('sp', lambda e: e.dma_start(out=sbq[:], in_=sgb.ap()), (), ('sbq',))
                P.dma('sp', lambda e: e.dma_start(out=snb[:], in_=sg_norm.ap().to_broadcast([128, D])), (), ('snb',))

                def tile_gen(T):
                    g = T % G
                    sx = "_%d" % g
                    P.dma('sp', lambda e: e.dma_start(out=xt[g][:], in_=out_d[T * 128:(T + 1) * 128, :]), (('xd', T),), ('gxt' + sx,))
                    P.op('act', lambda e: e.activation(out=junk[:], in_=xt[g][:], func=AF.Square, accum_out=ssq[g][:]), ('gxt' + sx,), ('junk', 'gssq' + sx))
                    P.op('act', lambda e: e.activation(out=rstd[g][:], in_=ssq[g][:], func=AF.Sqrt, scale=1.0 / D, bias=epsc[:, 0:1]), ('gssq' + sx, 'epsc'), ('grstd' + sx,))
                    P.op('dve', lambda e: e.reciprocal(out=rstd[g][:], in_=rstd[g][:]), ('grstd' + sx,), ('grstd' + sx,))
                    P.op('dve', lambda e: e.scalar_tensor_tensor(out=tmp[g][:], in0=xt[g][:], scalar=rstd[g][:, 0:1], in1=modb[:, 1, :], op0=ALU.mult, op1=ALU.mult),
                         ('gxt' + sx, 'grstd' + sx, 'mod'), ('gtmp' + sx,))
                    P.op('dve', lambda e: e.tensor_tensor(out=hb[g][:], in0=tmp[g][:], in1=modb[:, 0, :], op=ALU.add), ('gtmp' + sx, 'mod'), ('ghb' + sx,))
                    pt, kk = transpose_mm(bk, hb[g], 'ghb' + sx)
                    yield
                    P.op('act', lambda e: e.activation(out=hT[g][:].rearrange("p k t -> p (k t)"), in_=pt, func=AF.Copy), kk, ('ghT' + sx,))
                    for n in range(4):
                        pz, kz = bk.one()
                        for k in range(8):
                            P.op('pe', lambda e, k=k, n=n, pz=pz: e.matmul(pz, lhsT=hT[g][:, k, :], rhs=wi[:, k, n * 512:(n + 1) * 512],
                                                                          start=(k == 0), stop=(k == 7)), ('ghT' + sx, 'gwi'), (kz,))
                        P.op('act', lambda e, n=n, pz=pz: e.activation(out=z[g][:, n * 512:(n + 1) * 512], in_=pz, func=AF.Gelu_apprx_tanh),
                             (kz,), (('gz' + sx, n),))
                        if n == 1:
                            yield
                    yield
                    zv = (('gz' + sx, 2), ('gz' + sx, 3))
                    P.op('act', lambda e: e.activation(out=junk[:], in_=z[g][:, D:2 * D], func=AF.Square, accum_out=ssq[g][:]), zv, ('junk', 'gssq' + sx))
                    P.op('act', lambda e: e.activation(out=rstd[g][:], in_=ssq[g][:], func=AF.Sqrt, scale=1.0 / D, bias=epsc[:, 0:1]), ('gssq' + sx, 'epsc'), ('grstd' + sx,))
                    P.op('dve', lambda e: e.reciprocal(out=rstd[g][:], in_=rstd[g][:]), ('grstd' + sx,), ('grstd' + sx,))
                    P.op('dve', lambda e: e.scalar_tensor_tensor(out=vnb[g][:], in0=z[g][:, D:2 * D], scalar=rstd[g][:, 0:1], in1=snb[:], op0=ALU.mult, op1=ALU.mult),
                         zv + ('grstd' + sx, 'snb'), ('gvnb' + sx,))
                    pm, km = bk.two()
                    for gg in range(8):
                        P.op('pe', lambda e, gg=gg: e.matmul(pm[:, gg * 128:(gg + 1) * 128], lhsT=swT[:, gg, :], rhs=vnb[g][:, gg * 128:(gg + 1) * 128],
                                                             start=True, stop=True), ('swT', 'gvnb' + sx), km)
                    yield
                    for gg in range(8):
                        P.op('dve', lambda e, gg=gg: e.scalar_tensor_tensor(out=mb[g][:, gg * 128:(gg + 1) * 128], in0=pm[:, gg * 128:(gg + 1) * 128],
                                                                            scalar=sbq[:, gg:gg + 1], in1=z[g][:, gg * 128:(gg + 1) * 128],
                                                                            op0=ALU.add, op1=ALU.mult),
                             km + ('sbq', ('gz' + sx, 0), ('gz' + sx, 1)), ('gmb' + sx,))
                    pt2, kk2 = transpose_mm(bk, mb[g], 'gmb' + sx)
                    yield
                    P.op('act', lambda e: e.activation(out=mT[g][:].rearrange("p k t -> p (k t)"), in_=pt2, func=AF.Copy), kk2, ('gmT' + sx,))
                    pys = []
                    for hh in range(2):
                        py_, ky = bk.one()
                        pys.append((py_, ky))
                        for k in range(8):
                            P.op('pe', lambda e, k=k, hh=hh, py_=py_: e.matmul(py_, lhsT=mT[g][:, k, :], rhs=wo[:, k, hh * 512:(hh + 1) * 512],
                                                                              start=(k == 0), stop=(k == 7)), ('gmT' + sx, 'gwo'), (ky,))
                    yield
                    for hh in range(2):
                        py_, ky = pys[hh]
                        P.op('dve', lambda e, hh=hh, py_=py_: e.tensor_tensor(out=x3[g][:, hh * 512:(hh + 1) * 512], in0=py_,
                                                                             in1=modb[:, 2, hh * 512:(hh + 1) * 512], op=ALU.mult),
                             (ky, 'mod'), ('x3' + sx,))
                    P.op('dve', lambda e: e.tensor_tensor(out=x3[g][:], in0=x3[g][:], in1=xt[g][:], op=ALU.add), ('x3' + sx, 'gxt' + sx), ('x3' + sx,))
                    P.dma('sp', lambda e: e.dma_start(out=out_d[T * 128:(T + 1) * 128, :], in_=x3[g][:]), ('x3' + sx,), (('xd', T),))
                    yield from ffn_prep_g(bk, fpb[g], rw, junk, x3[g][:], 'x3' + sx, T)

                interleave([tile_gen(T) for T in range(NT)], G)
                P.barrier()

        def ffn(layer):
            with ExitStack() as se:
                lo = sb(se, "lo", [128, E]); hi = sb(se, "hi", [128, E]); mid = sb(se, "mid", [128, E])
                cmpb = sb(se, "cmpb", [128, NT, E], BF16); cnt = sb(se, "cnt", [128, E]); ge = sb(se, "ge", [128, E])
                t1 = sb(se, "t1", [128, E]); t2 = sb(se, "t2", [128, E])
                mf = sb(se, "mf", [128, NT, E]); offs = sb(se, "offs", [128, NT, E]); tot = sb(se, "tot", [128, NT, E])
                gpos = sb(se, "gpos", [128, NT, E])
                ohb = [sb(se, "ohb%d" % i, [128, 512], BF16) for i in range(4)]
                idf = sb(se, "idf", [128, E * 4])
                pc = ps(se, "pc", [128, 512]); pA = ps(se, "pA", [128, 512]); pB = ps(se, "pB", [128, 512])
                pid = ps(se, "pid", [128, E * 4 * 4])
                idg = sb(se, "idg", [128, NT, E, 4], BF16); hif = sb(se, "hif", [128, NT, E])
                affr = tuple(('aff', T) for T in range(NT))
                P.op('dve', lambda e: e.memset(lo[:], 0.0), (), ('lo',))
                P.op('dve', lambda e: e.memset(mid[:], 0.5), (), ('mid',))
                for it in range(NBIS):
                    w = 2.0 ** -(it + 1)
                    P.op('dve', lambda e: e.tensor_tensor(out=cmpb[:], in0=aff[:], in1=mid[:].unsqueeze(1).to_broadcast([128, NT, E]), op=ALU.is_ge),
                         affr + ('mid',), ('cmpb',))
                    P.op('pe', lambda e: e.matmul(pc[:], lhsT=ones_b[:], rhs=cmpb[:].rearrange("p t e -> p (t e)"), start=True, stop=True),
                         ('cmpb', 'ones_b'), ('pc',))
                    P.op('dve', lambda e: e.tensor_reduce(out=cnt[:], in_=pc[:].rearrange("p (t e) -> p e t", e=E), axis=AX.X, op=ALU.add),
                         ('pc',), ('cnt',))
                    P.op('dve', lambda e, w=w: e.tensor_scalar(out=ge[:], in0=cnt[:], scalar1=float(CAP), scalar2=w, op0=ALU.is_ge, op1=ALU.mult),
                         ('cnt',), ('ge',))
                    P.op('dve', lambda e: e.tensor_tensor(out=lo[:], in0=lo[:], in1=ge[:], op=ALU.add), ('lo', 'ge'), ('lo',))
                    P.op('dve', lambda e, w=w: e.tensor_scalar(out=mid[:], in0=lo[:], scalar1=0.5 * w, scalar2=None, op0=ALU.add), ('lo',), ('mid',))
                P.op('dve', lambda e: e.tensor_tensor(out=mf[:], in0=aff[:], in1=lo[:].unsqueeze(1).to_broadcast([128, NT, E]), op=ALU.is_ge),
                     affr + ('lo',), ('mf',))
                P.op('dve', lambda e: e.tensor_copy(out=cmpb[:], in_=mf[:]), ('mf',), ('cmpb',))
                P.op('pe', lambda e: e.matmul(pA[:], lhsT=tri_b[:], rhs=cmpb[:].rearrange("p t e -> p (t e)"), start=True, stop=True),
                     ('cmpb', 'tri_b'), ('pA',))
                P.op('pe', lambda e: e.matmul(pB[:], lhsT=ones_b[:], rhs=cmpb[:].rearrange("p t e -> p (t e)"), start=True, stop=True),
                     ('cmpb', 'ones_b'), ('pB',))
                P.op('dve', lambda e: e.tensor_copy(out=tot[:].rearrange("p t e -> p (t e)"), in_=pB[:]), ('pB',), ('tot',))
                P.op('dve', lambda e: e.memset(offs[:, 0, :], 0.0), (), ('offs',))
                for T in range(1, NT):
                    P.op('dve', lambda e, T=T: e.tensor_tensor(out=offs[:, T, :], in0=offs[:, T - 1, :], in1=tot[:, T - 1, :], op=ALU.add),
                         ('offs', 'tot'), ('offs',))
                P.op('dve', lambda e: e.tensor_tensor(out=gpos[:].rearrange("p t e -> p (t e)"), in0=pA[:], in1=offs[:].rearrange("p t e -> p (t e)"), op=ALU.add),
                     ('pA', 'offs'), ('gpos',))
                P.op('dve', lambda e: e.tensor_tensor(out=gpos[:], in0=gpos[:], in1=mf[:], op=ALU.mult), ('gpos', 'mf'), ('gpos',))
                P.op('dve', lambda e: e.tensor_scalar(out=gpos[:], in0=gpos[:], scalar1=-1.0, scalar2=None, op0=ALU.add), ('gpos',), ('gpos',))
                P.op('dve', lambda e: e.tensor_copy(out=idg[:, :, :, 0:2], in_=idcols[:].unsqueeze(2).to_broadcast([128, NT, E, 2])), ('idcols',), ('idg',))
                P.op('dve', lambda e: e.tensor_copy(out=idg[:, :, :, 2], in_=aff[:]), affr, ('idg',))
                P.op('dve', lambda e: e.tensor_copy(out=hif[:], in_=idg[:, :, :, 2]), ('idg',), ('hif',))
                P.op('dve', lambda e: e.tensor_tensor(out=idg[:, :, :, 3], in0=aff[:], in1=hif[:], op=ALU.subtract), affr + ('hif',), ('idg',))
                first = True
                n = 0
                for T in range(NT):
                    for ex in range(E):
                        b = n % 4
                        eng = 'dve'
                        P.op(eng, lambda e, b=b, T=T, ex=ex: e.tensor_scalar(out=ohb[b][:], in0=iota_h[:], scalar1=gpos[:, T, ex:ex + 1], scalar2=None,
                                                                             op0=ALU.is_equal), ('iota_h', 'gpos'), ('ohb%d' % b,))
                        for jt in range(4):
                            col = (ex * 4 + jt) * 4
                            P.op('pe', lambda e, b=b, jt=jt, col=col, T=T, ex=ex, first=first: e.matmul(
                                pid[:, col:col + 4], lhsT=ohb[b][:, jt * 128:(jt + 1) * 128], rhs=idg[:, T, ex, :],
                                start=first, stop=(T == NT - 1), skip_group_check=True), ('ohb%d' % b, 'idg'), ('pid',))
                            first = False
                        n += 1
                P.op('dve', lambda e: e.tensor_reduce(out=idf[:], in_=pid[:].rearrange("p (n c) -> p n c", c=4)[:, :, 0:2], axis=AX.X, op=ALU.add), ('pid',), ('idf',))
                P.op('dve', lambda e: e.tensor_reduce(out=gall[:], in_=pid[:].rearrange("p (n c) -> p n c", c=4)[:, :, 2:4], axis=AX.X, op=ALU.add), ('pid',), ('gall',))
                P.op('dve', lambda e: e.tensor_copy(out=idx_i[:], in_=idf[:]), ('idf',), ('idx_i',))
                P.barrier()
            with ExitStack() as sf:
                US = [sb(sf, "US%d" % i, [128, 2, 8, 512], BF16) for i in range(3)]
                VS = [sb(sf, "VS%d" % i, [128, 4, D], BF16) for i in range(NG)]
                xs = [sb(sf, "xs%d" % i, [128, 4, D], BF16) for i in range(2)]
                xsT = [sb(sf, "xsT%d" % i, [128, 8, 512], BF16) for i in range(2)]
                hid = sb(sf, "hid", [128, NF, 512], BF16)
                sg = [sb(sf, "sg%d" % i, [128, 512]) for i in range(2)]
                ysb = [sb(sf, "ysb%d" % i, [128, D]) for i in range(4)]
                pst = ps(sf, "fpst", [128, 1024], BF16)
                ph1 = [ps(sf, "ph1_%d" % i, [128, 512]) for i in range(2)]
                ph3 = [ps(sf, "ph3_%d" % i, [128, 512]) for i in range(2)]
                py = [ps(sf, "fpy%d" % i, [128, 512]) for i in range(2)]
                allxd = tuple(('xd', T) for T in range(NT))

                def load_U(ex, q):
                    u = ex * NG + q
                    slot = u % 3
                    f0, nf = FG[q]
                    for wi_, W in enumerate((exp_w1, exp_w3)):
                        P.dma('pool', lambda e, W=W, wi_=wi_, slot=slot, f0=f0, nf=nf, ex=ex: e.dma_start(
                            out=US[slot][:, wi_, :, 0:nf * 128],
                            in_=W[layer, ex, :, f0 * 128:(f0 + nf) * 128].rearrange("(k p) f -> p k f", p=128)), (), ('US%d' % slot,))

                def load_V(ex, q):
                    f0, nf = FG[q]
                    P.dma('pool', lambda e, q=q, f0=f0, nf=nf, ex=ex: e.dma_start(
                        out=VS[q][:, 0:nf, :], in_=exp_w2[layer, ex, f0 * 128:(f0 + nf) * 128, :].rearrange("(f p) d -> p f d", p=128)),
                        (), ('VS%d' % q,))

                def gathers(ex):
                    b = ex % 2
                    for jt in range(4):
                        col = ex * 4 + jt
                        P.dma('pool', lambda e, b=b, jt=jt, col=col: e.indirect_dma_start(
                            out=xs[b][:, jt, :], out_offset=None, in_=h_d[:, :],
                            in_offset=bass.IndirectOffsetOnAxis(ap=idx_i[:, col:col + 1], axis=0)),
                            ('idx_i',), (('xs', b, jt),))

                def transposes(ex, jt):
                    b = ex % 2
                    for k in range(8):
                        P.op('pe', lambda e, k=k: e.transpose(pst[:, k * 128:(k + 1) * 128], xs[b][:, jt, k * 128:(k + 1) * 128], ident_b[:]),
                             (('xs', b, jt), 'ident_b'), ('fpst',))
                    P.op('act', lambda e: e.activation(out=xsT[b][:, :, jt * 128:(jt + 1) * 128],
                                                       in_=pst[:].rearrange("p (k t) -> p k t", k=8), func=AF.Copy), ('fpst',), ('xsT%d' % b,))

                gathers(0)
                for q in range(3):
                    load_U(0, q)
                for q in range(NG):
                    load_V(0, q)
                for jt in range(4):
                    transposes(0, jt)
                for ex in range(E):
                    b = ex % 2
                    for q in range(NG):
                        u = ex * NG + q
                        slot = u % 3
                        f0, nf = FG[q]
                        for fl in range(nf):
                            f = f0 + fl
                            pb_ = f % 2
                            for k in range(8):
                                P.op('pe', lambda e, slot=slot, fl=fl, k=k, pb_=pb_: e.matmul(
                                    ph1[pb_][:], lhsT=US[slot][:, 0, k, fl * 128:(fl + 1) * 128], rhs=xsT[b][:, k, :], start=(k == 0), stop=(k == 7)),
                                    ('US%d' % slot, 'xsT%d' % b), ('ph1_%d' % pb_,))
                            for k in range(8):
                                P.op('pe', lambda e, slot=slot, fl=fl, k=k, pb_=pb_: e.matmul(
                                    ph3[pb_][:], lhsT=US[slot][:, 1, k, fl * 128:(fl + 1) * 128], rhs=xsT[b][:, k, :], start=(k == 0), stop=(k == 7)),
                                    ('US%d' % slot, 'xsT%d' % b), ('ph3_%d' % pb_,))
                            P.op('act', lambda e, pb_=pb_: e.activation(out=sg[pb_][:], in_=ph1[pb_][:], func=AF.Silu), ('ph1_%d' % pb_,), ('sg%d' % pb_,))
                            P.op('dve', lambda e, pb_=pb_, f=f: e.tensor_tensor(out=hid[:, f, :], in0=ph3[pb_][:], in1=sg[pb_][:], op=ALU.mult),
                                 ('ph3_%d' % pb_, 'sg%d' % pb_), (('hid', f),))
                        un = u + 3
                        if un < E * NG:
                            load_U(un // NG, un % NG)
                        if q == 0 and ex + 1 < E:
                            gathers(ex + 1)
                        if q == 2 and ex > 0:
                            for qq in range(NG):
                                load_V(ex, qq)
                    gi = 0
                    for jt in range(4):
                        for hh in range(2):
                            for f in range(NF):
                                q = FQ[f]
                                fl = f - FG[q][0]
                                P.op('pe', lambda e, jt=jt, hh=hh, f=f, q=q, fl=fl: e.matmul(
                                    py[hh][:], lhsT=hid[:, f, jt * 128:(jt + 1) * 128], rhs=VS[q][:, fl, hh * 512:(hh + 1) * 512],
                                    start=(f == 0), stop=(f == NF - 1)), (('hid', f), 'VS%d' % q), ('fpy%d' % hh,))
                            P.op('dve', lambda e, jt=jt, hh=hh, ex=ex: e.scalar_tensor_tensor(
                                out=ysb[jt][:, hh * 512:(hh + 1) * 512], in0=py[hh][:], scalar=gall[:, ex * 4 + jt:ex * 4 + jt + 1],
                                in1=modb[:, 5, hh * 512:(hh + 1) * 512], op0=ALU.mult, op1=ALU.mult),
                                ('fpy%d' % hh, 'gall', 'mod'), ('ysb%d' % jt,))
                            if ex + 1 < E and gi >= 4:
                                transposes(ex + 1, gi - 4)
                            gi += 1
                        col = ex * 4 + jt
                        P.dma('pool', lambda e, jt=jt, col=col: e.indirect_dma_start(
                            out=out_d[:, :], out_offset=bass.IndirectOffsetOnAxis(ap=idx_i[:, col:col + 1], axis=0),
                            in_=ysb[jt][:, :], in_offset=None, compute_op=ALU.add),
                            ('ysb%d' % jt, 'idx_i'), allxd)
                P.barrier()

        if 'm0' in phases:
            mixer0()
        if 'f0' in phases:
            ffn(0)
        if 'm1' in phases:
            mixer1()
        if 'f1' in phases:
            ffn(1)
        P.barrier()
    return nc


def _consts():
    n = N
    rows = n // 64
    row_idx = np.repeat(np.arange(rows, dtype=np.float32), 64)
    col_idx = np.tile(np.arange(64, dtype=np.float32), rows)
    inv_freq = (np.float32(10000.0) ** (-np.arange(32, dtype=np.float32) / np.float32(32))).astype(np.float32)
    ang_r = (row_idx[None, :] * inv_freq[:, None]).astype(np.float32)
    ang_c = (col_idx[None, :] * inv_freq[:, None]).astype(np.float32)
    cos_t = np.concatenate([np.cos(ang_r), np.cos(ang_r), np.cos(ang_c), np.cos(ang_c)], 0).astype(np.float32)
    sin_t = np.concatenate([-np.sin(ang_r), np.sin(ang_r), -np.sin(ang_c), np.sin(ang_c)], 0).astype(np.float32)
    ident = np.eye(128, dtype=np.float32)
    tri = np.triu(np.ones((128, 128), dtype=np.float32))
    iota = np.tile(np.arange(512, dtype=np.float32)[None, :], (128, 1))
    idcols = np.zeros((128, 32, 2), dtype=np.float32)
    idcols[:, :, 0] = np.arange(128, dtype=np.float32)[:, None]
    idcols[:, :, 1] = (128.0 * np.arange(32, dtype=np.float32))[None, :]
    return dict(cos_t=np.ascontiguousarray(cos_t), sin_t=np.ascontiguousarray(sin_t), ident=ident, tri=tri, iota=iota,
                idcols=np.ascontiguousarray(idcols.reshape(128, 64)))


def _swap_perm():
    p = np.arange(128)
    blk = p // 32
    return (blk ^ 1) * 32 + (p % 32)


def prepare_shared(inputs):
    f = lambda a: np.ascontiguousarray(np.asarray(a, dtype=np.float32))
    perm = _swap_perm()
    w = np.asarray(inputs['ab_w_in'][0], dtype=np.float32)
    qsw = np.concatenate([w[:, h * 128:(h + 1) * 128][:, perm] for h in range(4)], 1)
    ksw = np.concatenate([w[:, 512 + h * 128:512 + (h + 1) * 128][:, perm] for h in range(2)], 1)
    w_in_ext = np.concatenate([w, qsw, ksw], 1)
    qn = np.asarray(inputs['ab_q_norm'][0], dtype=np.float32); kn = np.asarray(inputs['ab_k_norm'][0], dtype=np.float32)
    gains = np.stack([qn, qn[perm], kn, kn[perm]], 1)
    cw = np.asarray(inputs['ab_conv_w'][0], dtype=np.float32)
    convw = cw.reshape(3, 4, 128).transpose(2, 1, 0).reshape(128, 12)
    sgw = np.asarray(inputs['sg_w'][0], dtype=np.float32)
    sgwT = sgw.transpose(2, 0, 1).reshape(128, 8 * 128)
    sgb = np.asarray(inputs['sg_b'][0], dtype=np.float32).T
    cctx = np.asarray(inputs['c_ctx'], dtype=np.float32)
    sh = dict(
        cccol=f(cctx.reshape(8, 128).T), ada_w=f(inputs['ada_w']), ada_b=f(inputs['ada_b']),
        w_in=f(w_in_ext), gains=f(gains), convw=f(convw), w_out0=f(inputs['ab_w_out'][0]),
        sg_w_in=f(inputs['sg_w_in'][0]), sg_norm=f(np.asarray(inputs['sg_norm'][0]).reshape(1, D)),
        sgwT=f(sgwT), sgb=f(sgb), sg_w_out=f(inputs['sg_w_out'][0]), router_w=f(inputs['router_w']),
        exp_w1=f(inputs['exp_w1']), exp_w3=f(inputs['exp_w3']), exp_w2=f(inputs['exp_w2']),
    )
    sh.update(_consts())
    return sh


def core_inputs(inputs, shared, b):
    m = dict(shared)
    m['x'] = np.ascontiguousarray(np.asarray(inputs['x'][b], dtype=np.float32))
    m['ctx'] = np.ascontiguousarray(np.asarray(inputs['ctx'][b], dtype=np.float32))
    m['ccol'] = np.ascontiguousarray(np.asarray(inputs['c'][b], dtype=np.float32).reshape(8, 128).T)
    return m


def kernel(**inputs):
    nb = inputs['x'].shape[0]
    shared = prepare_shared(inputs)
    nc = build()
    in_maps = [core_inputs(inputs, shared, b) for b in range(nb)]
    res = run_bass_kernel_spmd(nc, in_maps, core_ids=list(range(nb)))
    return np.stack([np.asarray(r['out'], dtype=np.float32) for r in res.results], 0)
```
